# Optimizing a Trainium2 kernel written in Bass

```python
import math
import jax
import jax.numpy as jnp
from jax import lax
import numpy as np

D_MODEL = 1024
BATCH = 16
SEQ = 2048
DEPTH = 4

CHUNK = 64
Q_BLOCK = 128
HEAD_DIM = 64
CONV_CH = D_MODEL // 2
CONV_K = 3
FOX_HEADS = (D_MODEL // 2) // HEAD_DIM
FOX_WIDTH = FOX_HEADS * HEAD_DIM
DIFF_HEADS = D_MODEL // (2 * HEAD_DIM)
N_GROUPS = 4
EXPERTS_PER_GROUP = 8
N_EXPERTS = N_GROUPS * EXPERTS_PER_GROUP
TOP_K = 2
D_EXPERT = D_MODEL // 2
MOE_BLOCK = 128
N_EVEN = (DEPTH + 1) // 2
N_ODD = DEPTH // 2
EVEN_IN = 3 * CONV_CH + 3 * FOX_WIDTH + FOX_HEADS
ODD_IN = 3 * D_MODEL
ALPHA = (2.0 * DEPTH) ** 0.25
BETA = (8.0 * DEPTH) ** -0.25
LN_EPS = 1e-5
RMS_EPS = 1e-5
FORGET_BIAS = 3.0
NEG_INF = -1e30

kernel_name = 'hybrid_chunk_stream_block'


def layer_norm(x, g, b):
    xf = x.astype(jnp.float32)
    mu = jnp.mean(xf, axis=-1, keepdims=True)
    var = jnp.mean(jnp.square(xf - mu), axis=-1, keepdims=True)
    return ((xf - mu) * lax.rsqrt(var + LN_EPS) * g + b).astype(x.dtype)


def short_gated_conv(xv, gate_b, gate_c, conv_w):
    u = gate_c * xv
    seq = u.shape[1]
    up = jnp.pad(u, ((0, 0), (CONV_K - 1, 0), (0, 0)))
    y = up[:, 0:seq] * conv_w[:, 0]
    for j in range(1, CONV_K):
        y = y + up[:, j:j + seq] * conv_w[:, j]
    return gate_b * y


def forgetting_attention(q, k, v, f_logit):
    seq = q.shape[1]
    scale = HEAD_DIM ** -0.5
    c = jnp.cumsum(jax.nn.log_sigmoid(f_logit.astype(jnp.float32)), axis=1)
    c = jnp.transpose(c, (0, 2, 1))
    outs = []
    for blk in range(seq // Q_BLOCK):
        q_lo, q_hi = blk * Q_BLOCK, (blk + 1) * Q_BLOCK
        t = jnp.arange(q_lo, q_hi)[:, None]
        s = jnp.arange(q_hi)[None, :]
        logits = jnp.einsum('bqhd,bkhd->bhqk', q[:, q_lo:q_hi], k[:, :q_hi],
                            preferred_element_type=jnp.float32) * scale
        logits = logits + c[:, :, q_lo:q_hi, None] - c[:, :, None, :q_hi]
        p = jax.nn.softmax(jnp.where(s <= t, logits, NEG_INF), axis=-1)
        outs.append(jnp.einsum('bhqk,bkhd->bqhd', p.astype(v.dtype), v[:, :q_hi]))
    return jnp.concatenate(outs, axis=1)


def alibi_slopes(n_heads):
    return jnp.asarray(2.0 ** (-8.0 * np.arange(1, n_heads + 1) / n_heads), dtype=jnp.float32)


def chunk_block_probs(q, k, bias, visible, q_lo, q_hi):
    scale = HEAD_DIM ** -0.5
    logits = jnp.einsum('bqhd,bkhd->bhqk', q[:, q_lo:q_hi], k[:, :q_hi],
                        preferred_element_type=jnp.float32) * scale + bias
    return jax.nn.softmax(jnp.where(visible, logits, NEG_INF), axis=-1)


def differential_attention(q1, q2, k1, k2, v, lam, lam_init, subln_g):
    seq = q1.shape[1]
    slopes = alibi_slopes(DIFF_HEADS)[:, None, None]
    outs = []
    for blk in range(seq // Q_BLOCK):
        q_lo, q_hi = blk * Q_BLOCK, (blk + 1) * Q_BLOCK
        t = jnp.arange(q_lo, q_hi)[:, None]
        s = jnp.arange(q_hi)[None, :]
        bias = -slopes * jnp.abs(t - s).astype(jnp.float32)
        visible = (s // CHUNK) <= (t // CHUNK)
        p = (chunk_block_probs(q1, k1, bias, visible, q_lo, q_hi)
             - lam * chunk_block_probs(q2, k2, bias, visible, q_lo, q_hi))
        outs.append(jnp.einsum('bhqk,bkhe->bqhe', p.astype(v.dtype), v[:, :q_hi]))
    o = jnp.concatenate(outs, axis=1).astype(jnp.float32)
    o = o * lax.rsqrt(jnp.mean(jnp.square(o), axis=-1, keepdims=True) + RMS_EPS)
    return (o * subln_g * (1.0 - lam_init)).astype(v.dtype)


def hierarchical_moe(x, w_group, b_group, w_expert, b_expert, w_gate, w_up, w_down):
    bsz, seq, dm = x.shape
    n_tok = bsz * seq
    xf = x.reshape(n_tok, dm)
    g_logit = (xf @ w_group + b_group).astype(jnp.float32)
    g_prob = jax.nn.softmax(g_logit, axis=-1)
    g_sel = jnp.argmax(g_logit, axis=-1)
    g_w = jnp.take_along_axis(g_prob, g_sel[:, None], axis=-1)[:, 0]
    e_logit = (xf @ w_expert + b_expert).astype(jnp.float32).reshape(n_tok, N_GROUPS, EXPERTS_PER_GROUP)
    e_in = jnp.take_along_axis(e_logit, g_sel[:, None, None], axis=1)[:, 0]
    top_v, top_i = lax.top_k(e_in, TOP_K)
    top_w = jax.nn.softmax(top_v, axis=-1) * g_w[:, None]
    eid = (g_sel[:, None] * EXPERTS_PER_GROUP + top_i).reshape(-1)
    tok = jnp.repeat(jnp.arange(n_tok), TOP_K)
    wt = top_w.reshape(-1)
    order = jnp.argsort(eid)
    s_eid, s_tok, s_wt = eid[order], tok[order], wt[order]
    counts = jnp.bincount(eid, length=N_EXPERTS)
    padded = (counts + MOE_BLOCK - 1) // MOE_BLOCK * MOE_BLOCK
    pad_end = jnp.cumsum(padded)
    pad_start = pad_end - padded
    start = jnp.cumsum(counts) - counts
    dest = pad_start[s_eid] + jnp.arange(eid.shape[0]) - start[s_eid]
    cap = -(-(n_tok * TOP_K) // MOE_BLOCK) * MOE_BLOCK + N_EXPERTS * MOE_BLOCK
    n_blocks = cap // MOE_BLOCK
    xb = jnp.zeros((cap, dm), x.dtype).at[dest].set(xf[s_tok])
    blk_e = jnp.minimum(jnp.searchsorted(pad_end, jnp.arange(n_blocks) * MOE_BLOCK, side='right'),
                        N_EXPERTS - 1)

    def expert_block(args):
        xblk, e = args
        hid = jax.nn.silu(xblk @ w_gate[e]) * (xblk @ w_up[e])
        return hid @ w_down[e]

    yb = lax.map(expert_block, (xb.reshape(n_blocks, MOE_BLOCK, dm), blk_e)).reshape(cap, dm)
    y = jnp.zeros((n_tok, dm), x.dtype).at[s_tok].add(yb[dest] * s_wt[:, None].astype(x.dtype))
    return y.reshape(bsz, seq, dm)


def setup_inputs(seed: int = 0) -> dict:
    key = jax.random.key(seed)
    ks = jax.random.split(key, 24)
    nrm = jax.random.normal
    f32 = jnp.float32
    even_scale = np.ones((EVEN_IN,), np.float32)
    even_scale[2 * CONV_CH:3 * CONV_CH] = BETA
    even_scale[3 * CONV_CH + 2 * FOX_WIDTH:3 * CONV_CH + 3 * FOX_WIDTH] = BETA
    odd_scale = np.ones((ODD_IN,), np.float32)
    odd_scale[2 * D_MODEL:] = BETA
    return {
        'x': nrm(ks[0], (BATCH, SEQ, D_MODEL), f32),
        'ab_w_in': nrm(ks[1], (N_EVEN, D_MODEL, EVEN_IN), f32) * D_MODEL ** -0.5 * jnp.asarray(even_scale),
        'ab_b_forget': FORGET_BIAS + 0.5 * nrm(ks[2], (N_EVEN, FOX_HEADS), f32),
        'ab_conv_w': nrm(ks[3], (N_EVEN, CONV_CH, CONV_K), f32) * CONV_K ** -0.5,
        'ab_w_out': nrm(ks[4], (N_EVEN, D_MODEL, D_MODEL), f32) * D_MODEL ** -0.5 * BETA,
        'c_w_in': nrm(ks[5], (N_ODD, D_MODEL, ODD_IN), f32) * D_MODEL ** -0.5 * jnp.asarray(odd_scale),
        'c_lam_q1': 0.1 * nrm(ks[6], (N_ODD, HEAD_DIM), f32),
        'c_lam_k1': 0.1 * nrm(ks[7], (N_ODD, HEAD_DIM), f32),
        'c_lam_q2': 0.1 * nrm(ks[8], (N_ODD, HEAD_DIM), f32),
        'c_lam_k2': 0.1 * nrm(ks[9], (N_ODD, HEAD_DIM), f32),
        'c_subln_g': 1.0 + 0.02 * nrm(ks[10], (N_ODD, 2 * HEAD_DIM), f32),
        'c_w_out': nrm(ks[11], (N_ODD, D_MODEL, D_MODEL), f32) * D_MODEL ** -0.5 * BETA,
        'ln_mix_g': 1.0 + 0.02 * nrm(ks[12], (DEPTH, D_MODEL), f32),
        'ln_mix_b': 0.02 * nrm(ks[13], (DEPTH, D_MODEL), f32),
        'ln_ffn_g': 1.0 + 0.02 * nrm(ks[14], (DEPTH, D_MODEL), f32),
        'ln_ffn_b': 0.02 * nrm(ks[15], (DEPTH, D_MODEL), f32),
        'moe_w_group': nrm(ks[16], (DEPTH, D_MODEL, N_GROUPS), f32) * D_MODEL ** -0.5,
        'moe_b_group': 0.01 * nrm(ks[17], (DEPTH, N_GROUPS), f32),
        'moe_w_expert': nrm(ks[18], (DEPTH, D_MODEL, N_EXPERTS), f32) * D_MODEL ** -0.5,
        'moe_b_expert': 0.01 * nrm(ks[19], (DEPTH, N_EXPERTS), f32),
        'moe_w_gate': nrm(ks[20], (DEPTH, N_EXPERTS, D_MODEL, D_EXPERT), f32) * D_MODEL ** -0.5,
        'moe_w_up': nrm(ks[21], (DEPTH, N_EXPERTS, D_MODEL, D_EXPERT), f32) * D_MODEL ** -0.5,
        'moe_w_down': nrm(ks[22], (DEPTH, N_EXPERTS, D_EXPERT, D_MODEL), f32) * D_EXPERT ** -0.5 * BETA,
    }


def reference(x, ab_w_in, ab_b_forget, ab_conv_w, ab_w_out, c_w_in, c_lam_q1, c_lam_k1,
              c_lam_q2, c_lam_k2, c_subln_g, c_w_out, ln_mix_g, ln_mix_b, ln_ffn_g, ln_ffn_b,
              moe_w_group, moe_b_group, moe_w_expert, moe_b_expert, moe_w_gate, moe_w_up,
              moe_w_down):
    bsz, seq, _ = x.shape
    for layer in range(DEPTH):
        i = layer // 2
        if layer % 2 == 0:
            h = x @ ab_w_in[i]
            gate_b = h[..., 0:CONV_CH]
            gate_c = h[..., CONV_CH:2 * CONV_CH]
            xv = h[..., 2 * CONV_CH:3 * CONV_CH]
            off = 3 * CONV_CH
            q = h[..., off:off + FOX_WIDTH].reshape(bsz, seq, FOX_HEADS, HEAD_DIM)
            k = h[..., off + FOX_WIDTH:off + 2 * FOX_WIDTH].reshape(bsz, seq, FOX_HEADS, HEAD_DIM)
            v = h[..., off + 2 * FOX_WIDTH:off + 3 * FOX_WIDTH].reshape(bsz, seq, FOX_HEADS, HEAD_DIM)
            f_logit = h[..., off + 3 * FOX_WIDTH:] + ab_b_forget[i]
            a_out = short_gated_conv(xv, gate_b, gate_c, ab_conv_w[i])
            b_out = forgetting_attention(q, k, v, f_logit).reshape(bsz, seq, FOX_WIDTH)
            mix = jnp.concatenate([a_out, b_out], axis=-1) @ ab_w_out[i]
        else:
            h = x @ c_w_in[i]
            q = h[..., 0:D_MODEL].reshape(bsz, seq, DIFF_HEADS, 2, HEAD_DIM)
            k = h[..., D_MODEL:2 * D_MODEL].reshape(bsz, seq, DIFF_HEADS, 2, HEAD_DIM)
            v = h[..., 2 * D_MODEL:].reshape(bsz, seq, DIFF_HEADS, 2 * HEAD_DIM)
            lam_init = 0.8 - 0.6 * math.exp(-0.3 * layer)
            lam = (jnp.exp(jnp.sum(c_lam_q1[i].astype(jnp.float32) * c_lam_k1[i].astype(jnp.float32)))
                   - jnp.exp(jnp.sum(c_lam_q2[i].astype(jnp.float32) * c_lam_k2[i].astype(jnp.float32)))
                   + lam_init)
            o = differential_attention(q[..., 0, :], q[..., 1, :], k[..., 0, :], k[..., 1, :], v,
                                       lam, lam_init, c_subln_g[i])
            mix = o.reshape(bsz, seq, D_MODEL) @ c_w_out[i]
        x = layer_norm(ALPHA * x + mix, ln_mix_g[layer], ln_mix_b[layer])
        ffn = hierarchical_moe(x, moe_w_group[layer], moe_b_group[layer], moe_w_expert[layer],
                               moe_b_expert[layer], moe_w_gate[layer], moe_w_up[layer],
                               moe_w_down[layer])
        x = layer_norm(ALPHA * x + ffn, ln_ffn_g[layer], ln_ffn_b[layer])
    return x
```

```python
import math
from contextlib import ExitStack

import ml_dtypes
import numpy as np

import concourse.bass as bass
import concourse.mybir as mybir
from concourse.bass_utils import run_bass_kernel_spmd

F32 = mybir.dt.float32
BF16 = mybir.dt.bfloat16
I32 = mybir.dt.int32
AF = mybir.ActivationFunctionType
ALU = mybir.AluOpType
AX = mybir.AxisListType

N_CORES = 8
D = 1024
SEQ = 2048
NSEQ = 2
NTOK = NSEQ * SEQ
NT = NTOK // 128
TPS = SEQ // 128
DEPTH = 4
ALPHA = (2.0 * DEPTH) ** 0.25
LN_EPS = 1e-5
RMS_EPS = 1e-5
EVEN_IN = 3080
NEXP = 32
CAP = 512
STRIDE = CAP + 1
NSLOT = NEXP * STRIDE
MODE = "fused"


class Sched:
    COMPUTE = ("pe", "act", "dve", "pool")
    QUEUES = ("sp", "act", "pool")

    def __init__(self, nc, stack, same_engine_sync=True):
        self.nc = nc
        self.same_engine_sync = same_engine_sync
        self.engnames = ("pe", "act", "dve", "pool", "sp")
        self.streams = {e: [] for e in self.engnames}
        self.count = {e: 0 for e in self.COMPUTE}
        self.sem = {e: stack.enter_context(nc.semaphore("sem_" + e)) for e in self.COMPUTE}
        self.spare_sems = [{e: stack.enter_context(nc.semaphore("sem%d_%s" % (i, e))) for e in self.COMPUTE} for i in range(1)]
        nslots = {"sp": 16, "act": 1, "pool": 16}
        self.slots = {}
        for q in self.QUEUES:
            self.slots[q] = [
                {"sem": stack.enter_context(nc.semaphore("dsem_%s_%d" % (q, i))), "total": 0, "id": (q, i)}
                for i in range(nslots[q])
            ]
        self.slot_rr = {q: 0 for q in self.QUEUES}
        self.slot_by_id = {s["id"]: s for q in self.QUEUES for s in self.slots[q]}
        self.known = {e: {} for e in self.engnames}
        self.last_write = {}
        self.readers = {}

    def _deps(self, reads, writes):
        deps = []
        for k in reads:
            ev = self.last_write.get(k)
            if ev is not None:
                deps.append(ev)
        for k in writes:
            ev = self.last_write.get(k)
            if ev is not None:
                deps.append(ev)
            deps.extend(self.readers.get(k, ()))
        return deps

    def _commit(self, ev, reads, writes):
        for k in reads:
            self.readers.setdefault(k, []).append(ev)
        for k in writes:
            self.last_write[k] = ev
            self.readers[k] = []

    def _wait_list(self, eng, deps):
        need = {}
        for kind, key, val in deps:
            if kind == "e" and key == eng and (eng == "pe" or not self.same_engine_sync):
                continue
            sk = (kind, key)
            if self.known[eng].get(sk, 0) >= val:
                continue
            if need.get(sk, 0) < val:
                need[sk] = val
        out = []
        for sk, val in need.items():
            self.known[eng][sk] = val
            sem = self.sem[sk[1]] if sk[0] == "e" else self.slot_by_id[sk[1]]["sem"]
            out.append((sem, val))
        return out

    def op(self, eng, fn, reads=(), writes=()):
        waits = self._wait_list(eng, self._deps(reads, writes))
        self.count[eng] += 1
        ev = ("e", eng, self.count[eng])
        sem = self.sem[eng]

        def emit(e, fn=fn, waits=waits, sem=sem):
            for s, v in waits:
                e.wait_ge(s, v)
            fn(e).then_inc(sem, 1)

        self.streams[eng].append(emit)
        self._commit(ev, reads, writes)
        return ev

    def dma(self, queue, fn, reads=(), writes=()):
        slots = self.slots[queue]
        slot = slots[self.slot_rr[queue] % len(slots)]
        self.slot_rr[queue] += 1
        deps = self._deps(reads, writes)
        if slot["total"] > 0:
            deps.append(("d", slot["id"], slot["total"]))
        waits = self._wait_list(queue, deps)
        slot["total"] += 16
        ev = ("d", slot["id"], slot["total"])
        sem = slot["sem"]

        def emit(e, fn=fn, waits=waits, sem=sem):
            for s, v in waits:
                e.wait_ge(s, v)
            fn(e).then_inc(sem, 16)

        self.streams[queue].append(emit)
        self._commit(ev, reads, writes)
        return ev

    def drain(self):
        waits = []
        for q in self.QUEUES:
            for s in self.slots[q]:
                if s["total"] > 0:
                    waits.append((s["sem"], s["total"]))
                    for e in self.engnames:
                        self.known[e][("d", s["id"])] = s["total"]
        for c in self.COMPUTE:
            if self.count[c] > 0:
                waits.append((self.sem[c], self.count[c]))
                for e in self.engnames:
                    self.known[e][("e", c)] = self.count[c]

        def emit(e, waits=waits):
            for s, v in waits:
                e.wait_ge(s, v)

        self.streams["sp"].append(emit)
        self.last_write = {}
        self.readers = {}

    def rotate_engine_sems(self):
        if not self.spare_sems:
            return
        self.sem = self.spare_sems.pop(0)
        for c in self.COMPUTE:
            self.count[c] = 0
            for e in self.engnames:
                self.known[e].pop(("e", c), None)

    def reset_engine_sems(self):
        nc = self.nc
        sem = self.sem
        with nc.Block() as block:
            @block.tensor
            def _(e):
                e.sem_clear(sem["pe"])

            @block.scalar
            def _(e):
                e.sem_clear(sem["act"])

            @block.vector
            def _(e):
                e.sem_clear(sem["dve"])

            @block.gpsimd
            def _(e):
                e.sem_clear(sem["pool"])
        for c in self.COMPUTE:
            self.count[c] = 0
            for e in self.engnames:
                self.known[e].pop(("e", c), None)

    def emit_block(self):
        nc = self.nc
        streams = self.streams
        with nc.Block() as block:
            @block.tensor
            def _(e):
                for f in streams["pe"]:
                    f(e)

            @block.scalar
            def _(e):
                for f in streams["act"]:
                    f(e)

            @block.vector
            def _(e):
                for f in streams["dve"]:
                    f(e)

            @block.gpsimd
            def _(e):
                for f in streams["pool"]:
                    f(e)

            @block.sync
            def _(e):
                for f in streams["sp"]:
                    f(e)
        self.streams = {e: [] for e in self.engnames}


def host_consts():
    bf = ml_dtypes.bfloat16
    c = {}
    c["ident_f"] = np.eye(128, dtype=np.float32)
    c["ident_b"] = np.eye(128, dtype=np.float32).astype(bf)
    k = np.arange(128)[:, None]
    q = np.arange(128)[None, :]
    c["tri"] = (k <= q).astype(np.float32).astype(bf)
    c["ones_b"] = np.ones((128, 128), np.float32).astype(bf)
    m12 = np.zeros((32, 2), np.float32)
    m12[0:8, 0] = -1.0
    m12[8:16, 1] = -1.0
    m12[16:24, 0] = 1.0
    m12[24:32, 1] = 1.0
    c["m12"] = m12
    slopes = 2.0 ** (-8.0 * np.arange(1, 9) / 8.0)
    pos = np.arange(SEQ)
    a = (pos // 64).astype(np.float64)
    b = (pos % 64).astype(np.float64)
    augq = np.zeros((8, 4, SEQ), np.float64)
    augk = np.zeros((8, 4, SEQ), np.float64)
    corr = np.zeros((128, 8, 128), np.float64)
    for h in range(8):
        s = slopes[h]
        augq[h, 0] = -8.0 * s * 64.0 * a
        augq[h, 1] = -8.0 * s * b
        augq[h, 2] = 1.0
        augq[h, 3] = 1.0
        augk[h, 0] = 1.0
        augk[h, 1] = 1.0
        augk[h, 2] = 8.0 * s * 64.0 * a
        augk[h, 3] = 8.0 * s * b
        vis = (k // 64) <= (q // 64)
        cc = np.where(k > q, np.exp(-2.0 * s * (k - q)), 1.0)
        corr[:, h, :] = np.where(vis, cc, 0.0)
    c["augq"] = augq.astype(np.float32).astype(bf)
    c["augk"] = augk.astype(np.float32).astype(bf)
    c["corr"] = corr.astype(np.float32).astype(bf)
    c["basem1"] = np.broadcast_to((np.arange(NEXP) * STRIDE - 1).astype(np.float32)[None, :], (128, NEXP)).copy()
    return c


CONST_SPECS = {
    "ident_f": ([128, 128], F32), "ident_b": ([128, 128], BF16), "tri": ([128, 128], BF16),
    "ones_b": ([128, 128], BF16), "m12": ([32, 2], F32), "augq": ([8, 4, SEQ], BF16),
    "augk": ([8, 4, SEQ], BF16), "corr": ([128, 8, 128], BF16), "basem1": ([128, NEXP], F32),
}


class Ctx:
    pass


def new_phase(C):
    C.st = ExitStack()
    nc = C.nc
    C.phase_no = getattr(C, "phase_no", -1) + 1
    pfx = "f%d_" % C.phase_no
    C.sb = lambda name, shape, dt: C.st.enter_context(nc.sbuf_tensor(pfx + name, shape, dt))
    C.banks = [C.st.enter_context(nc.psum_tensor(pfx + "bank%d" % i, [128, 512], F32)) for i in range(7)]
    C.pTr = C.st.enter_context(nc.psum_tensor(pfx + "pTr", [128, 1024], BF16))


def end_phase(C):
    C.S.drain()
    C.S.emit_block()
    if C.phase_no == 3:
        C.S.rotate_engine_sems()
    C.st.close()


def load_const(C, name, tile_ap, key):
    C.S.dma("sp", lambda e: e.dma_start(out=tile_ap, in_=C.consts[name]), writes=[key])


def cast_load_w(C, dst, src, nk, ncols, key):
    S = C.S
    v = src.rearrange("(k p) n -> p k n", p=128)
    step = 1024 if ncols % 1024 == 0 else (1540 if ncols == 3080 else ncols)
    keys = []
    for k in range(nk):
        for c0 in range(0, ncols, step):
            c1 = min(ncols, c0 + step)
            kk = (key, k, c0)
            keys.append(kk)
            S.dma("pool", lambda e, k=k, c0=c0, c1=c1: e.dma_start(out=dst[:, k, c0:c1], in_=v[:, k, c0:c1]),
                  writes=[kk])
    return keys


def layer_norm_store(C, yt, gbc, bbc, out_ap, ykey, tag):
    S = C.S
    L = C.ln
    S.op("dve", lambda e: e.bn_stats(out=L["stats"][:, 0:6], in_=yt[:, 0:512]), reads=[ykey], writes=["ln_stats"])
    S.op("dve", lambda e: e.bn_stats(out=L["stats"][:, 6:12], in_=yt[:, 512:1024]), reads=[ykey], writes=["ln_stats"])
    S.op("dve", lambda e: e.bn_aggr(out=L["mv"][:], in_=L["stats"][:]), reads=["ln_stats"], writes=["ln_mv"])
    S.op("act", lambda e: e.activation(out=L["rstd"][:], in_=L["mv"][:, 1:2], func=AF.Ln, bias=L["eps"][:], scale=1.0),
         reads=["ln_mv", "ln_eps"], writes=["ln_rstd"])
    S.op("act", lambda e: e.activation(out=L["rstd"][:], in_=L["rstd"][:], func=AF.Exp, scale=-0.5),
         reads=["ln_rstd"], writes=["ln_rstd"])
    S.op("dve", lambda e: e.tensor_scalar(out=L["nmr"][:], in0=L["mv"][:, 0:1], scalar1=-1.0, scalar2=L["rstd"][:],
                                          op0=ALU.mult, op1=ALU.mult), reads=["ln_mv", "ln_rstd"], writes=["ln_nmr"])
    S.op("act", lambda e: e.activation(out=yt[:], in_=yt[:], func=AF.Identity, bias=L["nmr"][:], scale=L["rstd"][:]),
         reads=[ykey, "ln_nmr", "ln_rstd"], writes=[ykey])
    S.op("dve", lambda e: e.tensor_tensor(out=yt[:], in0=yt[:], in1=gbc[:], op=ALU.mult), reads=[ykey, "gbc"], writes=[ykey])
    S.op("dve", lambda e: e.tensor_tensor(out=yt[:], in0=yt[:], in1=bbc[:], op=ALU.add), reads=[ykey, "bbc"], writes=[ykey])
    S.dma("sp", lambda e: e.dma_start(out=out_ap, in_=yt[:]), reads=[ykey], writes=[("xout", tag)])


def alloc_ln(C):
    sb = C.sb
    C.ln = {"stats": sb("ln_stats", [128, 12], F32), "mv": sb("ln_mv", [128, 2], F32),
            "rstd": sb("ln_rstd", [128, 1], F32), "nmr": sb("ln_nmr", [128, 1], F32),
            "eps": sb("ln_eps", [128, 1], F32)}
    C.S.op("dve", lambda e: e.memset(C.ln["eps"][:], LN_EPS), writes=["ln_eps"])


def build_xT(C, x_in, s, xT, ident_f, xs):
    S = C.S
    for t in range(TPS):
        tt = s * TPS + t
        xb = xs[t % 2]
        xkey = ("xs", t % 2)
        S.dma("sp", lambda e, tt=tt, xb=xb: e.dma_start(out=xb[:], in_=x_in[tt * 128:(tt + 1) * 128, :]), writes=[xkey])
        for half in range(2):
            bk = C.banks[5 + half]
            bkey = ("bank", 5 + half)
            for j in range(4):
                k = half * 4 + j
                S.op("pe", lambda e, bk=bk, j=j, k=k, xb=xb: e.transpose(out=bk[:, j * 128:(j + 1) * 128],
                                                                         in_=xb[:, k * 128:(k + 1) * 128], identity=ident_f[:]),
                     reads=[xkey, "ident_f"], writes=[bkey])
            eng = "dve" if half == 0 else "act"
            if eng == "dve":
                S.op("dve", lambda e, bk=bk, half=half, t=t: e.tensor_copy(
                    out=xT[:, half * 4:half * 4 + 4, t * 128:(t + 1) * 128],
                    in_=bk[:, :].rearrange("p (j c) -> p j c", j=4)), reads=[bkey], writes=["xT"])
            else:
                S.op("act", lambda e, bk=bk, half=half, t=t: e.copy(
                    out=xT[:, half * 4:half * 4 + 4, t * 128:(t + 1) * 128],
                    in_=bk[:, :].rearrange("p (j c) -> p j c", j=4)), reads=[bkey], writes=["xT"])


def outproj_ln(C, x_in, x_out, s, catT, Wout, wout_keys, gbc, bbc, xs, yts):
    S = C.S
    for t in range(TPS):
        tt = s * TPS + t
        xb = xs[t % 2]
        xkey = ("xs", t % 2)
        yt = yts[t % len(yts)]
        ykey = ("yt", t % len(yts))
        S.dma("sp", lambda e, tt=tt, xb=xb: e.dma_start(out=xb[:], in_=x_in[tt * 128:(tt + 1) * 128, :]), writes=[xkey])
        for half in range(2):
            bk = C.banks[5 + half]
            bkey = ("bank", 5 + half)
            for k in range(8):
                S.op("pe", lambda e, bk=bk, k=k, t=t, half=half: e.matmul(
                    bk[:, :], lhsT=catT[:, k, t * 128:(t + 1) * 128], rhs=Wout[:, k, half * 512:(half + 1) * 512],
                    start=(k == 0), stop=(k == 7)), reads=["catT"] + wout_keys, writes=[bkey])
            S.op("dve", lambda e, bk=bk, half=half, xb=xb, yt=yt: e.scalar_tensor_tensor(
                out=yt[:, half * 512:(half + 1) * 512], in0=xb[:, half * 512:(half + 1) * 512], scalar=ALPHA,
                in1=bk[:, :], op0=ALU.mult, op1=ALU.add), reads=[xkey, bkey], writes=[ykey])
        layer_norm_store(C, yt, gbc, bbc, x_out[tt * 128:(tt + 1) * 128, :], ykey, tt)


def emit_even(C, x_in, x_out, w_in, b_forget, conv_w, w_out, ln_g, ln_b):
    new_phase(C)
    S, sb, nc = C.S, C.sb, C.nc
    B = C.banks
    Win = sb("Win", [128, 8, EVEN_IN], BF16)
    Wout = sb("Wout", [128, 8, D], BF16)
    Wf = sb("Wf", [128, 8, 32], BF16)
    cw = sb("cw", [128, 4, 3], F32)
    bf32 = sb("bf32", [32, 1], F32)
    gbc = sb("gbc", [128, D], F32)
    bbc = sb("bbc", [128, D], F32)
    ident_f = sb("ident_f", [128, 128], F32)
    ident_b = sb("ident_b", [128, 128], BF16)
    tri = sb("tri", [128, 128], BF16)
    m12 = sb("m12", [32, 2], F32)
    xT = sb("xT", [128, 8, SEQ], BF16)
    catT = sb("catT", [128, 8, SEQ], BF16)
    qa = [sb("qa%d" % i, [68, SEQ], BF16) for i in range(2)]
    ka = [sb("ka%d" % i, [68, SEQ], BF16) for i in range(2)]
    Vh = [sb("Vh%d" % i, [128, TPS, 65], BF16) for i in range(2)]
    aug32 = sb("aug32", [32, SEQ], BF16)
    fs = [sb("fs%d" % i, [32, 512], F32) for i in range(3)]
    fh = sb("fh", [32, 512], BF16)
    pt = [sb("pt%d" % i, [128, 512], BF16) for i in range(3)]
    u = sb("u", [128, SEQ + 2], F32)
    ytmp = sb("ytmp", [128, 512], F32)
    Csb = sb("Csb", [128, 512], F32)
    opair = sb("opair", [128, TPS, 128], BF16)
    rec = sb("rec", [128, 4], F32)
    xs = [sb("xs%d" % i, [128, D], F32) for i in range(2)]
    yts = [sb("yt0", [128, D], F32)]
    carry = sb("carry", [32, 1], F32)
    alloc_ln(C)

    win_keys = cast_load_w(C, Win, w_in, 8, EVEN_IN, "Win")
    wout_keys = cast_load_w(C, Wout, w_out, 8, D, "Wout")
    load_const(C, "ident_f", ident_f[:], "ident_f")
    load_const(C, "ident_b", ident_b[:], "ident_b")
    load_const(C, "tri", tri[:], "tri")
    load_const(C, "m12", m12[:], "m12")
    S.dma("sp", lambda e: e.dma_start(out=cw[:], in_=conv_w.rearrange("(c p) j -> p c j", p=128)), writes=["cw"])
    for r in range(4):
        S.dma("sp", lambda e, r=r: e.dma_start(out=bf32[r * 8:(r + 1) * 8, :], in_=b_forget.rearrange("(h o) -> h o", o=1)),
              writes=[("bf32", r)])
    S.dma("sp", lambda e: e.dma_start(out=gbc[:], in_=ln_g.partition_broadcast(128)), writes=["gbc"])
    S.dma("sp", lambda e: e.dma_start(out=bbc[:], in_=ln_b.partition_broadcast(128)), writes=["bbc"])
    S.op("dve", lambda e: e.tensor_scalar(out=bf32[:], in0=bf32[:], scalar1=-1.0, scalar2=None, op0=ALU.mult),
         reads=[("bf32", r) for r in range(4)], writes=["nbf"])
    for r in range(4):
        S.op("dve", lambda e, r=r: e.tensor_copy(out=Wf[:, :, r * 8:(r + 1) * 8], in_=Win[:, :, 3072:3080]),
             reads=win_keys, writes=["Wf"])
    S.op("dve", lambda e: e.memset(u[:, 0:2], 0.0), writes=["u"])
    for i in range(2):
        S.op("dve", lambda e, i=i: e.memset(Vh[i][:], 1.0), writes=[("Vh", i)])
        S.op("dve", lambda e, i=i: e.memset(qa[i][64:68, :], 1.0), writes=[("qa_aug", i)])
        S.op("dve", lambda e, i=i: e.memset(ka[i][64:68, :], 1.0), writes=[("ka_aug", i)])

    OFF_Q, OFF_K, OFF_V = 1536, 2048, 2560
    cnt = [0]

    for s in range(NSEQ):
        build_xT(C, x_in, s, xT, ident_f, xs)

        for r in range(4):
            bk = B[r % 2]
            bkey = ("bank", r % 2)
            for k in range(8):
                S.op("pe", lambda e, bk=bk, k=k, r=r: e.matmul(bk[0:32, :], lhsT=Wf[:, k, :], rhs=xT[:, k, r * 512:(r + 1) * 512],
                                                              start=(k == 0), stop=(k == 7)), reads=["Wf", "xT"], writes=[bkey])
            S.op("act", lambda e, bk=bk: e.activation(out=fs[0][:], in_=bk[0:32, :], func=AF.Exp, bias=bf32[:], scale=-1.0),
                 reads=[bkey, "nbf"], writes=["fs0"])
            S.op("act", lambda e: e.activation(out=fs[0][:], in_=fs[0][:], func=AF.Ln, bias=1.0, scale=1.0),
                 reads=["fs0"], writes=["fs0"])
            S.op("dve", lambda e: e.tensor_scalar(out=fs[0][:], in0=fs[0][:], scalar1=8.0, scalar2=None, op0=ALU.mult),
                 reads=["fs0"], writes=["fs0"])
            if r == 0:
                S.op("dve", lambda e: e.memset(fs[2][:], 1.0), writes=["fs2"])
                S.op("dve", lambda e: e.memset(carry[:], 0.0), writes=["carry"])
            else:
                S.op("dve", lambda e: e.tensor_copy(out=carry[:], in_=fs[1][:, 511:512]), reads=["fs1"], writes=["carry"])
            S.op("dve", lambda e: e.tensor_tensor_scan(out=fs[1][:], data0=fs[2][:], data1=fs[0][:], initial=carry[:],
                                                       op0=ALU.mult, op1=ALU.add), reads=["fs0", "fs2", "carry"], writes=["fs1"])
            S.op("dve", lambda e: e.tensor_copy(out=fh[:], in_=fs[1][:]), reads=["fs1"], writes=["fh"])
            S.op("dve", lambda e: e.tensor_tensor(out=fs[0][:], in0=fs[1][:], in1=fh[:], op=ALU.subtract),
                 reads=["fs1", "fh"], writes=["fs0"])
            S.op("dve", lambda e: e.tensor_scalar(out=fs[0][:], in0=fs[0][:], scalar1=m12[:, 1:2], scalar2=None, op0=ALU.mult),
                 reads=["fs0", "m12"], writes=["fs0"])
            S.op("dve", lambda e, r=r: e.scalar_tensor_tensor(out=aug32[:, r * 512:(r + 1) * 512], in0=fh[:], scalar=m12[:, 0:1],
                                                              in1=fs[0][:], op0=ALU.mult, op1=ALU.add),
                 reads=["fh", "fs0", "m12"], writes=["aug32"])

        for c in range(4):
            for r in range(4):
                pb, pc, px = B[0 + (r % 2)], B[2 + (r % 2)], B[5 + (r % 2)]
                kb, kc, kx = ("bank", r % 2), ("bank", 2 + r % 2), ("bank", 5 + r % 2)
                for (bk, bkey, col0) in ((pb, kb, 0), (pc, kc, 512), (px, kx, 1024)):
                    for k in range(8):
                        S.op("pe", lambda e, bk=bk, k=k, col0=col0, c=c, r=r: e.matmul(
                            bk[:, :], lhsT=Win[:, k, col0 + c * 128:col0 + (c + 1) * 128], rhs=xT[:, k, r * 512:(r + 1) * 512],
                            start=(k == 0), stop=(k == 7)), reads=win_keys + ["xT"], writes=[bkey])
                S.op("act", lambda e, pc=pc: e.copy(out=Csb[:], in_=pc[:, :]), reads=[kc], writes=["Csb"])
                S.op("dve", lambda e, px=px, r=r: e.tensor_tensor(out=u[:, 2 + r * 512:2 + (r + 1) * 512], in0=px[:, :], in1=Csb[:],
                                                                 op=ALU.mult), reads=[kx, "Csb"], writes=["u"])
                S.op("dve", lambda e, c=c, r=r: e.tensor_scalar(out=ytmp[:], in0=u[:, 2 + r * 512:2 + (r + 1) * 512],
                                                                scalar1=cw[:, c, 2:3], scalar2=None, op0=ALU.mult),
                     reads=["u", "cw"], writes=["ytmp"])
                S.op("dve", lambda e, c=c, r=r: e.scalar_tensor_tensor(out=ytmp[:], in0=u[:, 1 + r * 512:1 + (r + 1) * 512],
                                                                       scalar=cw[:, c, 1:2], in1=ytmp[:], op0=ALU.mult, op1=ALU.add),
                     reads=["u", "cw", "ytmp"], writes=["ytmp"])
                S.op("dve", lambda e, c=c, r=r: e.scalar_tensor_tensor(out=ytmp[:], in0=u[:, r * 512:(r + 1) * 512],
                                                                       scalar=cw[:, c, 0:1], in1=ytmp[:], op0=ALU.mult, op1=ALU.add),
                     reads=["u", "cw", "ytmp"], writes=["ytmp"])
                S.op("dve", lambda e, pb=pb, c=c, r=r: e.tensor_tensor(out=catT[:, c, r * 512:(r + 1) * 512], in0=pb[:, :], in1=ytmp[:],
                                                                      op=ALU.mult), reads=[kb, "ytmp"], writes=["catT"])

        def inproj(h):
            par = h % 2
            S.dma("sp", lambda e: e.dma_start(out=qa[par][64:65, :], in_=aug32[h:h + 1, :]), reads=["aug32"], writes=[("qa_aug", par)])
            S.dma("sp", lambda e: e.dma_start(out=qa[par][65:66, :], in_=aug32[8 + h:9 + h, :]), reads=["aug32"], writes=[("qa_aug2", par)])
            S.dma("sp", lambda e: e.dma_start(out=ka[par][66:67, :], in_=aug32[16 + h:17 + h, :]), reads=["aug32"], writes=[("ka_aug", par)])
            S.dma("sp", lambda e: e.dma_start(out=ka[par][67:68, :], in_=aug32[24 + h:25 + h, :]), reads=["aug32"], writes=[("ka_aug2", par)])
            for (dst, dkey, off) in ((qa[par], ("qa", par), OFF_Q), (ka[par], ("ka", par), OFF_K)):
                for r in range(4):
                    bi = cnt[0] % 2
                    cnt[0] += 1
                    bk, bkey = B[bi], ("bank", bi)
                    for k in range(8):
                        S.op("pe", lambda e, bk=bk, k=k, off=off, r=r: e.matmul(
                            bk[0:64, :], lhsT=Win[:, k, off + h * 64:off + (h + 1) * 64], rhs=xT[:, k, r * 512:(r + 1) * 512],
                            start=(k == 0), stop=(k == 7)), reads=win_keys + ["xT"], writes=[bkey])
                    S.op("dve", lambda e, bk=bk, dst=dst, r=r: e.tensor_copy(out=dst[0:64, r * 512:(r + 1) * 512], in_=bk[0:64, :]),
                         reads=[bkey], writes=[dkey])
            for g in range(2):
                bi = cnt[0] % 2
                cnt[0] += 1
                bk, bkey = B[bi], ("bank", bi)
                for tl in range(8):
                    t = g * 8 + tl
                    for k in range(8):
                        S.op("pe", lambda e, bk=bk, k=k, t=t, tl=tl: e.matmul(
                            bk[:, tl * 64:(tl + 1) * 64], lhsT=xT[:, k, t * 128:(t + 1) * 128],
                            rhs=Win[:, k, OFF_V + h * 64:OFF_V + (h + 1) * 64], start=(k == 0), stop=(k == 7)),
                            reads=win_keys + ["xT"], writes=[bkey])
                S.op("act", lambda e, bk=bk, g=g: e.copy(out=Vh[par][:, g * 8:(g + 1) * 8, 0:64],
                                                         in_=bk[:, :].rearrange("p (t c) -> p t c", t=8)),
                     reads=[bkey], writes=[("Vh", par)])

        def attn(h):
            par = h % 2
            qk_reads = [("qa", par), ("ka", par), ("qa_aug", par), ("qa_aug2", par), ("ka_aug", par), ("ka_aug2", par)]
            for qb in range(4):
                O = B[2 + (qb % 2)]
                okey = ("bank", 2 + qb % 2)
                for j in range(4 * qb + 4):
                    d = j - 4 * qb
                    n0 = max(0, d) * 128
                    si = cnt[0] % 2
                    pi = cnt[0] % 3
                    cnt[0] += 1
                    Sb, skey = B[5 + si], ("bank", 5 + si)
                    P, pkey = pt[pi], ("pt", pi)
                    S.op("pe", lambda e, Sb=Sb, j=j, qb=qb, n0=n0: e.matmul(
                        Sb[:, n0:512], lhsT=ka[par][0:68, j * 128:(j + 1) * 128], rhs=qa[par][0:68, qb * 512 + n0:(qb + 1) * 512],
                        start=True, stop=True), reads=qk_reads, writes=[skey])
                    S.op("act", lambda e, Sb=Sb, P=P, n0=n0: e.activation(out=P[:, n0:512], in_=Sb[:, n0:512], func=AF.Exp, scale=0.125),
                         reads=[skey], writes=[pkey])
                    if d >= 0:
                        S.op("dve", lambda e, P=P, n0=n0: e.tensor_tensor(out=P[:, n0:n0 + 128], in0=P[:, n0:n0 + 128], in1=tri[:],
                                                                          op=ALU.mult), reads=[pkey, "tri"], writes=[pkey])
                    for i in range(max(0, d), 4):
                        S.op("pe", lambda e, O=O, P=P, i=i, j=j, qb=qb: e.matmul(
                            O[:, i * 65:(i + 1) * 65], lhsT=P[:, i * 128:(i + 1) * 128], rhs=Vh[par][:, j, :],
                            start=(j == 0), stop=(j == 4 * qb + i)), reads=[pkey, ("Vh", par)], writes=[okey])
                S.op("dve", lambda e, O=O: e.reciprocal(out=rec[:, 0:4], in_=O[:, 0:260].rearrange("p (i c) -> p i c", i=4)[:, :, 64]),
                     reads=[okey], writes=["rec"])
                for i in range(4):
                    S.op("dve", lambda e, O=O, i=i, qb=qb: e.tensor_scalar(
                        out=opair[:, qb * 4 + i, (h % 2) * 64:(h % 2) * 64 + 64], in0=O[:, i * 65:i * 65 + 64],
                        scalar1=rec[:, i:i + 1], scalar2=None, op0=ALU.mult), reads=[okey, "rec"], writes=["opair"])
                if h % 2 == 1:
                    for i in range(4):
                        S.op("pe", lambda e, i=i, qb=qb: e.transpose(out=C.pTr[:, i * 128:(i + 1) * 128], in_=opair[:, qb * 4 + i, :],
                                                                     identity=ident_b[:]), reads=["opair", "ident_b"], writes=["pTr"])
                    S.op("act", lambda e, qb=qb: e.copy(out=catT[:, 4 + h // 2, qb * 512:(qb + 1) * 512], in_=C.pTr[:, 0:512]),
                         reads=["pTr"], writes=["catT"])

        inproj(0)
        for h in range(8):
            if h + 1 < 8:
                inproj(h + 1)
            attn(h)

        outproj_ln(C, x_in, x_out, s, catT, Wout, wout_keys, gbc, bbc, xs, yts)
    end_phase(C)


def emit_odd(C, layer, x_in, x_out, w_in, lam_q1, lam_k1, lam_q2, lam_k2, subln_g, w_out, ln_g, ln_b):
    new_phase(C)
    S, sb, nc = C.S, C.sb, C.nc
    B = C.banks
    lam_init = 0.8 - 0.6 * math.exp(-0.3 * layer)
    Win = sb("Win", [128, 8, 3 * D], BF16)
    Wout = sb("Wout", [128, 8, D], BF16)
    gbc = sb("gbc", [128, D], F32)
    bbc = sb("bbc", [128, D], F32)
    ident_f = sb("ident_f", [128, 128], F32)
    ident_b = sb("ident_b", [128, 128], BF16)
    corr = sb("corr", [128, 8, 128], BF16)
    xT = sb("xT", [128, 8, SEQ], BF16)
    catT = sb("catT", [128, 8, SEQ], BF16)
    NQB = 1
    q1a = [sb("q1a%d" % i, [68, SEQ], BF16) for i in range(NQB)]
    q2a = [sb("q2a%d" % i, [68, SEQ], BF16) for i in range(NQB)]
    k1a = [sb("k1a%d" % i, [68, SEQ], BF16) for i in range(NQB)]
    k2a = [sb("k2a%d" % i, [68, SEQ], BF16) for i in range(NQB)]
    Vh = [sb("Vh%d" % i, [128, TPS, 129], BF16) for i in range(2)]
    pt = [sb("pt%d" % i, [128, 512], BF16) for i in range(3)]
    lamv = [sb("lamv%d" % i, [128, 64], F32) for i in range(4)]
    lsc = sb("lsc", [128, 8], F32)
    gv = sb("gv", [128, 128], F32)
    ot = sb("ot", [128, 128], F32)
    junk = sb("junk", [128, 128], F32)
    ob = [sb("ob%d" % i, [128, 128], BF16) for i in range(2)]
    rr = sb("rr", [128, 8], F32)
    xs = [sb("xs%d" % i, [128, D], F32) for i in range(2)]
    yts = [sb("yt0", [128, D], F32)]
    alloc_ln(C)

    win_keys = cast_load_w(C, Win, w_in, 8, 3 * D, "Win")
    wout_keys = cast_load_w(C, Wout, w_out, 8, D, "Wout")
    load_const(C, "ident_f", ident_f[:], "ident_f")
    load_const(C, "ident_b", ident_b[:], "ident_b")
    load_const(C, "corr", corr[:], "corr")
    S.dma("sp", lambda e: e.dma_start(out=gbc[:], in_=ln_g.partition_broadcast(128)), writes=["gbc"])
    S.dma("sp", lambda e: e.dma_start(out=bbc[:], in_=ln_b.partition_broadcast(128)), writes=["bbc"])
    for i, v in enumerate((lam_q1, lam_k1, lam_q2, lam_k2)):
        S.dma("sp", lambda e, i=i, v=v: e.dma_start(out=lamv[i][:], in_=v.partition_broadcast(128)), writes=[("lamv", i)])
    S.dma("sp", lambda e: e.dma_start(out=gv[:], in_=subln_g.partition_broadcast(128)), writes=["gv_raw"])
    S.op("dve", lambda e: e.scalar_tensor_tensor(out=junk[:, 0:64], in0=lamv[0][:], scalar=1.0, in1=lamv[1][:], op0=ALU.mult,
                                                 op1=ALU.mult, accum_out=lsc[:, 0:1]), reads=[("lamv", 0), ("lamv", 1)], writes=["junk", "lsc0"])
    S.op("dve", lambda e: e.scalar_tensor_tensor(out=junk[:, 0:64], in0=lamv[2][:], scalar=1.0, in1=lamv[3][:], op0=ALU.mult,
                                                 op1=ALU.mult, accum_out=lsc[:, 1:2]), reads=[("lamv", 2), ("lamv", 3), "junk"], writes=["junk", "lsc1"])
    S.op("act", lambda e: e.activation(out=lsc[:, 2:4], in_=lsc[:, 0:2], func=AF.Exp), reads=["lsc0", "lsc1"], writes=["lsc23"])
    S.op("dve", lambda e: e.tensor_tensor(out=lsc[:, 4:5], in0=lsc[:, 3:4], in1=lsc[:, 2:3], op=ALU.subtract), reads=["lsc23"], writes=["lsc4"])
    S.op("dve", lambda e: e.tensor_scalar(out=lsc[:, 4:5], in0=lsc[:, 4:5], scalar1=-lam_init, scalar2=None, op0=ALU.add),
         reads=["lsc4"], writes=["nlam"])
    S.op("dve", lambda e: e.memset(lsc[:, 5:6], RMS_EPS), writes=["rmseps"])
    S.op("dve", lambda e: e.tensor_scalar(out=gv[:], in0=gv[:], scalar1=(1.0 - lam_init), scalar2=None, op0=ALU.mult),
         reads=["gv_raw"], writes=["gv"])
    for i in range(2):
        S.op("dve", lambda e, i=i: e.memset(Vh[i][:], 1.0), writes=[("Vh", i)])

    cnt = [0]
    for s in range(NSEQ):
        build_xT(C, x_in, s, xT, ident_f, xs)

        def inproj(h):
            par = h % NQB
            vpar = h % 2
            for (t_, nm) in ((q1a, "q1"), (q2a, "q2")):
                S.dma("sp", lambda e, t_=t_: e.dma_start(out=t_[par][64:68, :], in_=C.consts["augq"][h]), writes=[(nm + "_aug", par)])
            for (t_, nm) in ((k1a, "k1"), (k2a, "k2")):
                S.dma("sp", lambda e, t_=t_: e.dma_start(out=t_[par][64:68, :], in_=C.consts["augk"][h]), writes=[(nm + "_aug", par)])
            for (d1, d2, n1, n2, off) in ((q1a[par], q2a[par], "q1", "q2", 0), (k1a[par], k2a[par], "k1", "k2", D)):
                for r in range(4):
                    bi = cnt[0] % 2
                    cnt[0] += 1
                    bk, bkey = B[bi], ("bank", bi)
                    for k in range(8):
                        S.op("pe", lambda e, bk=bk, k=k, off=off, r=r: e.matmul(
                            bk[:, :], lhsT=Win[:, k, off + h * 128:off + (h + 1) * 128], rhs=xT[:, k, r * 512:(r + 1) * 512],
                            start=(k == 0), stop=(k == 7)), reads=win_keys + ["xT"], writes=[bkey])
                    S.op("dve", lambda e, bk=bk, d1=d1, r=r: e.tensor_copy(out=d1[0:64, r * 512:(r + 1) * 512], in_=bk[0:64, :]),
                         reads=[bkey], writes=[(n1, par)])
                    S.op("dve", lambda e, bk=bk, d2=d2, r=r: e.tensor_copy(out=d2[0:64, r * 512:(r + 1) * 512], in_=bk[64:128, :]),
                         reads=[bkey], writes=[(n2, par)])
            for g in range(4):
                bi = cnt[0] % 2
                cnt[0] += 1
                bk, bkey = B[bi], ("bank", bi)
                for tl in range(4):
                    t = g * 4 + tl
                    for k in range(8):
                        S.op("pe", lambda e, bk=bk, k=k, t=t, tl=tl: e.matmul(
                            bk[:, tl * 128:(tl + 1) * 128], lhsT=xT[:, k, t * 128:(t + 1) * 128],
                            rhs=Win[:, k, 2 * D + h * 128:2 * D + (h + 1) * 128], start=(k == 0), stop=(k == 7)),
                            reads=win_keys + ["xT"], writes=[bkey])
                S.op("act", lambda e, bk=bk, g=g: e.copy(out=Vh[vpar][:, g * 4:(g + 1) * 4, 0:128],
                                                         in_=bk[:, :].rearrange("p (t c) -> p t c", t=4)),
                     reads=[bkey], writes=[("Vh", vpar)])

        def attn(h):
            par = h % NQB
            vpar = h % 2
            rd1 = [("q1", par), ("k1", par), ("q1_aug", par), ("k1_aug", par)]
            rd2 = [("q2", par), ("k2", par), ("q2_aug", par), ("k2_aug", par)]
            QB = 256
            for qb in range(SEQ // QB):
                O1, O2 = B[2], B[3]
                k1_, k2_ = ("bank", 2), ("bank", 3)
                for j in range(2 * qb + 2):
                    d = j - 2 * qb
                    n0 = max(0, d) * 128
                    si = cnt[0] % 2
                    pi = cnt[0] % 3
                    cnt[0] += 1
                    Sb, skey = B[5 + si], ("bank", 5 + si)
                    P, pkey = pt[pi], ("pt", pi)
                    for (br, qq, kk, rd) in ((0, q1a[par], k1a[par], rd1), (1, q2a[par], k2a[par], rd2)):
                        S.op("pe", lambda e, Sb=Sb, j=j, qb=qb, n0=n0, br=br, qq=qq, kk=kk: e.matmul(
                            Sb[:, br * 256 + n0:(br + 1) * 256], lhsT=kk[0:68, j * 128:(j + 1) * 128],
                            rhs=qq[0:68, qb * QB + n0:(qb + 1) * QB], start=True, stop=True), reads=rd, writes=[skey])
                    if n0 == 0:
                        S.op("act", lambda e, Sb=Sb, P=P: e.activation(out=P[:, :], in_=Sb[:, :], func=AF.Exp, scale=0.125),
                             reads=[skey], writes=[pkey])
                    else:
                        for br in range(2):
                            S.op("act", lambda e, Sb=Sb, P=P, br=br, n0=n0: e.activation(
                                out=P[:, br * 256 + n0:(br + 1) * 256], in_=Sb[:, br * 256 + n0:(br + 1) * 256], func=AF.Exp, scale=0.125),
                                reads=[skey], writes=[pkey])
                    if d >= 0:
                        for br in range(2):
                            S.op("dve", lambda e, P=P, n0=n0, br=br: e.tensor_tensor(
                                out=P[:, br * 256 + n0:br * 256 + n0 + 128], in0=P[:, br * 256 + n0:br * 256 + n0 + 128],
                                in1=corr[:, h, :], op=ALU.mult), reads=[pkey, "corr"], writes=[pkey])
                    for i in range(max(0, d), 2):
                        for (br, O, ok) in ((0, O1, k1_), (1, O2, k2_)):
                            S.op("pe", lambda e, O=O, P=P, i=i, j=j, qb=qb, br=br: e.matmul(
                                O[:, i * 129:(i + 1) * 129], lhsT=P[:, br * 256 + i * 128:br * 256 + (i + 1) * 128],
                                rhs=Vh[vpar][:, j, :], start=(j == 0), stop=(j == 2 * qb + i)), reads=[pkey, ("Vh", vpar)], writes=[ok])
                S.op("dve", lambda e: e.reciprocal(out=rr[:, 0:2], in_=O1[:, 0:258].rearrange("p (i c) -> p i c", i=2)[:, :, 128]),
                     reads=[k1_], writes=["rr01"])
                S.op("dve", lambda e: e.reciprocal(out=rr[:, 2:4], in_=O2[:, 0:258].rearrange("p (i c) -> p i c", i=2)[:, :, 128]),
                     reads=[k2_], writes=["rr23"])
                S.op("dve", lambda e: e.tensor_scalar(out=rr[:, 2:4], in0=rr[:, 2:4], scalar1=lsc[:, 4:5], scalar2=None, op0=ALU.mult),
                     reads=["rr23", "nlam"], writes=["rr23"])
                for i in range(2):
                    obt, obk = ob[i], ("ob", i)
                    S.op("dve", lambda e, i=i: e.tensor_scalar(out=ot[:], in0=O1[:, i * 129:i * 129 + 128], scalar1=rr[:, i:i + 1],
                                                               scalar2=None, op0=ALU.mult), reads=[k1_, "rr01"], writes=["ot"])
                    S.op("dve", lambda e, i=i: e.scalar_tensor_tensor(out=ot[:], in0=O2[:, i * 129:i * 129 + 128], scalar=rr[:, 2 + i:3 + i],
                                                                      in1=ot[:], op0=ALU.mult, op1=ALU.add), reads=[k2_, "rr23", "ot"], writes=["ot"])
                    S.op("dve", lambda e: e.scalar_tensor_tensor(out=junk[:], in0=ot[:], scalar=1.0, in1=ot[:], op0=ALU.mult, op1=ALU.mult,
                                                                 accum_out=rr[:, 4:5]), reads=["ot", "junk"], writes=["junk", "ss"])
                    S.op("act", lambda e: e.activation(out=rr[:, 5:6], in_=rr[:, 4:5], func=AF.Ln, bias=lsc[:, 5:6], scale=1.0 / 128.0),
                         reads=["ss", "rmseps"], writes=["rs_"])
                    S.op("act", lambda e: e.activation(out=rr[:, 5:6], in_=rr[:, 5:6], func=AF.Exp, scale=-0.5), reads=["rs_"], writes=["rs_"])
                    S.op("dve", lambda e, obt=obt: e.scalar_tensor_tensor(out=obt[:], in0=ot[:], scalar=rr[:, 5:6], in1=gv[:], op0=ALU.mult,
                                                                          op1=ALU.mult), reads=["ot", "rs_", "gv"], writes=[obk])
                    S.op("pe", lambda e, i=i, obt=obt: e.transpose(out=C.pTr[:, i * 128:(i + 1) * 128], in_=obt[:], identity=ident_b[:]),
                         reads=[obk, "ident_b"], writes=["pTr"])
                S.op("act", lambda e, qb=qb: e.copy(out=catT[:, h, qb * QB:(qb + 1) * QB], in_=C.pTr[:, 0:256]), reads=["pTr"], writes=["catT"])

        inproj(0)
        for h in range(8):
            if NQB == 2 and h + 1 < 8:
                inproj(h + 1)
            attn(h)
            if NQB == 1 and h + 1 < 8:
                inproj(h + 1)

        outproj_ln(C, x_in, x_out, s, catT, Wout, wout_keys, gbc, bbc, xs, yts)
    end_phase(C)


def emit_moe(C, x_in, x_out, w_group, b_group, w_expert, b_expert, w_gate, w_up, w_down, ln_g, ln_b, xslots, yslots):
    new_phase(C)
    S, sb, nc = C.S, C.sb, C.nc
    B = C.banks
    NB = CAP // 128
    ident_f = sb("ident_f", [128, 128], F32)
    ident_b = sb("ident_b", [128, 128], BF16)
    tri = sb("tri", [128, 128], BF16)
    ones_b = sb("ones_b", [128, 128], BF16)
    basem1 = sb("basem1", [128, NEXP], F32)
    Wr = sb("Wr", [128, 8, 36], F32)
    brt = sb("brt", [128, 36], F32)
    gbc = sb("gbc", [128, D], F32)
    bbc = sb("bbc", [128, D], F32)
    xs = [sb("xs%d" % i, [128, D], F32) for i in range(2)]
    xb = [sb("xb%d" % i, [128, D], BF16) for i in range(2)]
    xTt = sb("xTt", [128, 8, 128], F32)
    lg = sb("lg", [128, 36], F32)
    me = sb("me", [128, 32], F32)
    oh1 = sb("oh1", [128, 32], F32)
    oh2 = sb("oh2", [128, 32], F32)
    ohb = sb("ohb", [128, 32], BF16)
    Q = sb("Q", [128, 32], F32)
    cntt = sb("cntt", [128, 32], F32)
    sc = sb("sc", [128, 16], F32)
    junk = sb("junk", [128, 32], F32)
    sf = sb("sf", [128, 2], F32)
    idx_all = sb("idx_all", [128, NT, 2], I32)
    w_all = sb("w_all", [128, NT, 2], F32)
    wg = [sb("wg%d" % i, [128, 8, 512], BF16) for i in range(2)]
    wu = [sb("wu%d" % i, [128, 8, 512], BF16) for i in range(2)]
    wd = [sb("wd%d" % i, [128, 4, D], BF16) for i in range(2)]
    xg = [sb("xg%d" % i, [128, D], BF16) for i in range(2)]
    XT = [sb("XT%d" % i, [128, 8, CAP], BF16) for i in range(2)]
    sg = [sb("sg%d" % i, [128, CAP], F32) for i in range(2)]
    hT = [sb("hT%d" % i, [128, 4, CAP], BF16) for i in range(2)]
    yo = [sb("yo%d" % i, [128, D], BF16) for i in range(2)]
    y1 = [sb("y1_%d" % i, [128, D], BF16) for i in range(2)]
    y2 = [sb("y2_%d" % i, [128, D], BF16) for i in range(2)]
    acc = [sb("acc%d" % i, [128, D], F32) for i in range(2)]
    alloc_ln(C)

    load_const(C, "ident_f", ident_f[:], "ident_f")
    load_const(C, "ident_b", ident_b[:], "ident_b")
    load_const(C, "tri", tri[:], "tri")
    load_const(C, "ones_b", ones_b[:], "ones_b")
    load_const(C, "basem1", basem1[:], "basem1")
    S.dma("sp", lambda e: e.dma_start(out=Wr[:, :, 0:4], in_=w_group.rearrange("(k p) n -> p k n", p=128)), writes=["Wr_g"])
    S.dma("sp", lambda e: e.dma_start(out=Wr[:, :, 4:36], in_=w_expert.rearrange("(k p) n -> p k n", p=128)), writes=["Wr_e"])
    S.dma("sp", lambda e: e.dma_start(out=brt[:, 0:4], in_=b_group.partition_broadcast(128)), writes=["br_g"])
    S.dma("sp", lambda e: e.dma_start(out=brt[:, 4:36], in_=b_expert.partition_broadcast(128)), writes=["br_e"])
    S.dma("sp", lambda e: e.dma_start(out=gbc[:], in_=ln_g.partition_broadcast(128)), writes=["gbc"])
    S.dma("sp", lambda e: e.dma_start(out=bbc[:], in_=ln_b.partition_broadcast(128)), writes=["bbc"])
    S.op("dve", lambda e: e.memset(cntt[:], 0.0), writes=["cntt"])

    def load_expert(e_):
        p = e_ % 2
        keys = []
        for (dst, src, nk, ncol, nm) in ((wg[p], w_gate[e_], 8, 512, "wg"), (wu[p], w_up[e_], 8, 512, "wu"), (wd[p], w_down[e_], 4, D, "wd")):
            v = src.rearrange("(k p) n -> p k n", p=128)
            half = nk // 2
            for a in range(2):
                kk = (nm, p, a)
                keys.append(kk)
                S.dma("pool", lambda e, dst=dst, v=v, a=a, half=half: e.dma_start(out=dst[:, a * half:(a + 1) * half, :],
                                                                                 in_=v[:, a * half:(a + 1) * half, :]), writes=[kk])
        return keys

    scat_keys = []
    for t in range(NT):
        p = t % 2
        xkey = ("xs", p)
        S.dma("sp", lambda e, t=t, p=p: e.dma_start(out=xs[p][:], in_=x_in[t * 128:(t + 1) * 128, :]), writes=[xkey])
        S.op("act", lambda e, p=p: e.copy(out=xb[p][:], in_=xs[p][:]), reads=[xkey], writes=[("xb", p)])
        for half in range(2):
            bk, bkey = B[half], ("bank", half)
            for j in range(4):
                k = half * 4 + j
                S.op("pe", lambda e, bk=bk, j=j, k=k, p=p: e.transpose(out=bk[:, j * 128:(j + 1) * 128], in_=xs[p][:, k * 128:(k + 1) * 128],
                                                                       identity=ident_f[:]), reads=[xkey, "ident_f"], writes=[bkey])
            S.op("dve" if half == 0 else "act",
                 (lambda e, bk=bk, half=half: e.tensor_copy(out=xTt[:, half * 4:half * 4 + 4, :], in_=bk[:, :].rearrange("p (j c) -> p j c", j=4)))
                 if half == 0 else
                 (lambda e, bk=bk, half=half: e.copy(out=xTt[:, half * 4:half * 4 + 4, :], in_=bk[:, :].rearrange("p (j c) -> p j c", j=4))),
                 reads=[bkey], writes=[("xTt", half)])
        L, lkey = B[2], ("bank", 2)
        for k in range(8):
            S.op("pe", lambda e, k=k: e.matmul(L[:, 0:36], lhsT=xTt[:, k, :], rhs=Wr[:, k, :], start=(k == 0), stop=(k == 7)),
                 reads=[("xTt", 0), ("xTt", 1), "Wr_g", "Wr_e"], writes=[lkey])
        S.op("dve", lambda e: e.tensor_tensor(out=lg[:], in0=L[:, 0:36], in1=brt[:], op=ALU.add), reads=[lkey, "br_g", "br_e"], writes=["lg"])
        S.op("dve", lambda e: e.reduce_max(out=sc[:, 0:1], in_=lg[:, 0:4], axis=AX.X), reads=["lg"], writes=["gmax"])
        S.op("dve", lambda e: e.tensor_scalar(out=sc[:, 1:2], in0=sc[:, 0:1], scalar1=-1.0, scalar2=None, op0=ALU.mult), reads=["gmax"], writes=["ngmax"])
        S.op("act", lambda e: e.activation(out=junk[:, 0:4], in_=lg[:, 0:4], func=AF.Exp, bias=sc[:, 1:2], scale=1.0, accum_out=sc[:, 2:3]),
             reads=["lg", "ngmax", "junk"], writes=["junk", "gsum"])
        S.op("dve", lambda e: e.reciprocal(out=sc[:, 3:4], in_=sc[:, 2:3]), reads=["gsum"], writes=["gw"])
        S.op("dve", lambda e: e.tensor_scalar(out=sc[:, 4:8], in0=lg[:, 0:4], scalar1=sc[:, 0:1], scalar2=None, op0=ALU.is_equal),
             reads=["lg", "gmax"], writes=["goh"])
        S.op("dve", lambda e: e.tensor_scalar(out=sc[:, 4:8], in0=sc[:, 4:8], scalar1=1e30, scalar2=-1e30, op0=ALU.mult, op1=ALU.add),
             reads=["goh"], writes=["pen"])
        for g in range(4):
            S.op("dve", lambda e, g=g: e.tensor_scalar(out=me[:, g * 8:(g + 1) * 8], in0=lg[:, 4 + g * 8:12 + g * 8], scalar1=sc[:, 4 + g:5 + g],
                                                       scalar2=None, op0=ALU.add), reads=["lg", "pen"], writes=["me"])
        S.op("dve", lambda e: e.reduce_max(out=sc[:, 8:9], in_=me[:], axis=AX.X), reads=["me"], writes=["m1"])
        S.op("dve", lambda e: e.tensor_scalar(out=oh1[:], in0=me[:], scalar1=sc[:, 8:9], scalar2=None, op0=ALU.is_equal), reads=["me", "m1"], writes=["oh1"])
        S.op("dve", lambda e: e.scalar_tensor_tensor(out=me[:], in0=oh1[:], scalar=-1e30, in1=me[:], op0=ALU.mult, op1=ALU.add),
             reads=["oh1", "me"], writes=["me"])
        S.op("dve", lambda e: e.reduce_max(out=sc[:, 9:10], in_=me[:], axis=AX.X), reads=["me"], writes=["m2"])
        S.op("dve", lambda e: e.tensor_scalar(out=oh2[:], in0=me[:], scalar1=sc[:, 9:10], scalar2=None, op0=ALU.is_equal), reads=["me", "m2"], writes=["oh2"])
        S.op("dve", lambda e: e.tensor_tensor(out=sc[:, 10:11], in0=sc[:, 9:10], in1=sc[:, 8:9], op=ALU.subtract), reads=["m1", "m2"], writes=["dm"])
        S.op("act", lambda e: e.activation(out=sc[:, 10:11], in_=sc[:, 10:11], func=AF.Exp), reads=["dm"], writes=["dm"])
        S.op("dve", lambda e: e.tensor_scalar(out=sc[:, 10:11], in0=sc[:, 10:11], scalar1=1.0, scalar2=None, op0=ALU.add), reads=["dm"], writes=["dm"])
        S.op("dve", lambda e: e.reciprocal(out=sc[:, 11:12], in_=sc[:, 10:11]), reads=["dm"], writes=["sg1"])
        S.op("dve", lambda e, t=t: e.tensor_tensor(out=w_all[:, t, 0:1], in0=sc[:, 11:12], in1=sc[:, 3:4], op=ALU.mult), reads=["sg1", "gw"], writes=[("w_all", t)])
        S.op("dve", lambda e, t=t: e.tensor_tensor(out=w_all[:, t, 1:2], in0=sc[:, 3:4], in1=w_all[:, t, 0:1], op=ALU.subtract),
             reads=["gw", ("w_all", t)], writes=[("w_all", t)])
        S.op("dve", lambda e: e.tensor_tensor(out=ohb[:], in0=oh1[:], in1=oh2[:], op=ALU.add), reads=["oh1", "oh2"], writes=["ohb"])
        R, rkey = B[3], ("bank", 3)
        S.op("pe", lambda e: e.matmul(R[:, 0:32], lhsT=tri[:], rhs=ohb[:], start=True, stop=True), reads=["tri", "ohb"], writes=[rkey])
        S.op("pe", lambda e: e.matmul(R[:, 32:64], lhsT=ones_b[:], rhs=ohb[:], start=True, stop=True), reads=["ones_b", "ohb"], writes=[rkey])
        S.op("dve", lambda e: e.tensor_tensor(out=Q[:], in0=R[:, 0:32], in1=cntt[:], op=ALU.add), reads=[rkey, "cntt"], writes=["Q"])
        S.op("dve", lambda e: e.tensor_tensor(out=cntt[:], in0=R[:, 32:64], in1=cntt[:], op=ALU.add), reads=[rkey, "cntt", "Q"], writes=["cntt"])
        S.op("dve", lambda e: e.tensor_scalar(out=Q[:], in0=Q[:], scalar1=float(CAP + 1), scalar2=None, op0=ALU.min), reads=["Q"], writes=["Q"])
        S.op("dve", lambda e: e.tensor_tensor(out=Q[:], in0=Q[:], in1=basem1[:], op=ALU.add), reads=["Q", "basem1"], writes=["Q"])
        S.op("dve", lambda e: e.scalar_tensor_tensor(out=junk[:], in0=oh1[:], scalar=1.0, in1=Q[:], op0=ALU.mult, op1=ALU.mult, accum_out=sf[:, 0:1]),
             reads=["oh1", "Q", "junk"], writes=["junk", "sf0"])
        S.op("dve", lambda e: e.scalar_tensor_tensor(out=junk[:], in0=oh2[:], scalar=1.0, in1=Q[:], op0=ALU.mult, op1=ALU.mult, accum_out=sf[:, 1:2]),
             reads=["oh2", "Q", "junk"], writes=["junk", "sf1"])
        S.op("dve", lambda e, t=t: e.tensor_copy(out=idx_all[:, t, :], in_=sf[:]), reads=["sf0", "sf1"], writes=[("idx", t)])
        for kk in range(2):
            sk = ("scat", t, kk)
            scat_keys.append(sk)
            S.dma("pool", lambda e, t=t, kk=kk, p=p: e.indirect_dma_start(
                out=xslots, out_offset=bass.IndirectOffsetOnAxis(ap=idx_all[:, t, kk:kk + 1], axis=0), in_=xb[p][:], in_offset=None),
                reads=[("xb", p), ("idx", t)], writes=[sk])

    wkeys = {0: load_expert(0)}
    ycnt = 0
    ykeys = []
    for e_ in range(NEXP):
        p = e_ % 2
        if e_ + 1 < NEXP:
            wkeys[e_ + 1] = load_expert(e_ + 1)
        wk = wkeys[e_]
        for blk in range(NB):
            gp = (e_ * NB + blk) % 2
            row0 = e_ * STRIDE + blk * 128
            S.dma("sp", lambda e, row0=row0, gp=gp: e.dma_start(out=xg[gp][:], in_=xslots[row0:row0 + 128, :]),
                  reads=scat_keys, writes=[("xg", gp)])
            for k in range(8):
                S.op("pe", lambda e, k=k, gp=gp: e.transpose(out=C.pTr[:, k * 128:(k + 1) * 128], in_=xg[gp][:, k * 128:(k + 1) * 128],
                                                             identity=ident_b[:]), reads=[("xg", gp), "ident_b"], writes=["pTr"])
            S.op("dve", lambda e, p=p, blk=blk: e.tensor_copy(out=XT[p][:, :, blk * 128:(blk + 1) * 128],
                                                              in_=C.pTr[:, :].rearrange("p (k c) -> p k c", k=8)), reads=["pTr"], writes=[("XT", p)])
        for hc in range(4):
            G, gkey = B[0 + hc % 2], ("bank", hc % 2)
            U, ukey = B[2 + hc % 2], ("bank", 2 + hc % 2)
            for (bk, bkey, w_) in ((G, gkey, wg[p]), (U, ukey, wu[p])):
                for k in range(8):
                    S.op("pe", lambda e, bk=bk, k=k, w_=w_, hc=hc, p=p: e.matmul(bk[:, 0:CAP], lhsT=w_[:, k, hc * 128:(hc + 1) * 128], rhs=XT[p][:, k, :],
                                                                                 start=(k == 0), stop=(k == 7)), reads=wk + [("XT", p)], writes=[bkey])
            S.op("act", lambda e, G=G, hc=hc: e.activation(out=sg[hc % 2][:], in_=G[:, 0:CAP], func=AF.Silu), reads=[gkey], writes=[("sg", hc % 2)])
            S.op("dve", lambda e, U=U, hc=hc, p=p: e.tensor_tensor(out=hT[p][:, hc, :], in0=U[:, 0:CAP], in1=sg[hc % 2][:], op=ALU.mult),
                 reads=[ukey, ("sg", hc % 2)], writes=[("hT", p)])
        for blk in range(NB):
            yp = ycnt % 2
            ycnt += 1
            for half in range(2):
                Y, ykey = B[4 + half], ("bank", 4 + half)
                for hc in range(4):
                    S.op("pe", lambda e, Y=Y, hc=hc, blk=blk, half=half, p=p: e.matmul(
                        Y[:, :], lhsT=hT[p][:, hc, blk * 128:(blk + 1) * 128], rhs=wd[p][:, hc, half * 512:(half + 1) * 512],
                        start=(hc == 0), stop=(hc == 3)), reads=wk + [("hT", p)], writes=[ykey])
                if half == 0:
                    S.op("act", lambda e, Y=Y, yp=yp: e.copy(out=yo[yp][:, 0:512], in_=Y[:, :]), reads=[ykey], writes=[("yo", yp, 0)])
                else:
                    S.op("dve", lambda e, Y=Y, yp=yp: e.tensor_copy(out=yo[yp][:, 512:1024], in_=Y[:, :]), reads=[ykey], writes=[("yo", yp, 1)])
            row0 = e_ * STRIDE + blk * 128
            yk = ("ysl", e_, blk)
            ykeys.append(yk)
            S.dma("sp", lambda e, row0=row0, yp=yp: e.dma_start(out=yslots[row0:row0 + 128, :], in_=yo[yp][:]),
                  reads=[("yo", yp, 0), ("yo", yp, 1)], writes=[yk])

    for t in range(NT):
        p = t % 2
        xkey = ("xs", p)
        S.dma("sp", lambda e, t=t, p=p: e.dma_start(out=xs[p][:], in_=x_in[t * 128:(t + 1) * 128, :]), writes=[xkey])
        for (yy, nm, kk) in ((y1[p], "y1", 0), (y2[p], "y2", 1)):
            S.dma("pool", lambda e, t=t, kk=kk, yy=yy: e.indirect_dma_start(
                out=yy[:], out_offset=None, in_=yslots, in_offset=bass.IndirectOffsetOnAxis(ap=idx_all[:, t, kk:kk + 1], axis=0)),
                reads=ykeys + [("idx", t)], writes=[(nm, p)])
        a, akey = acc[p], ("acc", p)
        S.op("act", lambda e, t=t, a=a, p=p: e.activation(out=a[:], in_=y2[p][:], func=AF.Identity, scale=w_all[:, t, 1:2]),
             reads=[("y2", p), ("w_all", t)], writes=[akey])
        S.op("dve", lambda e, t=t, a=a, p=p: e.scalar_tensor_tensor(out=a[:], in0=y1[p][:], scalar=w_all[:, t, 0:1], in1=a[:], op0=ALU.mult, op1=ALU.add),
             reads=[("y1", p), ("w_all", t), akey], writes=[akey])
        S.op("dve", lambda e, a=a, p=p: e.scalar_tensor_tensor(out=a[:], in0=xs[p][:], scalar=ALPHA, in1=a[:], op0=ALU.mult, op1=ALU.add),
             reads=[xkey, akey], writes=[akey])
        layer_norm_store(C, a, gbc, bbc, x_out[t * 128:(t + 1) * 128, :], akey, t)
    end_phase(C)


def build_program(phases):
    nc = bass.Bass("TRN2", target_bir_lowering=False)
    C = Ctx()
    C.nc = nc
    outer = ExitStack()
    C.S = Sched(nc, outer)
    din = lambda name, shape, dt=F32: nc.dram_tensor(name, shape, dt, kind="ExternalInput").ap()
    C.consts = {k: din("c_" + k, shp, dt) for k, (shp, dt) in CONST_SPECS.items()}
    x_cur = din("x", [NTOK, D])
    need_moe = any(p[0] == "moe" for p in phases)
    if need_moe:
        xslots = nc.dram_tensor("xslots", [NSLOT, D], BF16, kind="Internal").ap()
        yslots = nc.dram_tensor("yslots", [NSLOT, D], BF16, kind="Internal").ap()
    for pi, (kind, layer) in enumerate(phases):
        last = pi == len(phases) - 1
        if last:
            x_next = nc.dram_tensor("y", [NTOK, D], F32, kind="ExternalOutput").ap()
        else:
            x_next = nc.dram_tensor("xact%d" % pi, [NTOK, D], F32, kind="Internal").ap()
        pre = "p%d_" % pi
        if kind == "even":
            emit_even(C, x_cur, x_next, din(pre + "w_in", [D, EVEN_IN]), din(pre + "b_forget", [8]), din(pre + "conv_w", [512, 3]),
                      din(pre + "w_out", [D, D]), din(pre + "ln_g", [D]), din(pre + "ln_b", [D]))
        elif kind == "odd":
            emit_odd(C, layer, x_cur, x_next, din(pre + "w_in", [D, 3 * D]), din(pre + "lam_q1", [64]), din(pre + "lam_k1", [64]),
                     din(pre + "lam_q2", [64]), din(pre + "lam_k2", [64]), din(pre + "subln_g", [128]), din(pre + "w_out", [D, D]),
                     din(pre + "ln_g", [D]), din(pre + "ln_b", [D]))
        else:
            emit_moe(C, x_cur, x_next, din(pre + "w_group", [D, 4]), din(pre + "b_group", [4]), din(pre + "w_expert", [D, 32]),
                     din(pre + "b_expert", [32]), din(pre + "w_gate", [NEXP, D, 512]), din(pre + "w_up", [NEXP, D, 512]),
                     din(pre + "w_down", [NEXP, 512, D]), din(pre + "ln_g", [D]), din(pre + "ln_b", [D]), xslots, yslots)
        x_cur = x_next
    outer.close()
    return nc


def phase_inputs(pi, kind, layer, inp):
    pre = "p%d_" % pi
    i = layer // 2
    c = np.ascontiguousarray
    if kind == "even":
        return {pre + "w_in": c(inp["ab_w_in"][i]), pre + "b_forget": c(inp["ab_b_forget"][i]), pre + "conv_w": c(inp["ab_conv_w"][i]),
                pre + "w_out": c(inp["ab_w_out"][i]), pre + "ln_g": c(inp["ln_mix_g"][layer]), pre + "ln_b": c(inp["ln_mix_b"][layer])}
    if kind == "odd":
        return {pre + "w_in": c(inp["c_w_in"][i]), pre + "lam_q1": c(inp["c_lam_q1"][i]), pre + "lam_k1": c(inp["c_lam_k1"][i]),
                pre + "lam_q2": c(inp["c_lam_q2"][i]), pre + "lam_k2": c(inp["c_lam_k2"][i]), pre + "subln_g": c(inp["c_subln_g"][i]),
                pre + "w_out": c(inp["c_w_out"][i]), pre + "ln_g": c(inp["ln_mix_g"][layer]), pre + "ln_b": c(inp["ln_mix_b"][layer])}
    return {pre + "w_group": c(inp["moe_w_group"][layer]), pre + "b_group": c(inp["moe_b_group"][layer]),
            pre + "w_expert": c(inp["moe_w_expert"][layer]), pre + "b_expert": c(inp["moe_b_expert"][layer]),
            pre + "w_gate": c(inp["moe_w_gate"][layer]), pre + "w_up": c(inp["moe_w_up"][layer]), pre + "w_down": c(inp["moe_w_down"][layer]),
            pre + "ln_g": c(inp["ln_ffn_g"][layer]), pre + "ln_b": c(inp["ln_ffn_b"][layer])}


ALL_PHASES = []
for _l in range(DEPTH):
    ALL_PHASES.append(("even" if _l % 2 == 0 else "odd", _l))
    ALL_PHASES.append(("moe", _l))

_PROG_CACHE = {}


def run_phases(phases, x_shards, inp, core_ids=None, trace=False):
    key = tuple((k, (l if k == "odd" else 0)) for k, l in phases)
    if key not in _PROG_CACHE:
        _PROG_CACHE[key] = build_program(phases)
    nc = _PROG_CACHE[key]
    consts = {"c_" + k: v for k, v in host_consts().items()}
    shared = dict(consts)
    for pi, (kind, layer) in enumerate(phases):
        shared.update(phase_inputs(pi, kind, layer, inp))
    in_maps = []
    for xs in x_shards:
        m = dict(shared)
        m["x"] = np.ascontiguousarray(xs)
        in_maps.append(m)
    core_ids = core_ids if core_ids is not None else list(range(len(x_shards)))
    if trace:
        res = run_bass_kernel_spmd(nc, in_maps, core_ids=core_ids, trace=True)
        print("TRACE exec_time_ns", res.exec_time_ns)
    else:
        res = run_bass_kernel_spmd(nc, in_maps, core_ids=core_ids)
    return [r["y"] for r in res.results]


def kernel(**inputs):
    inp = {k: np.asarray(v) for k, v in inputs.items()}
    x = inp["x"].astype(np.float32, copy=False)
    shards = [x[NSEQ * c:NSEQ * (c + 1)].reshape(NTOK, D) for c in range(N_CORES)]
    if MODE == "fused":
        outs = run_phases(ALL_PHASES, shards, inp)
    else:
        outs = shards
        for ph in ALL_PHASES:
            outs = run_phases([ph], outs, inp)
    out = np.stack([o.reshape(NSEQ, SEQ, D) for o in outs], axis=0).reshape(N_CORES * NSEQ, SEQ, D)
    return out.astype(np.float32, copy=False)
```

```python
import math
from contextlib import ExitStack

import ml_dtypes
import numpy as np

import concourse.bass as bass
import concourse.mybir as mybir
from concourse.bass_utils import run_bass_kernel_spmd

F32 = mybir.dt.float32
BF16 = mybir.dt.bfloat16
I32 = mybir.dt.int32
AF = mybir.ActivationFunctionType
ALU = mybir.AluOpType
AX = mybir.AxisListType

N_CORES = 8
D = 1024
SEQ = 2048
NSEQ = 2
NTOK = NSEQ * SEQ
NT = NTOK // 128
TPS = SEQ // 128
DEPTH = 4
ALPHA = (2.0 * DEPTH) ** 0.25
LN_EPS = 1e-5
RMS_EPS = 1e-5
EVEN_IN = 3080
NEXP = 32
CAP = 512
STRIDE = CAP + 1
NSLOT = NEXP * STRIDE
MODE = "fused"


class Sched:
    COMPUTE = ("pe", "act", "dve", "pool")
    QUEUES = ("sp", "act", "pool")

    def __init__(self, nc, stack, same_engine_sync=True):
        self.nc = nc
        self.same_engine_sync = same_engine_sync
        self.engnames = ("pe", "act", "dve", "pool", "sp")
        self.streams = {e: [] for e in self.engnames}
        self.count = {e: 0 for e in self.COMPUTE}
        self.sem = {e: stack.enter_context(nc.semaphore("sem_" + e)) for e in self.COMPUTE}
        self.spare_sems = [{e: stack.enter_context(nc.semaphore("sem%d_%s" % (i, e))) for e in self.COMPUTE} for i in range(1)]
        nslots = {"sp": 16, "act": 1, "pool": 16}
        self.slots = {}
        for q in self.QUEUES:
            self.slots[q] = [
                {"sem": stack.enter_context(nc.semaphore("dsem_%s_%d" % (q, i))), "total": 0, "id": (q, i)}
                for i in range(nslots[q])
            ]
        self.slot_rr = {q: 0 for q in self.QUEUES}
        self.slot_by_id = {s["id"]: s for q in self.QUEUES for s in self.slots[q]}
        self.known = {e: {} for e in self.engnames}
        self.last_write = {}
        self.readers = {}

    def _deps(self, reads, writes):
        deps = []
        for k in reads:
            ev = self.last_write.get(k)
            if ev is not None:
                deps.append(ev)
        for k in writes:
            ev = self.last_write.get(k)
            if ev is not None:
                deps.append(ev)
            deps.extend(self.readers.get(k, ()))
        return deps

    def _commit(self, ev, reads, writes):
        for k in reads:
            self.readers.setdefault(k, []).append(ev)
        for k in writes:
            self.last_write[k] = ev
            self.readers[k] = []

    def _wait_list(self, eng, deps):
        need = {}
        for kind, key, val in deps:
            if kind == "e" and key == eng and (eng == "pe" or not self.same_engine_sync):
                continue
            sk = (kind, key)
            if self.known[eng].get(sk, 0) >= val:
                continue
            if need.get(sk, 0) < val:
                need[sk] = val
        out = []
        for sk, val in need.items():
            self.known[eng][sk] = val
            sem = self.sem[sk[1]] if sk[0] == "e" else self.slot_by_id[sk[1]]["sem"]
            out.append((sem, val))
        return out

    def op(self, eng, fn, reads=(), writes=()):
        waits = self._wait_list(eng, self._deps(reads, writes))
        self.count[eng] += 1
        ev = ("e", eng, self.count[eng])
        sem = self.sem[eng]

        def emit(e, fn=fn, waits=waits, sem=sem):
            for s, v in waits:
                e.wait_ge(s, v)
            fn(e).then_inc(sem, 1)

        self.streams[eng].append(emit)
        self._commit(ev, reads, writes)
        return ev

    def dma(self, queue, fn, reads=(), writes=()):
        slots = self.slots[queue]
        slot = slots[self.slot_rr[queue] % len(slots)]
        self.slot_rr[queue] += 1
        deps = self._deps(reads, writes)
        if slot["total"] > 0:
            deps.append(("d", slot["id"], slot["total"]))
        waits = self._wait_list(queue, deps)
        slot["total"] += 16
        ev = ("d", slot["id"], slot["total"])
        sem = slot["sem"]

        def emit(e, fn=fn, waits=waits, sem=sem):
            for s, v in waits:
                e.wait_ge(s, v)
            fn(e).then_inc(sem, 16)

        self.streams[queue].append(emit)
        self._commit(ev, reads, writes)
        return ev

    def drain(self):
        waits = []
        for q in self.QUEUES:
            for s in self.slots[q]:
                if s["total"] > 0:
                    waits.append((s["sem"], s["total"]))
                    for e in self.engnames:
                        self.known[e][("d", s["id"])] = s["total"]
        for c in self.COMPUTE:
            if self.count[c] > 0:
                waits.append((self.sem[c], self.count[c]))
                for e in self.engnames:
                    self.known[e][("e", c)] = self.count[c]

        def emit(e, waits=waits):
            for s, v in waits:
                e.wait_ge(s, v)

        self.streams["sp"].append(emit)
        self.last_write = {}
        self.readers = {}

    def rotate_engine_sems(self):
        if not self.spare_sems:
            return
        self.sem = self.spare_sems.pop(0)
        for c in self.COMPUTE:
            self.count[c] = 0
            for e in self.engnames:
                self.known[e].pop(("e", c), None)

    def reset_engine_sems(self):
        nc = self.nc
        sem = self.sem
        with nc.Block() as block:
            @block.tensor
            def _(e):
                e.sem_clear(sem["pe"])

            @block.scalar
            def _(e):
                e.sem_clear(sem["act"])

            @block.vector
            def _(e):
                e.sem_clear(sem["dve"])

            @block.gpsimd
            def _(e):
                e.sem_clear(sem["pool"])
        for c in self.COMPUTE:
            self.count[c] = 0
            for e in self.engnames:
                self.known[e].pop(("e", c), None)

    def emit_block(self):
        nc = self.nc
        streams = self.streams
        with nc.Block() as block:
            @block.tensor
            def _(e):
                for f in streams["pe"]:
                    f(e)

            @block.scalar
            def _(e):
                for f in streams["act"]:
                    f(e)

            @block.vector
            def _(e):
                for f in streams["dve"]:
                    f(e)

            @block.gpsimd
            def _(e):
                for f in streams["pool"]:
                    f(e)

            @block.sync
            def _(e):
                for f in streams["sp"]:
                    f(e)
        self.streams = {e: [] for e in self.engnames}


def host_consts():
    bf = ml_dtypes.bfloat16
    c = {}
    c["ident_f"] = np.eye(128, dtype=np.float32)
    c["ident_b"] = np.eye(128, dtype=np.float32).astype(bf)
    k = np.arange(128)[:, None]
    q = np.arange(128)[None, :]
    c["tri"] = (k <= q).astype(np.float32).astype(bf)
    c["ones_b"] = np.ones((128, 128), np.float32).astype(bf)
    m12 = np.zeros((32, 2), np.float32)
    m12[0:8, 0] = -1.0
    m12[8:16, 1] = -1.0
    m12[16:24, 0] = 1.0
    m12[24:32, 1] = 1.0
    c["m12"] = m12
    slopes = 2.0 ** (-8.0 * np.arange(1, 9) / 8.0)
    pos = np.arange(SEQ)
    a = (pos // 64).astype(np.float64)
    b = (pos % 64).astype(np.float64)
    augq = np.zeros((8, 4, SEQ), np.float64)
    augk = np.zeros((8, 4, SEQ), np.float64)
    corr = np.zeros((128, 8, 128), np.float64)
    for h in range(8):
        s = slopes[h]
        augq[h, 0] = -8.0 * s * 64.0 * a
        augq[h, 1] = -8.0 * s * b
        augq[h, 2] = 1.0
        augq[h, 3] = 1.0
        augk[h, 0] = 1.0
        augk[h, 1] = 1.0
        augk[h, 2] = 8.0 * s * 64.0 * a
        augk[h, 3] = 8.0 * s * b
        vis = (k // 64) <= (q // 64)
        cc = np.where(k > q, np.exp(-2.0 * s * (k - q)), 1.0)
        corr[:, h, :] = np.where(vis, cc, 0.0)
    c["augq"] = augq.astype(np.float32).astype(bf)
    c["augk"] = augk.astype(np.float32).astype(bf)
    c["corr"] = corr.astype(np.float32).astype(bf)
    c["basem1"] = np.broadcast_to((np.arange(NEXP) * STRIDE - 1).astype(np.float32)[None, :], (128, NEXP)).copy()
    return c


CONST_SPECS = {
    "ident_f": ([128, 128], F32), "ident_b": ([128, 128], BF16), "tri": ([128, 128], BF16),
    "ones_b": ([128, 128], BF16), "m12": ([32, 2], F32), "augq": ([8, 4, SEQ], BF16),
    "augk": ([8, 4, SEQ], BF16), "corr": ([128, 8, 128], BF16), "basem1": ([128, NEXP], F32),
}


class Ctx:
    pass


def new_phase(C, n_f32=7, n_tr=1):
    C.st = ExitStack()
    nc = C.nc
    C.phase_no = getattr(C, "phase_no", -1) + 1
    pfx = "f%d_" % C.phase_no
    C.sb = lambda name, shape, dt: C.st.enter_context(nc.sbuf_tensor(pfx + name, shape, dt))
    C.banks = [C.st.enter_context(nc.psum_tensor(pfx + "bank%d" % i, [128, 512], F32)) for i in range(n_f32)]
    C.pTrs = [C.st.enter_context(nc.psum_tensor(pfx + "pTr%d" % i, [128, 1024], BF16)) for i in range(n_tr)]
    C.pTr = C.pTrs[0]


def end_phase(C):
    C.S.drain()
    C.S.emit_block()
    if C.phase_no == 3:
        C.S.rotate_engine_sems()
    C.st.close()


def load_const(C, name, tile_ap, key):
    C.S.dma("sp", lambda e: e.dma_start(out=tile_ap, in_=C.consts[name]), writes=[key])


def cast_load_w(C, dst, src, nk, ncols, key):
    S = C.S
    v = src.rearrange("(k p) n -> p k n", p=128)
    step = 1024 if ncols % 1024 == 0 else (1540 if ncols == 3080 else ncols)
    keys = []
    for k in range(nk):
        for c0 in range(0, ncols, step):
            c1 = min(ncols, c0 + step)
            kk = (key, k, c0)
            keys.append(kk)
            S.dma("pool", lambda e, k=k, c0=c0, c1=c1: e.dma_start(out=dst[:, k, c0:c1], in_=v[:, k, c0:c1]),
                  writes=[kk])
    return keys


def ln_stats(C, yt, ykey, li):
    S = C.S
    L = C.ln
    st, mv, rstd, nmr = L["stats"][:, li, :], L["mv"][:, li, :], L["rstd"][:, li:li + 1], L["nmr"][:, li:li + 1]
    k = lambda n: (n, li)
    S.op("dve", lambda e: e.bn_stats(out=st[:, 0:6], in_=yt[:, 0:512]), reads=[ykey], writes=[k("ln_stats")])
    S.op("dve", lambda e: e.bn_stats(out=st[:, 6:12], in_=yt[:, 512:1024]), reads=[ykey], writes=[k("ln_stats")])
    S.op("dve", lambda e: e.bn_aggr(out=mv, in_=st), reads=[k("ln_stats")], writes=[k("ln_mv")])
    S.op("act", lambda e: e.activation(out=rstd, in_=mv[:, 1:2], func=AF.Ln, bias=L["eps"][:], scale=1.0),
         reads=[k("ln_mv"), "ln_eps"], writes=[k("ln_rstd")])
    S.op("act", lambda e: e.activation(out=rstd, in_=rstd, func=AF.Exp, scale=-0.5), reads=[k("ln_rstd")], writes=[k("ln_rstd")])
    S.op("dve", lambda e: e.tensor_scalar(out=nmr, in0=mv[:, 0:1], scalar1=-1.0, scalar2=rstd, op0=ALU.mult, op1=ALU.mult),
         reads=[k("ln_mv"), k("ln_rstd")], writes=[k("ln_nmr")])


def ln_finish(C, yt, gbc, bbc, out_ap, ykey, tag, li):
    S = C.S
    L = C.ln
    rstd, nmr = L["rstd"][:, li:li + 1], L["nmr"][:, li:li + 1]
    k = lambda n: (n, li)
    S.op("act", lambda e: e.activation(out=yt[:], in_=yt[:], func=AF.Identity, bias=nmr, scale=rstd),
         reads=[ykey, k("ln_nmr"), k("ln_rstd")], writes=[ykey])
    S.op("dve", lambda e: e.tensor_tensor(out=yt[:], in0=yt[:], in1=gbc[:], op=ALU.mult), reads=[ykey, "gbc"], writes=[ykey])
    S.op("dve", lambda e: e.tensor_tensor(out=yt[:], in0=yt[:], in1=bbc[:], op=ALU.add), reads=[ykey, "bbc"], writes=[ykey])
    S.dma("sp", lambda e: e.dma_start(out=out_ap, in_=yt[:]), reads=[ykey], writes=[("xout", tag)])


def layer_norm_store(C, yt, gbc, bbc, out_ap, ykey, tag, li=0):
    ln_stats(C, yt, ykey, li)
    ln_finish(C, yt, gbc, bbc, out_ap, ykey, tag, li)


def alloc_ln(C):
    sb = C.sb
    C.ln = {"stats": sb("ln_stats", [128, 2, 12], F32), "mv": sb("ln_mv", [128, 2, 2], F32),
            "rstd": sb("ln_rstd", [128, 2], F32), "nmr": sb("ln_nmr", [128, 2], F32),
            "eps": sb("ln_eps", [128, 1], F32)}
    C.S.op("dve", lambda e: e.memset(C.ln["eps"][:], LN_EPS), writes=["ln_eps"])


def build_xT(C, x_in, s, xT, ident_f, xs):
    S = C.S
    for t in range(TPS):
        tt = s * TPS + t
        xb = xs[t % 2]
        xkey = ("xs", t % 2)
        S.dma("sp", lambda e, tt=tt, xb=xb: e.dma_start(out=xb[:], in_=x_in[tt * 128:(tt + 1) * 128, :]), writes=[xkey])
        for half in range(2):
            bk = C.banks[5 + half]
            bkey = ("bank", 5 + half)
            for j in range(4):
                k = half * 4 + j
                S.op("pe", lambda e, bk=bk, j=j, k=k, xb=xb: e.transpose(out=bk[:, j * 128:(j + 1) * 128],
                                                                         in_=xb[:, k * 128:(k + 1) * 128], identity=ident_f[:]),
                     reads=[xkey, "ident_f"], writes=[bkey])
            eng = "dve" if half == 0 else "act"
            if eng == "dve":
                S.op("dve", lambda e, bk=bk, half=half, t=t: e.tensor_copy(
                    out=xT[:, half * 4:half * 4 + 4, t * 128:(t + 1) * 128],
                    in_=bk[:, :].rearrange("p (j c) -> p j c", j=4)), reads=[bkey], writes=["xT"])
            else:
                S.op("act", lambda e, bk=bk, half=half, t=t: e.copy(
                    out=xT[:, half * 4:half * 4 + 4, t * 128:(t + 1) * 128],
                    in_=bk[:, :].rearrange("p (j c) -> p j c", j=4)), reads=[bkey], writes=["xT"])


def outproj_ln(C, x_in, x_out, s, catT, Wout, wout_keys, gbc, bbc, xs, yts):
    S = C.S
    pend = None
    for t in range(TPS):
        tt = s * TPS + t
        xb = xs[t % 2]
        xkey = ("xs", t % 2)
        yt = yts[t % len(yts)]
        ykey = ("yt", t % len(yts))
        S.dma("sp", lambda e, tt=tt, xb=xb: e.dma_start(out=xb[:], in_=x_in[tt * 128:(tt + 1) * 128, :]), writes=[xkey])
        for half in range(2):
            bk = C.banks[5 + half]
            bkey = ("bank", 5 + half)
            for k in range(8):
                S.op("pe", lambda e, bk=bk, k=k, t=t, half=half: e.matmul(
                    bk[:, :], lhsT=catT[:, k, t * 128:(t + 1) * 128], rhs=Wout[:, k, half * 512:(half + 1) * 512],
                    start=(k == 0), stop=(k == 7)), reads=["catT"] + wout_keys, writes=[bkey])
            S.op("dve", lambda e, bk=bk, half=half, xb=xb, yt=yt: e.scalar_tensor_tensor(
                out=yt[:, half * 512:(half + 1) * 512], in0=xb[:, half * 512:(half + 1) * 512], scalar=ALPHA,
                in1=bk[:, :], op0=ALU.mult, op1=ALU.add), reads=[xkey, bkey], writes=[ykey])
        if len(yts) < 2:
            layer_norm_store(C, yt, gbc, bbc, x_out[tt * 128:(tt + 1) * 128, :], ykey, tt)
        else:
            ln_stats(C, yt, ykey, t % 2)
            if pend is not None:
                ln_finish(C, *pend)
            pend = (yt, gbc, bbc, x_out[tt * 128:(tt + 1) * 128, :], ykey, tt, t % 2)
    if pend is not None:
        ln_finish(C, *pend)


def emit_even(C, x_in, x_out, w_in, b_forget, conv_w, w_out, ln_g, ln_b):
    new_phase(C)
    S, sb, nc = C.S, C.sb, C.nc
    B = C.banks
    Win = sb("Win", [128, 8, EVEN_IN], BF16)
    Wout = sb("Wout", [128, 8, D], BF16)
    Wf = sb("Wf", [128, 8, 32], BF16)
    cw = sb("cw", [128, 4, 3], F32)
    bf32 = sb("bf32", [32, 1], F32)
    gbc = sb("gbc", [128, D], F32)
    bbc = sb("bbc", [128, D], F32)
    ident_f = sb("ident_f", [128, 128], F32)
    ident_b = sb("ident_b", [128, 128], BF16)
    tri = sb("tri", [128, 128], BF16)
    m12 = sb("m12", [32, 2], F32)
    xT = sb("xT", [128, 8, SEQ], BF16)
    catT = sb("catT", [128, 8, SEQ], BF16)
    qa = [sb("qa%d" % i, [68, SEQ], BF16) for i in range(2)]
    ka = [sb("ka%d" % i, [68, SEQ], BF16) for i in range(2)]
    Vh = [sb("Vh%d" % i, [128, TPS, 65], BF16) for i in range(2)]
    aug32 = sb("aug32", [32, SEQ], BF16)
    fs = [sb("fs%d" % i, [32, 512], F32) for i in range(3)]
    fh = sb("fh", [32, 512], BF16)
    pt = [sb("pt%d" % i, [128, 512], BF16) for i in range(4)]
    u = sb("u", [128, SEQ + 2], F32)
    ytmp = sb("ytmp", [128, 512], F32)
    Csb = sb("Csb", [128, 512], F32)
    opair = sb("opair", [128, TPS, 128], BF16)
    rec = sb("rec", [128, 4], F32)
    xs = [sb("xs%d" % i, [128, D], F32) for i in range(2)]
    yts = [sb("yt%d" % i, [128, D], F32) for i in range(2)]
    carry = sb("carry", [32, 1], F32)
    alloc_ln(C)

    win_keys = cast_load_w(C, Win, w_in, 8, EVEN_IN, "Win")
    wout_keys = cast_load_w(C, Wout, w_out, 8, D, "Wout")
    load_const(C, "ident_f", ident_f[:], "ident_f")
    load_const(C, "ident_b", ident_b[:], "ident_b")
    load_const(C, "tri", tri[:], "tri")
    load_const(C, "m12", m12[:], "m12")
    S.dma("sp", lambda e: e.dma_start(out=cw[:], in_=conv_w.rearrange("(c p) j -> p c j", p=128)), writes=["cw"])
    for r in range(4):
        S.dma("sp", lambda e, r=r: e.dma_start(out=bf32[r * 8:(r + 1) * 8, :], in_=b_forget.rearrange("(h o) -> h o", o=1)),
              writes=[("bf32", r)])
    S.dma("sp", lambda e: e.dma_start(out=gbc[:], in_=ln_g.partition_broadcast(128)), writes=["gbc"])
    S.dma("sp", lambda e: e.dma_start(out=bbc[:], in_=ln_b.partition_broadcast(128)), writes=["bbc"])
    S.op("dve", lambda e: e.tensor_scalar(out=bf32[:], in0=bf32[:], scalar1=-1.0, scalar2=None, op0=ALU.mult),
         reads=[("bf32", r) for r in range(4)], writes=["nbf"])
    for r in range(4):
        S.op("dve", lambda e, r=r: e.tensor_copy(out=Wf[:, :, r * 8:(r + 1) * 8], in_=Win[:, :, 3072:3080]),
             reads=win_keys, writes=["Wf"])
    S.op("dve", lambda e: e.memset(u[:, 0:2], 0.0), writes=["u"])
    for i in range(2):
        S.op("dve", lambda e, i=i: e.memset(Vh[i][:], 1.0), writes=[("Vh", i)])
        S.op("dve", lambda e, i=i: e.memset(qa[i][64:68, :], 1.0), writes=[("qa_aug", i)])
        S.op("dve", lambda e, i=i: e.memset(ka[i][64:68, :], 1.0), writes=[("ka_aug", i)])

    OFF_Q, OFF_K, OFF_V = 1536, 2048, 2560
    ucnt = [0]
    if C.debug:
        print("even phase sbuf bytes remaining", nc.sbuf_bytes_remaining)
    cnt = [0]

    for s in range(NSEQ):
        build_xT(C, x_in, s, xT, ident_f, xs)

        for r in range(4):
            bk = B[r % 2]
            bkey = ("bank", r % 2)
            for k in range(8):
                S.op("pe", lambda e, bk=bk, k=k, r=r: e.matmul(bk[0:32, :], lhsT=Wf[:, k, :], rhs=xT[:, k, r * 512:(r + 1) * 512],
                                                              start=(k == 0), stop=(k == 7)), reads=["Wf", "xT"], writes=[bkey])
            S.op("act", lambda e, bk=bk: e.activation(out=fs[0][:], in_=bk[0:32, :], func=AF.Exp, bias=bf32[:], scale=-1.0),
                 reads=[bkey, "nbf"], writes=["fs0"])
            S.op("act", lambda e: e.activation(out=fs[0][:], in_=fs[0][:], func=AF.Ln, bias=1.0, scale=1.0),
                 reads=["fs0"], writes=["fs0"])
            S.op("dve", lambda e: e.tensor_scalar(out=fs[0][:], in0=fs[0][:], scalar1=8.0, scalar2=None, op0=ALU.mult),
                 reads=["fs0"], writes=["fs0"])
            if r == 0:
                S.op("dve", lambda e: e.memset(fs[2][:], 1.0), writes=["fs2"])
                S.op("dve", lambda e: e.memset(carry[:], 0.0), writes=["carry"])
            else:
                S.op("dve", lambda e: e.tensor_copy(out=carry[:], in_=fs[1][:, 511:512]), reads=["fs1"], writes=["carry"])
            S.op("dve", lambda e: e.tensor_tensor_scan(out=fs[1][:], data0=fs[2][:], data1=fs[0][:], initial=carry[:],
                                                       op0=ALU.mult, op1=ALU.add), reads=["fs0", "fs2", "carry"], writes=["fs1"])
            S.op("dve", lambda e: e.tensor_copy(out=fh[:], in_=fs[1][:]), reads=["fs1"], writes=["fh"])
            S.op("dve", lambda e: e.tensor_tensor(out=fs[0][:], in0=fs[1][:], in1=fh[:], op=ALU.subtract),
                 reads=["fs1", "fh"], writes=["fs0"])
            S.op("dve", lambda e: e.tensor_scalar(out=fs[0][:], in0=fs[0][:], scalar1=m12[:, 1:2], scalar2=None, op0=ALU.mult),
                 reads=["fs0", "m12"], writes=["fs0"])
            S.op("dve", lambda e, r=r: e.scalar_tensor_tensor(out=aug32[:, r * 512:(r + 1) * 512], in0=fh[:], scalar=m12[:, 0:1],
                                                              in1=fs[0][:], op0=ALU.mult, op1=ALU.add),
                 reads=["fh", "fs0", "m12"], writes=["aug32"])

        for c in range(4):
            for r in range(4):
                pb, pc, px = B[0 + (r % 2)], B[2 + (r % 2)], B[5 + (r % 2)]
                kb, kc, kx = ("bank", r % 2), ("bank", 2 + r % 2), ("bank", 5 + r % 2)
                for (bk, bkey, col0) in ((pb, kb, 0), (pc, kc, 512), (px, kx, 1024)):
                    for k in range(8):
                        S.op("pe", lambda e, bk=bk, k=k, col0=col0, c=c, r=r: e.matmul(
                            bk[:, :], lhsT=Win[:, k, col0 + c * 128:col0 + (c + 1) * 128], rhs=xT[:, k, r * 512:(r + 1) * 512],
                            start=(k == 0), stop=(k == 7)), reads=win_keys + ["xT"], writes=[bkey])
                S.op("act", lambda e, pc=pc: e.copy(out=Csb[:], in_=pc[:, :]), reads=[kc], writes=["Csb"])
                S.op("dve", lambda e, px=px, r=r: e.tensor_tensor(out=u[:, 2 + r * 512:2 + (r + 1) * 512], in0=px[:, :], in1=Csb[:],
                                                                 op=ALU.mult), reads=[kx, "Csb"], writes=["u"])
                S.op("dve", lambda e, c=c, r=r: e.tensor_scalar(out=ytmp[:], in0=u[:, 2 + r * 512:2 + (r + 1) * 512],
                                                                scalar1=cw[:, c, 2:3], scalar2=None, op0=ALU.mult),
                     reads=["u", "cw"], writes=["ytmp"])
                S.op("dve", lambda e, c=c, r=r: e.scalar_tensor_tensor(out=ytmp[:], in0=u[:, 1 + r * 512:1 + (r + 1) * 512],
                                                                       scalar=cw[:, c, 1:2], in1=ytmp[:], op0=ALU.mult, op1=ALU.add),
                     reads=["u", "cw", "ytmp"], writes=["ytmp"])
                S.op("dve", lambda e, c=c, r=r: e.scalar_tensor_tensor(out=ytmp[:], in0=u[:, r * 512:(r + 1) * 512],
                                                                       scalar=cw[:, c, 0:1], in1=ytmp[:], op0=ALU.mult, op1=ALU.add),
                     reads=["u", "cw", "ytmp"], writes=["ytmp"])
                S.op("dve", lambda e, pb=pb, c=c, r=r: e.tensor_tensor(out=catT[:, c, r * 512:(r + 1) * 512], in0=pb[:, :], in1=ytmp[:],
                                                                      op=ALU.mult), reads=[kb, "ytmp"], writes=["catT"])

        def inproj_units(h):
            par = h % 2
            units = []

            def aug():
                S.dma("sp", lambda e: e.dma_start(out=qa[par][64:65, :], in_=aug32[h:h + 1, :]), reads=["aug32"], writes=[("qa_aug", par)])
                S.dma("sp", lambda e: e.dma_start(out=qa[par][65:66, :], in_=aug32[8 + h:9 + h, :]), reads=["aug32"], writes=[("qa_aug2", par)])
                S.dma("sp", lambda e: e.dma_start(out=ka[par][66:67, :], in_=aug32[16 + h:17 + h, :]), reads=["aug32"], writes=[("ka_aug", par)])
                S.dma("sp", lambda e: e.dma_start(out=ka[par][67:68, :], in_=aug32[24 + h:25 + h, :]), reads=["aug32"], writes=[("ka_aug2", par)])

            def qk_unit(dst, dkey, off, r, first):
                def f():
                    if first:
                        aug()
                    bi = ucnt[0] % 2
                    ucnt[0] += 1
                    bk, bkey = B[bi], ("bank", bi)
                    for k in range(8):
                        S.op("pe", lambda e, k=k: e.matmul(
                            bk[0:64, :], lhsT=Win[:, k, off + h * 64:off + (h + 1) * 64], rhs=xT[:, k, r * 512:(r + 1) * 512],
                            start=(k == 0), stop=(k == 7)), reads=win_keys + ["xT"], writes=[bkey])
                    S.op("dve", lambda e: e.tensor_copy(out=dst[0:64, r * 512:(r + 1) * 512], in_=bk[0:64, :]),
                         reads=[bkey], writes=[dkey])
                return f

            def v_unit(g):
                def f():
                    bi = ucnt[0] % 2
                    ucnt[0] += 1
                    bk, bkey = B[bi], ("bank", bi)
                    for tl in range(8):
                        t = g * 8 + tl
                        for k in range(8):
                            S.op("pe", lambda e, k=k, t=t, tl=tl: e.matmul(
                                bk[:, tl * 64:(tl + 1) * 64], lhsT=xT[:, k, t * 128:(t + 1) * 128],
                                rhs=Win[:, k, OFF_V + h * 64:OFF_V + (h + 1) * 64], start=(k == 0), stop=(k == 7)),
                                reads=win_keys + ["xT"], writes=[bkey])
                    S.op("dve", lambda e: e.tensor_copy(out=Vh[par][:, g * 8:(g + 1) * 8, 0:64],
                                                        in_=bk[:, :].rearrange("p (t c) -> p t c", t=8)),
                         reads=[bkey], writes=[("Vh", par)])
                return f

            first = True
            for (dst, dkey, off) in ((qa[par], ("qa", par), OFF_Q), (ka[par], ("ka", par), OFF_K)):
                for r in range(4):
                    units.append(qk_unit(dst, dkey, off, r, first))
                    first = False
            for g in range(2):
                units.append(v_unit(g))
            return units

        def attn(h, fill):
            par = h % 2
            qk_reads = [("qa", par), ("ka", par), ("qa_aug", par), ("qa_aug2", par), ("ka_aug", par), ("ka_aug2", par)]
            items = [(qb, j) for qb in range(4) for j in range(4 * qb + 4)]
            info = {}

            def emit_score(idx):
                qb, j = items[idx]
                d = j - 4 * qb
                n0 = max(0, d) * 128
                si = cnt[0] % 3
                pi = cnt[0] % 4
                cnt[0] += 1
                Sb, skey = B[4 + si], ("bank", 4 + si)
                P, pkey = pt[pi], ("pt", pi)
                info[idx] = (Sb, skey, P, pkey, d, n0)
                S.op("pe", lambda e: e.matmul(
                    Sb[:, n0:512], lhsT=ka[par][0:68, j * 128:(j + 1) * 128], rhs=qa[par][0:68, qb * 512 + n0:(qb + 1) * 512],
                    start=True, stop=True), reads=qk_reads, writes=[skey])

            def emit_rest(idx):
                qb, j = items[idx]
                Sb, skey, P, pkey, d, n0 = info.pop(idx)
                O = B[2 + (qb % 2)]
                okey = ("bank", 2 + qb % 2)
                S.op("act", lambda e: e.activation(out=P[:, n0:512], in_=Sb[:, n0:512], func=AF.Exp, scale=0.125),
                     reads=[skey], writes=[pkey])
                if d >= 0:
                    S.op("dve", lambda e: e.tensor_tensor(out=P[:, n0:n0 + 128], in0=P[:, n0:n0 + 128], in1=tri[:],
                                                          op=ALU.mult), reads=[pkey, "tri"], writes=[pkey])
                for i in range(max(0, d), 4):
                    S.op("pe", lambda e, i=i: e.matmul(
                        O[:, i * 65:(i + 1) * 65], lhsT=P[:, i * 128:(i + 1) * 128], rhs=Vh[par][:, j, :],
                        start=(j == 0), stop=(j == 4 * qb + i)), reads=[pkey, ("Vh", par)], writes=[okey])
                if j != 4 * qb + 3:
                    return
                S.op("dve", lambda e: e.reciprocal(out=rec[:, 0:4], in_=O[:, 0:260].rearrange("p (i c) -> p i c", i=4)[:, :, 64]),
                     reads=[okey], writes=["rec"])
                for i in range(4):
                    S.op("dve", lambda e, i=i: e.tensor_scalar(
                        out=opair[:, qb * 4 + i, (h % 2) * 64:(h % 2) * 64 + 64], in0=O[:, i * 65:i * 65 + 64],
                        scalar1=rec[:, i:i + 1], scalar2=None, op0=ALU.mult), reads=[okey, "rec"], writes=["opair"])

            stride = max(1, len(items) // (len(fill) + 1))
            emit_score(0)
            emit_score(1)
            for idx in range(len(items)):
                if idx + 2 < len(items):
                    emit_score(idx + 2)
                emit_rest(idx)
                if fill and idx % stride == stride - 1:
                    fill.pop(0)()
            while fill:
                fill.pop(0)()

        def pair_transposes(h):
            for g in range(2):
                for i in range(8):
                    S.op("pe", lambda e, i=i, g=g: e.transpose(out=C.pTr[:, i * 128:(i + 1) * 128], in_=opair[:, g * 8 + i, :],
                                                               identity=ident_b[:]), reads=["opair", "ident_b"], writes=["pTr"])
                S.op("act", lambda e, g=g: e.copy(out=catT[:, 4 + h // 2, g * 1024:(g + 1) * 1024], in_=C.pTr[:, :]),
                     reads=["pTr"], writes=["catT"])

        for u_ in inproj_units(0):
            u_()
        for h in range(8):
            attn(h, inproj_units(h + 1) if h + 1 < 8 else [])
            if h % 2 == 1:
                pair_transposes(h)

        outproj_ln(C, x_in, x_out, s, catT, Wout, wout_keys, gbc, bbc, xs, yts)
    end_phase(C)


def emit_odd(C, layer, x_in, x_out, w_in, lam_q1, lam_k1, lam_q2, lam_k2, subln_g, w_out, ln_g, ln_b):
    new_phase(C)
    S, sb, nc = C.S, C.sb, C.nc
    B = C.banks
    lam_init = 0.8 - 0.6 * math.exp(-0.3 * layer)
    Win = sb("Win", [128, 8, 3 * D], BF16)
    Wout = sb("Wout", [128, 8, D], BF16)
    gbc = sb("gbc", [128, D], F32)
    bbc = sb("bbc", [128, D], F32)
    ident_f = sb("ident_f", [128, 128], F32)
    ident_b = sb("ident_b", [128, 128], BF16)
    corr = sb("corr", [128, 8, 128], BF16)
    xT = sb("xT", [128, 8, SEQ], BF16)
    catT = sb("catT", [128, 8, SEQ], BF16)
    NQB = 2
    q1a = [sb("q1a%d" % i, [68, SEQ], BF16) for i in range(NQB)]
    q2a = [sb("q2a%d" % i, [68, SEQ], BF16) for i in range(NQB)]
    k1a = [sb("k1a%d" % i, [68, SEQ], BF16) for i in range(NQB)]
    k2a = [sb("k2a%d" % i, [68, SEQ], BF16) for i in range(NQB)]
    Vh = [sb("Vh%d" % i, [128, TPS, 129], BF16) for i in range(2)]
    pt = [sb("pt%d" % i, [128, 512], BF16) for i in range(4)]
    lamv = [sb("lamv%d" % i, [128, 64], F32) for i in range(4)]
    lsc = sb("lsc", [128, 8], F32)
    gv = sb("gv", [128, 128], F32)
    ot = sb("ot", [128, 128], F32)
    junk = sb("junk", [128, 128], F32)
    ohead = sb("ohead", [128, TPS, 128], BF16)
    rr = sb("rr", [128, 8], F32)
    xs = [sb("xs%d" % i, [128, D], F32) for i in range(2)]
    yts = [sb("yt%d" % i, [128, D], F32) for i in range(2)]
    alloc_ln(C)

    win_keys = cast_load_w(C, Win, w_in, 8, 3 * D, "Win")
    wout_keys = cast_load_w(C, Wout, w_out, 8, D, "Wout")
    load_const(C, "ident_f", ident_f[:], "ident_f")
    load_const(C, "ident_b", ident_b[:], "ident_b")
    load_const(C, "corr", corr[:], "corr")
    S.dma("sp", lambda e: e.dma_start(out=gbc[:], in_=ln_g.partition_broadcast(128)), writes=["gbc"])
    S.dma("sp", lambda e: e.dma_start(out=bbc[:], in_=ln_b.partition_broadcast(128)), writes=["bbc"])
    for i, v in enumerate((lam_q1, lam_k1, lam_q2, lam_k2)):
        S.dma("sp", lambda e, i=i, v=v: e.dma_start(out=lamv[i][:], in_=v.partition_broadcast(128)), writes=[("lamv", i)])
    S.dma("sp", lambda e: e.dma_start(out=gv[:], in_=subln_g.partition_broadcast(128)), writes=["gv_raw"])
    S.op("dve", lambda e: e.scalar_tensor_tensor(out=junk[:, 0:64], in0=lamv[0][:], scalar=1.0, in1=lamv[1][:], op0=ALU.mult,
                                                 op1=ALU.mult, accum_out=lsc[:, 0:1]), reads=[("lamv", 0), ("lamv", 1)], writes=["junk", "lsc0"])
    S.op("dve", lambda e: e.scalar_tensor_tensor(out=junk[:, 0:64], in0=lamv[2][:], scalar=1.0, in1=lamv[3][:], op0=ALU.mult,
                                                 op1=ALU.mult, accum_out=lsc[:, 1:2]), reads=[("lamv", 2), ("lamv", 3), "junk"], writes=["junk", "lsc1"])
    S.op("act", lambda e: e.activation(out=lsc[:, 2:4], in_=lsc[:, 0:2], func=AF.Exp), reads=["lsc0", "lsc1"], writes=["lsc23"])
    S.op("dve", lambda e: e.tensor_tensor(out=lsc[:, 4:5], in0=lsc[:, 3:4], in1=lsc[:, 2:3], op=ALU.subtract), reads=["lsc23"], writes=["lsc4"])
    S.op("dve", lambda e: e.tensor_scalar(out=lsc[:, 4:5], in0=lsc[:, 4:5], scalar1=-lam_init, scalar2=None, op0=ALU.add),
         reads=["lsc4"], writes=["nlam"])
    S.op("dve", lambda e: e.memset(lsc[:, 5:6], RMS_EPS), writes=["rmseps"])
    S.op("dve", lambda e: e.memset(lsc[:, 6:7], -0.5), writes=["mhalf"])
    S.op("dve", lambda e: e.tensor_scalar(out=gv[:], in0=gv[:], scalar1=(1.0 - lam_init), scalar2=None, op0=ALU.mult),
         reads=["gv_raw"], writes=["gv"])
    for i in range(2):
        S.op("dve", lambda e, i=i: e.memset(Vh[i][:], 1.0), writes=[("Vh", i)])

    cnt = [0]
    if C.debug:
        print("odd phase sbuf bytes remaining", nc.sbuf_bytes_remaining)
    for s in range(NSEQ):
        build_xT(C, x_in, s, xT, ident_f, xs)

        def inproj_units(h):
            par = h % NQB
            vpar = h % 2
            units = []

            def aug():
                for (t_, nm) in ((q1a, "q1"), (q2a, "q2")):
                    S.dma("sp", lambda e, t_=t_: e.dma_start(out=t_[par][64:68, :], in_=C.consts["augq"][h]), writes=[(nm + "_aug", par)])
                for (t_, nm) in ((k1a, "k1"), (k2a, "k2")):
                    S.dma("sp", lambda e, t_=t_: e.dma_start(out=t_[par][64:68, :], in_=C.consts["augk"][h]), writes=[(nm + "_aug", par)])

            def qk_unit(d1, d2, n1, n2, off, r, first):
                def f():
                    if first:
                        aug()
                    bi = 4
                    bk, bkey = B[bi], ("bank", bi)
                    for k in range(8):
                        S.op("pe", lambda e, k=k: e.matmul(
                            bk[:, :], lhsT=Win[:, k, off + h * 128:off + (h + 1) * 128], rhs=xT[:, k, r * 512:(r + 1) * 512],
                            start=(k == 0), stop=(k == 7)), reads=win_keys + ["xT"], writes=[bkey])
                    S.op("dve", lambda e: e.tensor_copy(out=d1[0:64, r * 512:(r + 1) * 512], in_=bk[0:64, :]),
                         reads=[bkey], writes=[(n1, par)])
                    S.op("dve", lambda e: e.tensor_copy(out=d2[0:64, r * 512:(r + 1) * 512], in_=bk[64:128, :]),
                         reads=[bkey], writes=[(n2, par)])
                return f

            def v_unit(g):
                def f():
                    bi = 4
                    bk, bkey = B[bi], ("bank", bi)
                    for tl in range(4):
                        t = g * 4 + tl
                        for k in range(8):
                            S.op("pe", lambda e, k=k, t=t, tl=tl: e.matmul(
                                bk[:, tl * 128:(tl + 1) * 128], lhsT=xT[:, k, t * 128:(t + 1) * 128],
                                rhs=Win[:, k, 2 * D + h * 128:2 * D + (h + 1) * 128], start=(k == 0), stop=(k == 7)),
                                reads=win_keys + ["xT"], writes=[bkey])
                    S.op("dve", lambda e: e.tensor_copy(out=Vh[vpar][:, g * 4:(g + 1) * 4, 0:128],
                                                        in_=bk[:, :].rearrange("p (t c) -> p t c", t=4)),
                         reads=[bkey], writes=[("Vh", vpar)])
                return f

            first = True
            for (d1, d2, n1, n2, off) in ((q1a[par], q2a[par], "q1", "q2", 0), (k1a[par], k2a[par], "k1", "k2", D)):
                for r in range(4):
                    units.append(qk_unit(d1, d2, n1, n2, off, r, first))
                    first = False
            for g in range(4):
                units.append(v_unit(g))
            return units

        def attn(h, fill):
            par = h % NQB
            vpar = h % 2
            rd1 = [("q1", par), ("k1", par), ("q1_aug", par), ("k1_aug", par)]
            rd2 = [("q2", par), ("k2", par), ("q2_aug", par), ("k2_aug", par)]
            QB = 256
            items = [(qb, j) for qb in range(SEQ // QB) for j in range(2 * qb + 2)]
            info = {}

            def emit_score(idx):
                qb, j = items[idx]
                d = j - 2 * qb
                n0 = max(0, d) * 128
                si = cnt[0] % 2
                pi = cnt[0] % 4
                cnt[0] += 1
                Sb, skey = B[5 + si], ("bank", 5 + si)
                P, pkey = pt[pi], ("pt", pi)
                info[idx] = (Sb, skey, P, pkey, d, n0)
                for (br, qq, kk, rd) in ((0, q1a[par], k1a[par], rd1), (1, q2a[par], k2a[par], rd2)):
                    S.op("pe", lambda e, br=br, qq=qq, kk=kk: e.matmul(
                        Sb[:, br * 256 + n0:(br + 1) * 256], lhsT=kk[0:68, j * 128:(j + 1) * 128],
                        rhs=qq[0:68, qb * QB + n0:(qb + 1) * QB], start=True, stop=True), reads=rd, writes=[skey])

            def emit_rest(idx):
                qb, j = items[idx]
                Sb, skey, P, pkey, d, n0 = info.pop(idx)
                ob_ = 2 if qb % 2 == 0 else 0
                O1, O2 = B[ob_], B[ob_ + 1]
                k1_, k2_ = ("bank", ob_), ("bank", ob_ + 1)
                if n0 == 0:
                    S.op("act", lambda e: e.activation(out=P[:, :], in_=Sb[:, :], func=AF.Exp, scale=0.125),
                         reads=[skey], writes=[pkey])
                else:
                    for br in range(2):
                        S.op("act", lambda e, br=br: e.activation(
                            out=P[:, br * 256 + n0:(br + 1) * 256], in_=Sb[:, br * 256 + n0:(br + 1) * 256], func=AF.Exp, scale=0.125),
                            reads=[skey], writes=[pkey])
                if d >= 0:
                    for br in range(2):
                        S.op("dve", lambda e, br=br: e.tensor_tensor(
                            out=P[:, br * 256 + n0:br * 256 + n0 + 128], in0=P[:, br * 256 + n0:br * 256 + n0 + 128],
                            in1=corr[:, h, :], op=ALU.mult), reads=[pkey, "corr"], writes=[pkey])
                for i in range(max(0, d), 2):
                    for (br, O, ok) in ((0, O1, k1_), (1, O2, k2_)):
                        S.op("pe", lambda e, O=O, i=i, br=br: e.matmul(
                            O[:, i * 129:(i + 1) * 129], lhsT=P[:, br * 256 + i * 128:br * 256 + (i + 1) * 128],
                            rhs=Vh[vpar][:, j, :], start=(j == 0), stop=(j == 2 * qb + i)), reads=[pkey, ("Vh", vpar)], writes=[ok])
                if j != 2 * qb + 1:
                    return
                S.op("dve", lambda e: e.reciprocal(out=rr[:, 0:2], in_=O1[:, 0:258].rearrange("p (i c) -> p i c", i=2)[:, :, 128]),
                     reads=[k1_], writes=["rr01"])
                S.op("dve", lambda e: e.reciprocal(out=rr[:, 2:4], in_=O2[:, 0:258].rearrange("p (i c) -> p i c", i=2)[:, :, 128]),
                     reads=[k2_], writes=["rr23"])
                S.op("dve", lambda e: e.tensor_scalar(out=rr[:, 2:4], in0=rr[:, 2:4], scalar1=lsc[:, 4:5], scalar2=None, op0=ALU.mult),
                     reads=["rr23", "nlam"], writes=["rr23"])
                for i in range(2):
                    qt = qb * 2 + i
                    S.op("dve", lambda e, i=i: e.tensor_scalar(out=ot[:], in0=O1[:, i * 129:i * 129 + 128], scalar1=rr[:, i:i + 1],
                                                               scalar2=None, op0=ALU.mult), reads=[k1_, "rr01"], writes=["ot"])
                    S.op("dve", lambda e, i=i: e.scalar_tensor_tensor(out=ot[:], in0=O2[:, i * 129:i * 129 + 128], scalar=rr[:, 2 + i:3 + i],
                                                                      in1=ot[:], op0=ALU.mult, op1=ALU.add), reads=[k2_, "rr23", "ot"], writes=["ot"])
                    S.op("dve", lambda e: e.scalar_tensor_tensor(out=junk[:], in0=ot[:], scalar=1.0, in1=ot[:], op0=ALU.mult, op1=ALU.mult,
                                                                 accum_out=rr[:, 4:5]), reads=["ot", "junk"], writes=["junk", "ss"])
                    S.op("dve", lambda e: e.tensor_scalar(out=rr[:, 6:7], in0=rr[:, 4:5], scalar1=1.0 / 128.0, scalar2=RMS_EPS, op0=ALU.mult,
                                                          op1=ALU.add), reads=["ss"], writes=["msq"])
                    S.op("pool", lambda e: e.tensor_tensor(out=rr[:, 5:6], in0=rr[:, 6:7], in1=lsc[:, 6:7], op=ALU.pow),
                         reads=["msq", "mhalf"], writes=["rs_"])
                    S.op("dve", lambda e, qt=qt: e.scalar_tensor_tensor(out=ohead[:, qt, :], in0=ot[:], scalar=rr[:, 5:6], in1=gv[:], op0=ALU.mult,
                                                                        op1=ALU.mult), reads=["ot", "rs_", "gv"], writes=["ohead"])

            stride = max(1, len(items) // (len(fill) + 1))
            emit_score(0)
            for idx in range(len(items)):
                if idx + 1 < len(items):
                    emit_score(idx + 1)
                emit_rest(idx)
                if fill and idx % stride == stride - 1:
                    fill.pop(0)()
            while fill:
                fill.pop(0)()

        def head_transposes(h):
            for g in range(2):
                for i in range(8):
                    S.op("pe", lambda e, i=i, g=g: e.transpose(out=C.pTr[:, i * 128:(i + 1) * 128], in_=ohead[:, g * 8 + i, :],
                                                               identity=ident_b[:]), reads=["ohead", "ident_b"], writes=["pTr"])
                S.op("act", lambda e, g=g: e.copy(out=catT[:, h, g * 1024:(g + 1) * 1024], in_=C.pTr[:, :]),
                     reads=["pTr"], writes=["catT"])

        for u_ in inproj_units(0):
            u_()
        for h in range(8):
            attn(h, inproj_units(h + 1) if h + 1 < 8 else [])
            head_transposes(h)

        outproj_ln(C, x_in, x_out, s, catT, Wout, wout_keys, gbc, bbc, xs, yts)
    end_phase(C)


def emit_moe(C, x_in, x_out, w_group, b_group, w_expert, b_expert, w_gate, w_up, w_down, ln_g, ln_b, xslots, yslots):
    new_phase(C, n_f32=6, n_tr=2)
    S, sb, nc = C.S, C.sb, C.nc
    B = C.banks
    NB = CAP // 128
    ident_f = sb("ident_f", [128, 128], F32)
    ident_b = sb("ident_b", [128, 128], BF16)
    tri = sb("tri", [128, 128], BF16)
    ones_b = sb("ones_b", [128, 128], BF16)
    basem1 = sb("basem1", [128, NEXP], F32)
    Wr = sb("Wr", [128, 8, 36], F32)
    brt = sb("brt", [128, 36], F32)
    gbc = sb("gbc", [128, D], F32)
    bbc = sb("bbc", [128, D], F32)
    xs = [sb("xs%d" % i, [128, D], F32) for i in range(2)]
    xb = [sb("xb%d" % i, [128, D], BF16) for i in range(2)]
    xTt = sb("xTt", [128, 8, 128], F32)
    lg = sb("lg", [128, 36], F32)
    me = sb("me", [128, 32], F32)
    oh1 = sb("oh1", [128, 32], F32)
    oh2 = sb("oh2", [128, 32], F32)
    ohb = sb("ohb", [128, 32], BF16)
    Q = sb("Q", [128, 32], F32)
    cntt = sb("cntt", [128, 32], F32)
    sc = sb("sc", [128, 16], F32)
    junk = sb("junk", [128, 32], F32)
    sf = sb("sf", [128, 2], F32)
    idx_all = sb("idx_all", [128, NT, 2], I32)
    w_all = sb("w_all", [128, NT, 2], F32)
    wg = [sb("wg%d" % i, [128, 8, 512], BF16) for i in range(2)]
    wu = [sb("wu%d" % i, [128, 8, 512], BF16) for i in range(2)]
    wd = [sb("wd%d" % i, [128, 4, D], BF16) for i in range(2)]
    xg = [sb("xg%d" % i, [128, D], BF16) for i in range(2)]
    XT = [sb("XT%d" % i, [128, 8, CAP], BF16) for i in range(2)]
    sg = [sb("sg%d" % i, [128, CAP], F32) for i in range(2)]
    hT = [sb("hT%d" % i, [128, 4, CAP], BF16) for i in range(2)]
    yo = [sb("yo%d" % i, [128, D], BF16) for i in range(2)]
    y1 = [sb("y1_%d" % i, [128, D], BF16) for i in range(2)]
    y2 = [sb("y2_%d" % i, [128, D], BF16) for i in range(2)]
    acc = [sb("acc%d" % i, [128, D], F32) for i in range(2)]
    alloc_ln(C)

    load_const(C, "ident_f", ident_f[:], "ident_f")
    load_const(C, "ident_b", ident_b[:], "ident_b")
    load_const(C, "tri", tri[:], "tri")
    load_const(C, "ones_b", ones_b[:], "ones_b")
    load_const(C, "basem1", basem1[:], "basem1")
    S.dma("sp", lambda e: e.dma_start(out=Wr[:, :, 0:4], in_=w_group.rearrange("(k p) n -> p k n", p=128)), writes=["Wr_g"])
    S.dma("sp", lambda e: e.dma_start(out=Wr[:, :, 4:36], in_=w_expert.rearrange("(k p) n -> p k n", p=128)), writes=["Wr_e"])
    S.dma("sp", lambda e: e.dma_start(out=brt[:, 0:4], in_=b_group.partition_broadcast(128)), writes=["br_g"])
    S.dma("sp", lambda e: e.dma_start(out=brt[:, 4:36], in_=b_expert.partition_broadcast(128)), writes=["br_e"])
    S.dma("sp", lambda e: e.dma_start(out=gbc[:], in_=ln_g.partition_broadcast(128)), writes=["gbc"])
    S.dma("sp", lambda e: e.dma_start(out=bbc[:], in_=ln_b.partition_broadcast(128)), writes=["bbc"])
    S.op("dve", lambda e: e.memset(cntt[:], 0.0), writes=["cntt"])

    def load_expert(e_):
        p = e_ % 2
        keys = []
        for (dst, src, nk, ncol, nm) in ((wg[p], w_gate[e_], 8, 512, "wg"), (wu[p], w_up[e_], 8, 512, "wu"), (wd[p], w_down[e_], 4, D, "wd")):
            v = src.rearrange("(k p) n -> p k n", p=128)
            half = nk // 2
            for a in range(2):
                kk = (nm, p, a)
                keys.append(kk)
                S.dma("pool", lambda e, dst=dst, v=v, a=a, half=half: e.dma_start(out=dst[:, a * half:(a + 1) * half, :],
                                                                                 in_=v[:, a * half:(a + 1) * half, :]), writes=[kk])
        return keys

    wkeys = {0: load_expert(0), 1: load_expert(1)}
    if not getattr(C, "trash_zeroed", False):
        C.trash_zeroed = True
        S.op("dve", lambda e: e.memset(yo[0][0:32, :], 0.0), writes=[("yo", 0, 0), ("yo", 0, 1)])
        S.dma("sp", lambda e: e.dma_start(out=yslots.rearrange("(e s) d -> e s d", s=STRIDE)[:, CAP, :], in_=yo[0][0:32, :]),
              reads=[("yo", 0, 0), ("yo", 0, 1)], writes=["ytrash"])
    scat_keys = []
    for t in range(NT):
        p = t % 2
        xkey = ("xs", p)
        S.dma("sp", lambda e, t=t, p=p: e.dma_start(out=xs[p][:], in_=x_in[t * 128:(t + 1) * 128, :]), writes=[xkey])
        S.op("act", lambda e, p=p: e.copy(out=xb[p][:], in_=xs[p][:]), reads=[xkey], writes=[("xb", p)])
        for half in range(2):
            bk, bkey = B[half], ("bank", half)
            for j in range(4):
                k = half * 4 + j
                S.op("pe", lambda e, bk=bk, j=j, k=k, p=p: e.transpose(out=bk[:, j * 128:(j + 1) * 128], in_=xs[p][:, k * 128:(k + 1) * 128],
                                                                       identity=ident_f[:]), reads=[xkey, "ident_f"], writes=[bkey])
            S.op("dve" if half == 0 else "act",
                 (lambda e, bk=bk, half=half: e.tensor_copy(out=xTt[:, half * 4:half * 4 + 4, :], in_=bk[:, :].rearrange("p (j c) -> p j c", j=4)))
                 if half == 0 else
                 (lambda e, bk=bk, half=half: e.copy(out=xTt[:, half * 4:half * 4 + 4, :], in_=bk[:, :].rearrange("p (j c) -> p j c", j=4))),
                 reads=[bkey], writes=[("xTt", half)])
        L, lkey = B[2], ("bank", 2)
        for k in range(8):
            S.op("pe", lambda e, k=k: e.matmul(L[:, 0:36], lhsT=xTt[:, k, :], rhs=Wr[:, k, :], start=(k == 0), stop=(k == 7)),
                 reads=[("xTt", 0), ("xTt", 1), "Wr_g", "Wr_e"], writes=[lkey])
        S.op("dve", lambda e: e.tensor_tensor(out=lg[:], in0=L[:, 0:36], in1=brt[:], op=ALU.add), reads=[lkey, "br_g", "br_e"], writes=["lg"])
        S.op("dve", lambda e: e.reduce_max(out=sc[:, 0:1], in_=lg[:, 0:4], axis=AX.X), reads=["lg"], writes=["gmax"])
        S.op("dve", lambda e: e.tensor_scalar(out=sc[:, 1:2], in0=sc[:, 0:1], scalar1=-1.0, scalar2=None, op0=ALU.mult), reads=["gmax"], writes=["ngmax"])
        S.op("act", lambda e: e.activation(out=junk[:, 0:4], in_=lg[:, 0:4], func=AF.Exp, bias=sc[:, 1:2], scale=1.0, accum_out=sc[:, 2:3]),
             reads=["lg", "ngmax", "junk"], writes=["junk", "gsum"])
        S.op("dve", lambda e: e.reciprocal(out=sc[:, 3:4], in_=sc[:, 2:3]), reads=["gsum"], writes=["gw"])
        S.op("dve", lambda e: e.tensor_scalar(out=sc[:, 4:8], in0=lg[:, 0:4], scalar1=sc[:, 0:1], scalar2=None, op0=ALU.is_equal),
             reads=["lg", "gmax"], writes=["goh"])
        S.op("dve", lambda e: e.tensor_scalar(out=sc[:, 4:8], in0=sc[:, 4:8], scalar1=1e30, scalar2=-1e30, op0=ALU.mult, op1=ALU.add),
             reads=["goh"], writes=["pen"])
        for g in range(4):
            S.op("dve", lambda e, g=g: e.tensor_scalar(out=me[:, g * 8:(g + 1) * 8], in0=lg[:, 4 + g * 8:12 + g * 8], scalar1=sc[:, 4 + g:5 + g],
                                                       scalar2=None, op0=ALU.add), reads=["lg", "pen"], writes=["me"])
        S.op("dve", lambda e: e.reduce_max(out=sc[:, 8:9], in_=me[:], axis=AX.X), reads=["me"], writes=["m1"])
        S.op("dve", lambda e: e.tensor_scalar(out=oh1[:], in0=me[:], scalar1=sc[:, 8:9], scalar2=None, op0=ALU.is_equal), reads=["me", "m1"], writes=["oh1"])
        S.op("dve", lambda e: e.scalar_tensor_tensor(out=me[:], in0=oh1[:], scalar=-1e30, in1=me[:], op0=ALU.mult, op1=ALU.add),
             reads=["oh1", "me"], writes=["me"])
        S.op("dve", lambda e: e.reduce_max(out=sc[:, 9:10], in_=me[:], axis=AX.X), reads=["me"], writes=["m2"])
        S.op("dve", lambda e: e.tensor_scalar(out=oh2[:], in0=me[:], scalar1=sc[:, 9:10], scalar2=None, op0=ALU.is_equal), reads=["me", "m2"], writes=["oh2"])
        S.op("dve", lambda e: e.tensor_tensor(out=sc[:, 10:11], in0=sc[:, 9:10], in1=sc[:, 8:9], op=ALU.subtract), reads=["m1", "m2"], writes=["dm"])
        S.op("act", lambda e: e.activation(out=sc[:, 10:11], in_=sc[:, 10:11], func=AF.Exp), reads=["dm"], writes=["dm"])
        S.op("dve", lambda e: e.tensor_scalar(out=sc[:, 10:11], in0=sc[:, 10:11], scalar1=1.0, scalar2=None, op0=ALU.add), reads=["dm"], writes=["dm"])
        S.op("dve", lambda e: e.reciprocal(out=sc[:, 11:12], in_=sc[:, 10:11]), reads=["dm"], writes=["sg1"])
        S.op("dve", lambda e, t=t: e.tensor_tensor(out=w_all[:, t, 0:1], in0=sc[:, 11:12], in1=sc[:, 3:4], op=ALU.mult), reads=["sg1", "gw"], writes=[("w_all", t)])
        S.op("dve", lambda e, t=t: e.tensor_tensor(out=w_all[:, t, 1:2], in0=sc[:, 3:4], in1=w_all[:, t, 0:1], op=ALU.subtract),
             reads=["gw", ("w_all", t)], writes=[("w_all", t)])
        S.op("dve", lambda e: e.tensor_tensor(out=ohb[:], in0=oh1[:], in1=oh2[:], op=ALU.add), reads=["oh1", "oh2"], writes=["ohb"])
        R, rkey = B[3], ("bank", 3)
        S.op("pe", lambda e: e.matmul(R[:, 0:32], lhsT=tri[:], rhs=ohb[:], start=True, stop=True), reads=["tri", "ohb"], writes=[rkey])
        S.op("pe", lambda e: e.matmul(R[:, 32:64], lhsT=ones_b[:], rhs=ohb[:], start=True, stop=True), reads=["ones_b", "ohb"], writes=[rkey])
        S.op("dve", lambda e: e.tensor_tensor(out=Q[:], in0=R[:, 0:32], in1=cntt[:], op=ALU.add), reads=[rkey, "cntt"], writes=["Q"])
        S.op("dve", lambda e: e.tensor_tensor(out=cntt[:], in0=R[:, 32:64], in1=cntt[:], op=ALU.add), reads=[rkey, "cntt", "Q"], writes=["cntt"])
        S.op("dve", lambda e: e.tensor_scalar(out=Q[:], in0=Q[:], scalar1=float(CAP + 1), scalar2=None, op0=ALU.min), reads=["Q"], writes=["Q"])
        S.op("dve", lambda e: e.tensor_tensor(out=Q[:], in0=Q[:], in1=basem1[:], op=ALU.add), reads=["Q", "basem1"], writes=["Q"])
        S.op("dve", lambda e: e.scalar_tensor_tensor(out=junk[:], in0=oh1[:], scalar=1.0, in1=Q[:], op0=ALU.mult, op1=ALU.mult, accum_out=sf[:, 0:1]),
             reads=["oh1", "Q", "junk"], writes=["junk", "sf0"])
        S.op("dve", lambda e: e.scalar_tensor_tensor(out=junk[:], in0=oh2[:], scalar=1.0, in1=Q[:], op0=ALU.mult, op1=ALU.mult, accum_out=sf[:, 1:2]),
             reads=["oh2", "Q", "junk"], writes=["junk", "sf1"])
        S.op("dve", lambda e, t=t: e.tensor_copy(out=idx_all[:, t, :], in_=sf[:]), reads=["sf0", "sf1"], writes=[("idx", t)])
        for kk in range(2):
            sk = ("scat", t, kk)
            scat_keys.append(sk)
            S.dma("pool", lambda e, t=t, kk=kk, p=p: e.indirect_dma_start(
                out=xslots, out_offset=bass.IndirectOffsetOnAxis(ap=idx_all[:, t, kk:kk + 1], axis=0), in_=xb[p][:], in_offset=None),
                reads=[("xb", p), ("idx", t)], writes=[sk])

    ycnt = 0
    ykeys = []
    xgc = [0]

    def prep_block(e_, blk):
        p = e_ % 2
        gp = xgc[0] % 2
        xgc[0] += 1
        pT, pTk = C.pTrs[gp], ("pTr", gp)
        row0 = e_ * STRIDE + blk * 128
        S.dma("sp", lambda e: e.dma_start(out=xg[gp][:], in_=xslots[row0:row0 + 128, :]), reads=scat_keys, writes=[("xg", gp)])
        for k in range(8):
            S.op("pe", lambda e, k=k: e.transpose(out=pT[:, k * 128:(k + 1) * 128], in_=xg[gp][:, k * 128:(k + 1) * 128],
                                                  identity=ident_b[:]), reads=[("xg", gp), "ident_b"], writes=[pTk])
        S.op("dve", lambda e: e.tensor_copy(out=XT[p][:, :, blk * 128:(blk + 1) * 128],
                                            in_=pT[:, :].rearrange("p (k c) -> p k c", k=8)), reads=[pTk], writes=[("XT", p)])

    for blk in range(NB):
        prep_block(0, blk)
    for e_ in range(NEXP):
        p = e_ % 2
        if e_ >= 1 and e_ + 1 < NEXP:
            wkeys[e_ + 1] = load_expert(e_ + 1)
        wk = wkeys[e_]
        for hc in range(4):
            G, gkey = B[0 + hc % 2], ("bank", hc % 2)
            U, ukey = B[2 + hc % 2], ("bank", 2 + hc % 2)
            for (bk, bkey, w_) in ((G, gkey, wg[p]), (U, ukey, wu[p])):
                for k in range(8):
                    S.op("pe", lambda e, bk=bk, k=k, w_=w_, hc=hc, p=p: e.matmul(bk[:, 0:CAP], lhsT=w_[:, k, hc * 128:(hc + 1) * 128], rhs=XT[p][:, k, :],
                                                                                 start=(k == 0), stop=(k == 7)), reads=wk + [("XT", p)], writes=[bkey])
            S.op("act", lambda e, G=G, hc=hc: e.activation(out=sg[hc % 2][:], in_=G[:, 0:CAP], func=AF.Silu), reads=[gkey], writes=[("sg", hc % 2)])
            S.op("dve", lambda e, U=U, hc=hc, p=p: e.tensor_tensor(out=hT[p][:, hc, :], in0=U[:, 0:CAP], in1=sg[hc % 2][:], op=ALU.mult),
                 reads=[ukey, ("sg", hc % 2)], writes=[("hT", p)])
            if e_ + 1 < NEXP and hc < NB:
                prep_block(e_ + 1, hc)
        if e_ + 1 < NEXP:
            for blk in range(4, NB):
                prep_block(e_ + 1, blk)
        for blk in range(NB):
            yp = ycnt % 2
            ycnt += 1
            for half in range(2):
                Y, ykey = B[4 + half], ("bank", 4 + half)
                for hc in range(4):
                    S.op("pe", lambda e, Y=Y, hc=hc, blk=blk, half=half, p=p: e.matmul(
                        Y[:, :], lhsT=hT[p][:, hc, blk * 128:(blk + 1) * 128], rhs=wd[p][:, hc, half * 512:(half + 1) * 512],
                        start=(hc == 0), stop=(hc == 3)), reads=wk + [("hT", p)], writes=[ykey])
                if half == 0:
                    S.op("act", lambda e, Y=Y, yp=yp: e.copy(out=yo[yp][:, 0:512], in_=Y[:, :]), reads=[ykey], writes=[("yo", yp, 0)])
                else:
                    S.op("dve", lambda e, Y=Y, yp=yp: e.tensor_copy(out=yo[yp][:, 512:1024], in_=Y[:, :]), reads=[ykey], writes=[("yo", yp, 1)])
            row0 = e_ * STRIDE + blk * 128
            yk = ("ysl", e_, blk)
            ykeys.append(yk)
            S.dma("sp", lambda e, row0=row0, yp=yp: e.dma_start(out=yslots[row0:row0 + 128, :], in_=yo[yp][:]),
                  reads=[("yo", yp, 0), ("yo", yp, 1)], writes=[yk])

    pend3 = None
    for t in range(NT):
        p = t % 2
        xkey = ("xs", p)
        S.dma("sp", lambda e, t=t, p=p: e.dma_start(out=xs[p][:], in_=x_in[t * 128:(t + 1) * 128, :]), writes=[xkey])
        for (yy, nm, kk) in ((y1[p], "y1", 0), (y2[p], "y2", 1)):
            S.dma("pool", lambda e, t=t, kk=kk, yy=yy: e.indirect_dma_start(
                out=yy[:], out_offset=None, in_=yslots, in_offset=bass.IndirectOffsetOnAxis(ap=idx_all[:, t, kk:kk + 1], axis=0)),
                reads=ykeys + [("idx", t)], writes=[(nm, p)])
        a, akey = acc[p], ("acc", p)
        S.op("act", lambda e, t=t, a=a, p=p: e.activation(out=a[:], in_=y2[p][:], func=AF.Identity, scale=w_all[:, t, 1:2]),
             reads=[("y2", p), ("w_all", t)], writes=[akey])
        S.op("dve", lambda e, t=t, a=a, p=p: e.scalar_tensor_tensor(out=a[:], in0=y1[p][:], scalar=w_all[:, t, 0:1], in1=a[:], op0=ALU.mult, op1=ALU.add),
             reads=[("y1", p), ("w_all", t), akey], writes=[akey])
        S.op("dve", lambda e, a=a, p=p: e.scalar_tensor_tensor(out=a[:], in0=xs[p][:], scalar=ALPHA, in1=a[:], op0=ALU.mult, op1=ALU.add),
             reads=[xkey, akey], writes=[akey])
        ln_stats(C, a, akey, p)
        if pend3 is not None:
            ln_finish(C, *pend3)
        pend3 = (a, gbc, bbc, x_out[t * 128:(t + 1) * 128, :], akey, t, p)
    ln_finish(C, *pend3)
    end_phase(C)


def build_program(phases):
    nc = bass.Bass("TRN2", target_bir_lowering=False)
    C = Ctx()
    C.nc = nc
    C.debug = False
    outer = ExitStack()
    C.S = Sched(nc, outer)
    din = lambda name, shape, dt=F32: nc.dram_tensor(name, shape, dt, kind="ExternalInput").ap()
    C.consts = {k: din("c_" + k, shp, dt) for k, (shp, dt) in CONST_SPECS.items()}
    x_cur = din("x", [NTOK, D])
    need_moe = any(p[0] == "moe" for p in phases)
    if need_moe:
        xslots = nc.dram_tensor("xslots", [NSLOT, D], BF16, kind="Internal").ap()
        yslots = nc.dram_tensor("yslots", [NSLOT, D], BF16, kind="Internal").ap()
    for pi, (kind, layer) in enumerate(phases):
        last = pi == len(phases) - 1
        if last:
            x_next = nc.dram_tensor("y", [NTOK, D], F32, kind="ExternalOutput").ap()
        else:
            x_next = nc.dram_tensor("xact%d" % pi, [NTOK, D], F32, kind="Internal").ap()
        pre = "p%d_" % pi
        if kind == "even":
            emit_even(C, x_cur, x_next, din(pre + "w_in", [D, EVEN_IN]), din(pre + "b_forget", [8]), din(pre + "conv_w", [512, 3]),
                      din(pre + "w_out", [D, D]), din(pre + "ln_g", [D]), din(pre + "ln_b", [D]))
        elif kind == "odd":
            emit_odd(C, layer, x_cur, x_next, din(pre + "w_in", [D, 3 * D]), din(pre + "lam_q1", [64]), din(pre + "lam_k1", [64]),
                     din(pre + "lam_q2", [64]), din(pre + "lam_k2", [64]), din(pre + "subln_g", [128]), din(pre + "w_out", [D, D]),
                     din(pre + "ln_g", [D]), din(pre + "ln_b", [D]))
        else:
            emit_moe(C, x_cur, x_next, din(pre + "w_group", [D, 4]), din(pre + "b_group", [4]), din(pre + "w_expert", [D, 32]),
                     din(pre + "b_expert", [32]), din(pre + "w_gate", [NEXP, D, 512]), din(pre + "w_up", [NEXP, D, 512]),
                     din(pre + "w_down", [NEXP, 512, D]), din(pre + "ln_g", [D]), din(pre + "ln_b", [D]), xslots, yslots)
        x_cur = x_next
    outer.close()
    return nc


def phase_inputs(pi, kind, layer, inp):
    pre = "p%d_" % pi
    i = layer // 2
    c = np.ascontiguousarray
    if kind == "even":
        return {pre + "w_in": c(inp["ab_w_in"][i]), pre + "b_forget": c(inp["ab_b_forget"][i]), pre + "conv_w": c(inp["ab_conv_w"][i]),
                pre + "w_out": c(inp["ab_w_out"][i]), pre + "ln_g": c(inp["ln_mix_g"][layer]), pre + "ln_b": c(inp["ln_mix_b"][layer])}
    if kind == "odd":
        return {pre + "w_in": c(inp["c_w_in"][i]), pre + "lam_q1": c(inp["c_lam_q1"][i]), pre + "lam_k1": c(inp["c_lam_k1"][i]),
                pre + "lam_q2": c(inp["c_lam_q2"][i]), pre + "lam_k2": c(inp["c_lam_k2"][i]), pre + "subln_g": c(inp["c_subln_g"][i]),
                pre + "w_out": c(inp["c_w_out"][i]), pre + "ln_g": c(inp["ln_mix_g"][layer]), pre + "ln_b": c(inp["ln_mix_b"][layer])}
    return {pre + "w_group": c(inp["moe_w_group"][layer]), pre + "b_group": c(inp["moe_b_group"][layer]),
            pre + "w_expert": c(inp["moe_w_expert"][layer]), pre + "b_expert": c(inp["moe_b_expert"][layer]),
            pre + "w_gate": c(inp["moe_w_gate"][layer]), pre + "w_up": c(inp["moe_w_up"][layer]), pre + "w_down": c(inp["moe_w_down"][layer]),
            pre + "ln_g": c(inp["ln_ffn_g"][layer]), pre + "ln_b": c(inp["ln_ffn_b"][layer])}


ALL_PHASES = []
for _l in range(DEPTH):
    ALL_PHASES.append(("even" if _l % 2 == 0 else "odd", _l))
    ALL_PHASES.append(("moe", _l))

_PROG_CACHE = {}


def run_phases(phases, x_shards, inp, core_ids=None, trace=False):
    key = tuple((k, (l if k == "odd" else 0)) for k, l in phases)
    if key not in _PROG_CACHE:
        _PROG_CACHE[key] = build_program(phases)
    nc = _PROG_CACHE[key]
    consts = {"c_" + k: v for k, v in host_consts().items()}
    shared = dict(consts)
    for pi, (kind, layer) in enumerate(phases):
        shared.update(phase_inputs(pi, kind, layer, inp))
    in_maps = []
    for xs in x_shards:
        m = dict(shared)
        m["x"] = np.ascontiguousarray(xs)
        in_maps.append(m)
    core_ids = core_ids if core_ids is not None else list(range(len(x_shards)))
    if trace:
        res = run_bass_kernel_spmd(nc, in_maps, core_ids=core_ids, trace=True)
        print("TRACE exec_time_ns", res.exec_time_ns)
    else:
        res = run_bass_kernel_spmd(nc, in_maps, core_ids=core_ids)
    return [r["y"] for r in res.results]


def kernel(**inputs):
    inp = {k: np.asarray(v) for k, v in inputs.items()}
    x = inp["x"].astype(np.float32, copy=False)
    shards = [x[NSEQ * c:NSEQ * (c + 1)].reshape(NTOK, D) for c in range(N_CORES)]
    if MODE == "fused":
        outs = run_phases(ALL_PHASES, shards, inp)
    else:
        outs = shards
        for ph in ALL_PHASES:
            outs = run_phases([ph], outs, inp)
    out = np.stack([o.reshape(NSEQ, SEQ, D) for o in outs], axis=0).reshape(N_CORES * NSEQ, SEQ, D)
    return out.astype(np.float32, copy=False)
```

```python
import math
from contextlib import ExitStack

import ml_dtypes
import numpy as np

import concourse.bass as bass
import concourse.mybir as mybir
from concourse.bass_utils import run_bass_kernel_spmd

F32 = mybir.dt.float32
BF16 = mybir.dt.bfloat16
I32 = mybir.dt.int32
AF = mybir.ActivationFunctionType
ALU = mybir.AluOpType
AX = mybir.AxisListType

N_CORES = 8
D = 1024
SEQ = 2048
NSEQ = 2
NTOK = NSEQ * SEQ
NT = NTOK // 128
TPS = SEQ // 128
DEPTH = 4
ALPHA = (2.0 * DEPTH) ** 0.25
LN_EPS = 1e-5
RMS_EPS = 1e-5
EVEN_IN = 3080
NEXP = 32
CAP = 512
STRIDE = CAP + 1
NSLOT = NEXP * STRIDE
MODE = "fused"


class Sched:
    COMPUTE = ("pe", "act", "dve", "pool")
    QUEUES = ("sp", "act", "pool")

    def __init__(self, nc, stack, same_engine_sync=True):
        self.nc = nc
        self.same_engine_sync = same_engine_sync
        self.engnames = ("pe", "act", "dve", "pool", "sp")
        self.streams = {e: [] for e in self.engnames}
        self.count = {e: 0 for e in self.COMPUTE}
        self.sem = {e: stack.enter_context(nc.semaphore("sem_" + e)) for e in self.COMPUTE}
        self.spare_sems = [{e: stack.enter_context(nc.semaphore("sem%d_%s" % (i, e))) for e in self.COMPUTE} for i in range(1)]
        nslots = {"sp": 16, "act": 1, "pool": 16}
        self.slots = {}
        for q in self.QUEUES:
            self.slots[q] = [
                {"sem": stack.enter_context(nc.semaphore("dsem_%s_%d" % (q, i))), "total": 0, "id": (q, i)}
                for i in range(nslots[q])
            ]
        self.slot_rr = {q: 0 for q in self.QUEUES}
        self.slot_by_id = {s["id"]: s for q in self.QUEUES for s in self.slots[q]}
        self.known = {e: {} for e in self.engnames}
        self.last_write = {}
        self.readers = {}

    def _deps(self, reads, writes):
        deps = []
        for k in reads:
            ev = self.last_write.get(k)
            if ev is not None:
                deps.append(ev)
        for k in writes:
            ev = self.last_write.get(k)
            if ev is not None:
                deps.append(ev)
            deps.extend(self.readers.get(k, ()))
        return deps

    def _commit(self, ev, reads, writes):
        for k in reads:
            self.readers.setdefault(k, []).append(ev)
        for k in writes:
            self.last_write[k] = ev
            self.readers[k] = []

    def _wait_list(self, eng, deps):
        need = {}
        for kind, key, val in deps:
            if kind == "e" and key == eng and (eng == "pe" or not self.same_engine_sync):
                continue
            sk = (kind, key)
            if self.known[eng].get(sk, 0) >= val:
                continue
            if need.get(sk, 0) < val:
                need[sk] = val
        out = []
        for sk, val in need.items():
            self.known[eng][sk] = val
            sem = self.sem[sk[1]] if sk[0] == "e" else self.slot_by_id[sk[1]]["sem"]
            out.append((sem, val))
        return out

    def op(self, eng, fn, reads=(), writes=()):
        waits = self._wait_list(eng, self._deps(reads, writes))
        self.count[eng] += 1
        ev = ("e", eng, self.count[eng])
        sem = self.sem[eng]

        def emit(e, fn=fn, waits=waits, sem=sem):
            for s, v in waits:
                e.wait_ge(s, v)
            fn(e).then_inc(sem, 1)

        self.streams[eng].append(emit)
        self._commit(ev, reads, writes)
        return ev

    def dma(self, queue, fn, reads=(), writes=()):
        slots = self.slots[queue]
        slot = slots[self.slot_rr[queue] % len(slots)]
        self.slot_rr[queue] += 1
        deps = self._deps(reads, writes)
        if slot["total"] > 0:
            deps.append(("d", slot["id"], slot["total"]))
        waits = self._wait_list(queue, deps)
        slot["total"] += 16
        ev = ("d", slot["id"], slot["total"])
        sem = slot["sem"]

        def emit(e, fn=fn, waits=waits, sem=sem):
            for s, v in waits:
                e.wait_ge(s, v)
            fn(e).then_inc(sem, 16)

        self.streams[queue].append(emit)
        self._commit(ev, reads, writes)
        return ev

    def drain(self):
        waits = []
        for q in self.QUEUES:
            for s in self.slots[q]:
                if s["total"] > 0:
                    waits.append((s["sem"], s["total"]))
                    for e in self.engnames:
                        self.known[e][("d", s["id"])] = s["total"]
        for c in self.COMPUTE:
            if self.count[c] > 0:
                waits.append((self.sem[c], self.count[c]))
                for e in self.engnames:
                    self.known[e][("e", c)] = self.count[c]

        def emit(e, waits=waits):
            for s, v in waits:
                e.wait_ge(s, v)

        self.streams["sp"].append(emit)
        self.last_write = {}
        self.readers = {}

    def rotate_engine_sems(self):
        if not self.spare_sems:
            return
        self.sem = self.spare_sems.pop(0)
        for c in self.COMPUTE:
            self.count[c] = 0
            for e in self.engnames:
                self.known[e].pop(("e", c), None)

    def reset_engine_sems(self):
        nc = self.nc
        sem = self.sem
        with nc.Block() as block:
            @block.tensor
            def _(e):
                e.sem_clear(sem["pe"])

            @block.scalar
            def _(e):
                e.sem_clear(sem["act"])

            @block.vector
            def _(e):
                e.sem_clear(sem["dve"])

            @block.gpsimd
            def _(e):
                e.sem_clear(sem["pool"])
        for c in self.COMPUTE:
            self.count[c] = 0
            for e in self.engnames:
                self.known[e].pop(("e", c), None)

    def emit_block(self):
        nc = self.nc
        streams = self.streams
        with nc.Block() as block:
            @block.tensor
            def _(e):
                for f in streams["pe"]:
                    f(e)

            @block.scalar
            def _(e):
                for f in streams["act"]:
                    f(e)

            @block.vector
            def _(e):
                for f in streams["dve"]:
                    f(e)

            @block.gpsimd
            def _(e):
                for f in streams["pool"]:
                    f(e)

            @block.sync
            def _(e):
                for f in streams["sp"]:
                    f(e)
        self.streams = {e: [] for e in self.engnames}


def host_consts():
    bf = ml_dtypes.bfloat16
    c = {}
    c["ident_f"] = np.eye(128, dtype=np.float32)
    c["ident_b"] = np.eye(128, dtype=np.float32).astype(bf)
    k = np.arange(128)[:, None]
    q = np.arange(128)[None, :]
    c["tri"] = (k <= q).astype(np.float32).astype(bf)
    c["ones_b"] = np.ones((128, 128), np.float32).astype(bf)
    m12 = np.zeros((32, 2), np.float32)
    m12[0:8, 0] = -1.0
    m12[8:16, 1] = -1.0
    m12[16:24, 0] = 1.0
    m12[24:32, 1] = 1.0
    c["m12"] = m12
    slopes = 2.0 ** (-8.0 * np.arange(1, 9) / 8.0)
    pos = np.arange(SEQ)
    a = (pos // 64).astype(np.float64)
    b = (pos % 64).astype(np.float64)
    augq = np.zeros((8, 4, SEQ), np.float64)
    augk = np.zeros((8, 4, SEQ), np.float64)
    corr = np.zeros((128, 8, 128), np.float64)
    for h in range(8):
        s = slopes[h]
        augq[h, 0] = -8.0 * s * 64.0 * a
        augq[h, 1] = -8.0 * s * b
        augq[h, 2] = 1.0
        augq[h, 3] = 1.0
        augk[h, 0] = 1.0
        augk[h, 1] = 1.0
        augk[h, 2] = 8.0 * s * 64.0 * a
        augk[h, 3] = 8.0 * s * b
        vis = (k // 64) <= (q // 64)
        cc = np.where(k > q, np.exp(-2.0 * s * (k - q)), 1.0)
        corr[:, h, :] = np.where(vis, cc, 0.0)
    c["augq"] = augq.astype(np.float32).astype(bf)
    c["augk"] = augk.astype(np.float32).astype(bf)
    c["corr"] = corr.astype(np.float32).astype(bf)
    c["basem1"] = np.broadcast_to((np.arange(NEXP) * STRIDE - 1).astype(np.float32)[None, :], (128, NEXP)).copy()
    return c


CONST_SPECS = {
    "ident_f": ([128, 128], F32), "ident_b": ([128, 128], BF16), "tri": ([128, 128], BF16),
    "ones_b": ([128, 128], BF16), "m12": ([32, 2], F32), "augq": ([8, 4, SEQ], BF16),
    "augk": ([8, 4, SEQ], BF16), "corr": ([128, 8, 128], BF16), "basem1": ([128, NEXP], F32),
}


class Ctx:
    pass


def new_phase(C, n_f32=7, n_tr=1):
    C.st = ExitStack()
    nc = C.nc
    C.phase_no = getattr(C, "phase_no", -1) + 1
    pfx = "f%d_" % C.phase_no
    C.sb = lambda name, shape, dt: C.st.enter_context(nc.sbuf_tensor(pfx + name, shape, dt))
    C.banks = [C.st.enter_context(nc.psum_tensor(pfx + "bank%d" % i, [128, 512], F32)) for i in range(n_f32)]
    C.pTrs = [C.st.enter_context(nc.psum_tensor(pfx + "pTr%d" % i, [128, 1024], BF16)) for i in range(n_tr)]
    C.pTr = C.pTrs[0]


def end_phase(C):
    C.S.drain()
    C.S.emit_block()
    if C.phase_no == 3:
        C.S.rotate_engine_sems()
    C.st.close()


def load_const(C, name, tile_ap, key):
    C.S.dma("sp", lambda e: e.dma_start(out=tile_ap, in_=C.consts[name]), writes=[key])


def cast_load_w(C, dst, src, nk, ncols, key):
    S = C.S
    v = src.rearrange("(k p) n -> p k n", p=128)
    step = 1024 if ncols % 1024 == 0 else (1540 if ncols == 3080 else ncols)
    keys = []
    for k in range(nk):
        for c0 in range(0, ncols, step):
            c1 = min(ncols, c0 + step)
            kk = (key, k, c0)
            keys.append(kk)
            S.dma("pool", lambda e, k=k, c0=c0, c1=c1: e.dma_start(out=dst[:, k, c0:c1], in_=v[:, k, c0:c1]),
                  writes=[kk])
    return keys


def ln_stats(C, yt, ykey, li):
    S = C.S
    L = C.ln
    st, mv, rstd, nmr = L["stats"][:, li, :], L["mv"][:, li, :], L["rstd"][:, li:li + 1], L["nmr"][:, li:li + 1]
    k = lambda n: (n, li)
    S.op("dve", lambda e: e.bn_stats(out=st[:, 0:6], in_=yt[:, 0:512]), reads=[ykey], writes=[k("ln_stats")])
    S.op("dve", lambda e: e.bn_stats(out=st[:, 6:12], in_=yt[:, 512:1024]), reads=[ykey], writes=[k("ln_stats")])
    S.op("dve", lambda e: e.bn_aggr(out=mv, in_=st), reads=[k("ln_stats")], writes=[k("ln_mv")])
    S.op("act", lambda e: e.activation(out=rstd, in_=mv[:, 1:2], func=AF.Ln, bias=L["eps"][:], scale=1.0),
         reads=[k("ln_mv"), "ln_eps"], writes=[k("ln_rstd")])
    S.op("act", lambda e: e.activation(out=rstd, in_=rstd, func=AF.Exp, scale=-0.5), reads=[k("ln_rstd")], writes=[k("ln_rstd")])
    S.op("dve", lambda e: e.tensor_scalar(out=nmr, in0=mv[:, 0:1], scalar1=-1.0, scalar2=rstd, op0=ALU.mult, op1=ALU.mult),
         reads=[k("ln_mv"), k("ln_rstd")], writes=[k("ln_nmr")])


def ln_finish(C, yt, gbc, bbc, out_ap, ykey, tag, li):
    S = C.S
    L = C.ln
    rstd, nmr = L["rstd"][:, li:li + 1], L["nmr"][:, li:li + 1]
    k = lambda n: (n, li)
    S.op("act", lambda e: e.activation(out=yt[:], in_=yt[:], func=AF.Identity, bias=nmr, scale=rstd),
         reads=[ykey, k("ln_nmr"), k("ln_rstd")], writes=[ykey])
    S.op("dve", lambda e: e.tensor_tensor(out=yt[:], in0=yt[:], in1=gbc[:], op=ALU.mult), reads=[ykey, "gbc"], writes=[ykey])
    S.op("dve", lambda e: e.tensor_tensor(out=yt[:], in0=yt[:], in1=bbc[:], op=ALU.add), reads=[ykey, "bbc"], writes=[ykey])
    S.dma("sp", lambda e: e.dma_start(out=out_ap, in_=yt[:]), reads=[ykey], writes=[("xout", tag)])


def layer_norm_store(C, yt, gbc, bbc, out_ap, ykey, tag, li=0):
    ln_stats(C, yt, ykey, li)
    ln_finish(C, yt, gbc, bbc, out_ap, ykey, tag, li)


def alloc_ln(C):
    sb = C.sb
    C.ln = {"stats": sb("ln_stats", [128, 4, 12], F32), "mv": sb("ln_mv", [128, 4, 2], F32),
            "rstd": sb("ln_rstd", [128, 4], F32), "nmr": sb("ln_nmr", [128, 4], F32),
            "eps": sb("ln_eps", [128, 1], F32)}
    C.S.op("dve", lambda e: e.memset(C.ln["eps"][:], LN_EPS), writes=["ln_eps"])


def build_xT(C, x_in, s, xT, ident_f, xs):
    S = C.S
    for t in range(TPS):
        tt = s * TPS + t
        xb = xs[t % 2]
        xkey = ("xs", t % 2)
        S.dma("sp", lambda e, tt=tt, xb=xb: e.dma_start(out=xb[:], in_=x_in[tt * 128:(tt + 1) * 128, :]), writes=[xkey])
        for half in range(2):
            bk = C.banks[5 + half]
            bkey = ("bank", 5 + half)
            for j in range(4):
                k = half * 4 + j
                S.op("pe", lambda e, bk=bk, j=j, k=k, xb=xb: e.transpose(out=bk[:, j * 128:(j + 1) * 128],
                                                                         in_=xb[:, k * 128:(k + 1) * 128], identity=ident_f[:]),
                     reads=[xkey, "ident_f"], writes=[bkey])
            eng = "dve" if half == 0 else "act"
            if eng == "dve":
                S.op("dve", lambda e, bk=bk, half=half, t=t: e.tensor_copy(
                    out=xT[:, half * 4:half * 4 + 4, t * 128:(t + 1) * 128],
                    in_=bk[:, :].rearrange("p (j c) -> p j c", j=4)), reads=[bkey], writes=["xT"])
            else:
                S.op("act", lambda e, bk=bk, half=half, t=t: e.copy(
                    out=xT[:, half * 4:half * 4 + 4, t * 128:(t + 1) * 128],
                    in_=bk[:, :].rearrange("p (j c) -> p j c", j=4)), reads=[bkey], writes=["xT"])


def outproj_ln(C, x_in, x_out, s, catT, Wout, wout_keys, gbc, bbc, xs, yts):
    S = C.S
    pend = None
    for t in range(TPS):
        tt = s * TPS + t
        xb = xs[t % 2]
        xkey = ("xs", t % 2)
        yt = yts[t % len(yts)]
        ykey = ("yt", t % len(yts))
        S.dma("sp", lambda e, tt=tt, xb=xb: e.dma_start(out=xb[:], in_=x_in[tt * 128:(tt + 1) * 128, :]), writes=[xkey])
        for half in range(2):
            bk = C.banks[5 + half]
            bkey = ("bank", 5 + half)
            for k in range(8):
                S.op("pe", lambda e, bk=bk, k=k, t=t, half=half: e.matmul(
                    bk[:, :], lhsT=catT[:, k, t * 128:(t + 1) * 128], rhs=Wout[:, k, half * 512:(half + 1) * 512],
                    start=(k == 0), stop=(k == 7)), reads=["catT"] + wout_keys, writes=[bkey])
            S.op("dve", lambda e, bk=bk, half=half, xb=xb, yt=yt: e.scalar_tensor_tensor(
                out=yt[:, half * 512:(half + 1) * 512], in0=xb[:, half * 512:(half + 1) * 512], scalar=ALPHA,
                in1=bk[:, :], op0=ALU.mult, op1=ALU.add), reads=[xkey, bkey], writes=[ykey])
        if len(yts) < 2:
            layer_norm_store(C, yt, gbc, bbc, x_out[tt * 128:(tt + 1) * 128, :], ykey, tt)
        else:
            ln_stats(C, yt, ykey, t % 2)
            if pend is not None:
                ln_finish(C, *pend)
            pend = (yt, gbc, bbc, x_out[tt * 128:(tt + 1) * 128, :], ykey, tt, t % 2)
    if pend is not None:
        ln_finish(C, *pend)


def emit_even(C, x_in, x_out, w_in, b_forget, conv_w, w_out, ln_g, ln_b):
    new_phase(C)
    S, sb, nc = C.S, C.sb, C.nc
    B = C.banks
    Win = sb("Win", [128, 8, EVEN_IN], BF16)
    Wout = sb("Wout", [128, 8, D], BF16)
    Wf = sb("Wf", [128, 8, 32], BF16)
    cw = sb("cw", [128, 4, 3], F32)
    bf32 = sb("bf32", [32, 1], F32)
    gbc = sb("gbc", [128, D], F32)
    bbc = sb("bbc", [128, D], F32)
    ident_f = sb("ident_f", [128, 128], F32)
    ident_b = sb("ident_b", [128, 128], BF16)
    tri = sb("tri", [128, 128], BF16)
    m12 = sb("m12", [32, 2], F32)
    xT = sb("xT", [128, 8, SEQ], BF16)
    catT = sb("catT", [128, 8, SEQ], BF16)
    qa = [sb("qa%d" % i, [68, SEQ], BF16) for i in range(2)]
    ka = [sb("ka%d" % i, [68, SEQ], BF16) for i in range(2)]
    Vh = [sb("Vh%d" % i, [128, TPS, 65], BF16) for i in range(2)]
    aug32 = sb("aug32", [32, SEQ], BF16)
    fs = [sb("fs%d" % i, [32, 512], F32) for i in range(3)]
    fh = sb("fh", [32, 512], BF16)
    pt = [sb("pt%d" % i, [128, 512], BF16) for i in range(4)]
    u = sb("u", [128, SEQ + 2], F32)
    ytmp = sb("ytmp", [128, 512], F32)
    Csb = sb("Csb", [128, 512], F32)
    opair = sb("opair", [128, TPS, 128], BF16)
    rec = sb("rec", [128, 4], F32)
    xs = [sb("xs%d" % i, [128, D], F32) for i in range(2)]
    yts = [sb("yt%d" % i, [128, D], F32) for i in range(2)]
    carry = sb("carry", [32, 1], F32)
    alloc_ln(C)

    win_keys = cast_load_w(C, Win, w_in, 8, EVEN_IN, "Win")
    wout_keys = cast_load_w(C, Wout, w_out, 8, D, "Wout")
    load_const(C, "ident_f", ident_f[:], "ident_f")
    load_const(C, "ident_b", ident_b[:], "ident_b")
    load_const(C, "tri", tri[:], "tri")
    load_const(C, "m12", m12[:], "m12")
    S.dma("sp", lambda e: e.dma_start(out=cw[:], in_=conv_w.rearrange("(c p) j -> p c j", p=128)), writes=["cw"])
    for r in range(4):
        S.dma("sp", lambda e, r=r: e.dma_start(out=bf32[r * 8:(r + 1) * 8, :], in_=b_forget.rearrange("(h o) -> h o", o=1)),
              writes=[("bf32", r)])
    S.dma("sp", lambda e: e.dma_start(out=gbc[:], in_=ln_g.partition_broadcast(128)), writes=["gbc"])
    S.dma("sp", lambda e: e.dma_start(out=bbc[:], in_=ln_b.partition_broadcast(128)), writes=["bbc"])
    S.op("dve", lambda e: e.tensor_scalar(out=bf32[:], in0=bf32[:], scalar1=-1.0, scalar2=None, op0=ALU.mult),
         reads=[("bf32", r) for r in range(4)], writes=["nbf"])
    for r in range(4):
        S.op("dve", lambda e, r=r: e.tensor_copy(out=Wf[:, :, r * 8:(r + 1) * 8], in_=Win[:, :, 3072:3080]),
             reads=win_keys, writes=["Wf"])
    S.op("dve", lambda e: e.memset(u[:, 0:2], 0.0), writes=["u"])
    for i in range(2):
        S.op("dve", lambda e, i=i: e.memset(Vh[i][:], 1.0), writes=[("Vh", i)])
        S.op("dve", lambda e, i=i: e.memset(qa[i][64:68, :], 1.0), writes=[("qa_aug", i)])
        S.op("dve", lambda e, i=i: e.memset(ka[i][64:68, :], 1.0), writes=[("ka_aug", i)])

    OFF_Q, OFF_K, OFF_V = 1536, 2048, 2560
    ucnt = [0]
    if C.debug:
        print("even phase sbuf bytes remaining", nc.sbuf_bytes_remaining)
    cnt = [0]

    for s in range(NSEQ):
        build_xT(C, x_in, s, xT, ident_f, xs)

        for r in range(4):
            bk = B[r % 2]
            bkey = ("bank", r % 2)
            for k in range(8):
                S.op("pe", lambda e, bk=bk, k=k, r=r: e.matmul(bk[0:32, :], lhsT=Wf[:, k, :], rhs=xT[:, k, r * 512:(r + 1) * 512],
                                                              start=(k == 0), stop=(k == 7)), reads=["Wf", "xT"], writes=[bkey])
            S.op("act", lambda e, bk=bk: e.activation(out=fs[0][:], in_=bk[0:32, :], func=AF.Exp, bias=bf32[:], scale=-1.0),
                 reads=[bkey, "nbf"], writes=["fs0"])
            S.op("act", lambda e: e.activation(out=fs[0][:], in_=fs[0][:], func=AF.Ln, bias=1.0, scale=1.0),
                 reads=["fs0"], writes=["fs0"])
            S.op("dve", lambda e: e.tensor_scalar(out=fs[0][:], in0=fs[0][:], scalar1=8.0, scalar2=None, op0=ALU.mult),
                 reads=["fs0"], writes=["fs0"])
            if r == 0:
                S.op("dve", lambda e: e.memset(fs[2][:], 1.0), writes=["fs2"])
                S.op("dve", lambda e: e.memset(carry[:], 0.0), writes=["carry"])
            else:
                S.op("dve", lambda e: e.tensor_copy(out=carry[:], in_=fs[1][:, 511:512]), reads=["fs1"], writes=["carry"])
            S.op("dve", lambda e: e.tensor_tensor_scan(out=fs[1][:], data0=fs[2][:], data1=fs[0][:], initial=carry[:],
                                                       op0=ALU.mult, op1=ALU.add), reads=["fs0", "fs2", "carry"], writes=["fs1"])
            S.op("dve", lambda e: e.tensor_copy(out=fh[:], in_=fs[1][:]), reads=["fs1"], writes=["fh"])
            S.op("dve", lambda e: e.tensor_tensor(out=fs[0][:], in0=fs[1][:], in1=fh[:], op=ALU.subtract),
                 reads=["fs1", "fh"], writes=["fs0"])
            S.op("dve", lambda e: e.tensor_scalar(out=fs[0][:], in0=fs[0][:], scalar1=m12[:, 1:2], scalar2=None, op0=ALU.mult),
                 reads=["fs0", "m12"], writes=["fs0"])
            S.op("dve", lambda e, r=r: e.scalar_tensor_tensor(out=aug32[:, r * 512:(r + 1) * 512], in0=fh[:], scalar=m12[:, 0:1],
                                                              in1=fs[0][:], op0=ALU.mult, op1=ALU.add),
                 reads=["fh", "fs0", "m12"], writes=["aug32"])

        for c in range(4):
            for r in range(4):
                pb, pc, px = B[0 + (r % 2)], B[2 + (r % 2)], B[5 + (r % 2)]
                kb, kc, kx = ("bank", r % 2), ("bank", 2 + r % 2), ("bank", 5 + r % 2)
                for (bk, bkey, col0) in ((pb, kb, 0), (pc, kc, 512), (px, kx, 1024)):
                    for k in range(8):
                        S.op("pe", lambda e, bk=bk, k=k, col0=col0, c=c, r=r: e.matmul(
                            bk[:, :], lhsT=Win[:, k, col0 + c * 128:col0 + (c + 1) * 128], rhs=xT[:, k, r * 512:(r + 1) * 512],
                            start=(k == 0), stop=(k == 7)), reads=win_keys + ["xT"], writes=[bkey])
                S.op("act", lambda e, pc=pc: e.copy(out=Csb[:], in_=pc[:, :]), reads=[kc], writes=["Csb"])
                S.op("dve", lambda e, px=px, r=r: e.tensor_tensor(out=u[:, 2 + r * 512:2 + (r + 1) * 512], in0=px[:, :], in1=Csb[:],
                                                                 op=ALU.mult), reads=[kx, "Csb"], writes=["u"])
                S.op("dve", lambda e, c=c, r=r: e.tensor_scalar(out=ytmp[:], in0=u[:, 2 + r * 512:2 + (r + 1) * 512],
                                                                scalar1=cw[:, c, 2:3], scalar2=None, op0=ALU.mult),
                     reads=["u", "cw"], writes=["ytmp"])
                S.op("dve", lambda e, c=c, r=r: e.scalar_tensor_tensor(out=ytmp[:], in0=u[:, 1 + r * 512:1 + (r + 1) * 512],
                                                                       scalar=cw[:, c, 1:2], in1=ytmp[:], op0=ALU.mult, op1=ALU.add),
                     reads=["u", "cw", "ytmp"], writes=["ytmp"])
                S.op("dve", lambda e, c=c, r=r: e.scalar_tensor_tensor(out=ytmp[:], in0=u[:, r * 512:(r + 1) * 512],
                                                                       scalar=cw[:, c, 0:1], in1=ytmp[:], op0=ALU.mult, op1=ALU.add),
                     reads=["u", "cw", "ytmp"], writes=["ytmp"])
                S.op("dve", lambda e, pb=pb, c=c, r=r: e.tensor_tensor(out=catT[:, c, r * 512:(r + 1) * 512], in0=pb[:, :], in1=ytmp[:],
                                                                      op=ALU.mult), reads=[kb, "ytmp"], writes=["catT"])

        def inproj_units(h):
            par = h % 2
            units = []

            def aug():
                S.dma("sp", lambda e: e.dma_start(out=qa[par][64:65, :], in_=aug32[h:h + 1, :]), reads=["aug32"], writes=[("qa_aug", par)])
                S.dma("sp", lambda e: e.dma_start(out=qa[par][65:66, :], in_=aug32[8 + h:9 + h, :]), reads=["aug32"], writes=[("qa_aug2", par)])
                S.dma("sp", lambda e: e.dma_start(out=ka[par][66:67, :], in_=aug32[16 + h:17 + h, :]), reads=["aug32"], writes=[("ka_aug", par)])
                S.dma("sp", lambda e: e.dma_start(out=ka[par][67:68, :], in_=aug32[24 + h:25 + h, :]), reads=["aug32"], writes=[("ka_aug2", par)])

            def qk_unit(dst, dkey, off, r, first):
                def f():
                    if first:
                        aug()
                    bi = ucnt[0] % 2
                    ucnt[0] += 1
                    bk, bkey = B[bi], ("bank", bi)
                    for k in range(8):
                        S.op("pe", lambda e, k=k: e.matmul(
                            bk[0:64, :], lhsT=Win[:, k, off + h * 64:off + (h + 1) * 64], rhs=xT[:, k, r * 512:(r + 1) * 512],
                            start=(k == 0), stop=(k == 7)), reads=win_keys + ["xT"], writes=[bkey])
                    S.op("dve", lambda e: e.tensor_copy(out=dst[0:64, r * 512:(r + 1) * 512], in_=bk[0:64, :]),
                         reads=[bkey], writes=[dkey])
                return f

            def v_unit(g):
                def f():
                    bi = ucnt[0] % 2
                    ucnt[0] += 1
                    bk, bkey = B[bi], ("bank", bi)
                    for tl in range(8):
                        t = g * 8 + tl
                        for k in range(8):
                            S.op("pe", lambda e, k=k, t=t, tl=tl: e.matmul(
                                bk[:, tl * 64:(tl + 1) * 64], lhsT=xT[:, k, t * 128:(t + 1) * 128],
                                rhs=Win[:, k, OFF_V + h * 64:OFF_V + (h + 1) * 64], start=(k == 0), stop=(k == 7)),
                                reads=win_keys + ["xT"], writes=[bkey])
                    S.op("dve", lambda e: e.tensor_copy(out=Vh[par][:, g * 8:(g + 1) * 8, 0:64],
                                                        in_=bk[:, :].rearrange("p (t c) -> p t c", t=8)),
                         reads=[bkey], writes=[("Vh", par)])
                return f

            first = True
            for (dst, dkey, off) in ((qa[par], ("qa", par), OFF_Q), (ka[par], ("ka", par), OFF_K)):
                for r in range(4):
                    units.append(qk_unit(dst, dkey, off, r, first))
                    first = False
            for g in range(2):
                units.append(v_unit(g))
            return units

        def attn(h, fill):
            par = h % 2
            qk_reads = [("qa", par), ("ka", par), ("qa_aug", par), ("qa_aug2", par), ("ka_aug", par), ("ka_aug2", par)]
            items = [(qb, j) for qb in range(4) for j in range(4 * qb + 4)]
            info = {}

            def emit_score(idx):
                qb, j = items[idx]
                d = j - 4 * qb
                n0 = max(0, d) * 128
                si = cnt[0] % 3
                pi = cnt[0] % 4
                cnt[0] += 1
                Sb, skey = B[4 + si], ("bank", 4 + si)
                P, pkey = pt[pi], ("pt", pi)
                info[idx] = (Sb, skey, P, pkey, d, n0)
                S.op("pe", lambda e: e.matmul(
                    Sb[:, n0:512], lhsT=ka[par][0:68, j * 128:(j + 1) * 128], rhs=qa[par][0:68, qb * 512 + n0:(qb + 1) * 512],
                    start=True, stop=True), reads=qk_reads, writes=[skey])

            def emit_rest(idx):
                qb, j = items[idx]
                Sb, skey, P, pkey, d, n0 = info.pop(idx)
                O = B[2 + (qb % 2)]
                okey = ("bank", 2 + qb % 2)
                S.op("act", lambda e: e.activation(out=P[:, n0:512], in_=Sb[:, n0:512], func=AF.Exp, scale=0.125),
                     reads=[skey], writes=[pkey])
                if d >= 0:
                    S.op("dve", lambda e: e.tensor_tensor(out=P[:, n0:n0 + 128], in0=P[:, n0:n0 + 128], in1=tri[:],
                                                          op=ALU.mult), reads=[pkey, "tri"], writes=[pkey])
                for i in range(max(0, d), 4):
                    S.op("pe", lambda e, i=i: e.matmul(
                        O[:, i * 65:(i + 1) * 65], lhsT=P[:, i * 128:(i + 1) * 128], rhs=Vh[par][:, j, :],
                        start=(j == 0), stop=(j == 4 * qb + i)), reads=[pkey, ("Vh", par)], writes=[okey])
                if j != 4 * qb + 3:
                    return
                S.op("dve", lambda e: e.reciprocal(out=rec[:, 0:4], in_=O[:, 0:260].rearrange("p (i c) -> p i c", i=4)[:, :, 64]),
                     reads=[okey], writes=["rec"])
                for i in range(4):
                    S.op("dve", lambda e, i=i: e.tensor_scalar(
                        out=opair[:, qb * 4 + i, (h % 2) * 64:(h % 2) * 64 + 64], in0=O[:, i * 65:i * 65 + 64],
                        scalar1=rec[:, i:i + 1], scalar2=None, op0=ALU.mult), reads=[okey, "rec"], writes=["opair"])

            stride = max(1, len(items) // (len(fill) + 1))
            emit_score(0)
            emit_score(1)
            for idx in range(len(items)):
                if idx + 2 < len(items):
                    emit_score(idx + 2)
                emit_rest(idx)
                if fill and idx % stride == stride - 1:
                    fill.pop(0)()
            while fill:
                fill.pop(0)()

        def pair_transposes(h):
            for g in range(2):
                for i in range(8):
                    S.op("pe", lambda e, i=i, g=g: e.transpose(out=C.pTr[:, i * 128:(i + 1) * 128], in_=opair[:, g * 8 + i, :],
                                                               identity=ident_b[:]), reads=["opair", "ident_b"], writes=["pTr"])
                S.op("act", lambda e, g=g: e.copy(out=catT[:, 4 + h // 2, g * 1024:(g + 1) * 1024], in_=C.pTr[:, :]),
                     reads=["pTr"], writes=["catT"])

        for u_ in inproj_units(0):
            u_()
        for h in range(8):
            attn(h, inproj_units(h + 1) if h + 1 < 8 else [])
            if h % 2 == 1:
                pair_transposes(h)

        outproj_ln(C, x_in, x_out, s, catT, Wout, wout_keys, gbc, bbc, xs, yts)
    end_phase(C)


def emit_odd(C, layer, x_in, x_out, w_in, lam_q1, lam_k1, lam_q2, lam_k2, subln_g, w_out, ln_g, ln_b):
    new_phase(C)
    S, sb, nc = C.S, C.sb, C.nc
    B = C.banks
    lam_init = 0.8 - 0.6 * math.exp(-0.3 * layer)
    Win = sb("Win", [128, 8, 3 * D], BF16)
    Wout = sb("Wout", [128, 8, D], BF16)
    gbc = sb("gbc", [128, D], F32)
    bbc = sb("bbc", [128, D], F32)
    ident_f = sb("ident_f", [128, 128], F32)
    ident_b = sb("ident_b", [128, 128], BF16)
    corr = sb("corr", [128, 8, 128], BF16)
    xT = sb("xT", [128, 8, SEQ], BF16)
    catT = sb("catT", [128, 8, SEQ], BF16)
    NQB = 2
    q1a = [sb("q1a%d" % i, [68, SEQ], BF16) for i in range(NQB)]
    q2a = [sb("q2a%d" % i, [68, SEQ], BF16) for i in range(NQB)]
    k1a = [sb("k1a%d" % i, [68, SEQ], BF16) for i in range(NQB)]
    k2a = [sb("k2a%d" % i, [68, SEQ], BF16) for i in range(NQB)]
    Vh = [sb("Vh%d" % i, [128, TPS, 129], BF16) for i in range(2)]
    pt = [sb("pt%d" % i, [128, 512], BF16) for i in range(4)]
    lamv = [sb("lamv%d" % i, [128, 64], F32) for i in range(4)]
    lsc = sb("lsc", [128, 8], F32)
    gv = sb("gv", [128, 128], F32)
    ot = sb("ot", [128, 128], F32)
    junk = sb("junk", [128, 128], F32)
    ohead = sb("ohead", [128, TPS, 128], BF16)
    rr = sb("rr", [128, 8], F32)
    xs = [sb("xs%d" % i, [128, D], F32) for i in range(2)]
    yts = [sb("yt%d" % i, [128, D], F32) for i in range(2)]
    alloc_ln(C)

    win_keys = cast_load_w(C, Win, w_in, 8, 3 * D, "Win")
    wout_keys = cast_load_w(C, Wout, w_out, 8, D, "Wout")
    load_const(C, "ident_f", ident_f[:], "ident_f")
    load_const(C, "ident_b", ident_b[:], "ident_b")
    load_const(C, "corr", corr[:], "corr")
    S.dma("sp", lambda e: e.dma_start(out=gbc[:], in_=ln_g.partition_broadcast(128)), writes=["gbc"])
    S.dma("sp", lambda e: e.dma_start(out=bbc[:], in_=ln_b.partition_broadcast(128)), writes=["bbc"])
    for i, v in enumerate((lam_q1, lam_k1, lam_q2, lam_k2)):
        S.dma("sp", lambda e, i=i, v=v: e.dma_start(out=lamv[i][:], in_=v.partition_broadcast(128)), writes=[("lamv", i)])
    S.dma("sp", lambda e: e.dma_start(out=gv[:], in_=subln_g.partition_broadcast(128)), writes=["gv_raw"])
    S.op("dve", lambda e: e.scalar_tensor_tensor(out=junk[:, 0:64], in0=lamv[0][:], scalar=1.0, in1=lamv[1][:], op0=ALU.mult,
                                                 op1=ALU.mult, accum_out=lsc[:, 0:1]), reads=[("lamv", 0), ("lamv", 1)], writes=["junk", "lsc0"])
    S.op("dve", lambda e: e.scalar_tensor_tensor(out=junk[:, 0:64], in0=lamv[2][:], scalar=1.0, in1=lamv[3][:], op0=ALU.mult,
                                                 op1=ALU.mult, accum_out=lsc[:, 1:2]), reads=[("lamv", 2), ("lamv", 3), "junk"], writes=["junk", "lsc1"])
    S.op("act", lambda e: e.activation(out=lsc[:, 2:4], in_=lsc[:, 0:2], func=AF.Exp), reads=["lsc0", "lsc1"], writes=["lsc23"])
    S.op("dve", lambda e: e.tensor_tensor(out=lsc[:, 4:5], in0=lsc[:, 3:4], in1=lsc[:, 2:3], op=ALU.subtract), reads=["lsc23"], writes=["lsc4"])
    S.op("dve", lambda e: e.tensor_scalar(out=lsc[:, 4:5], in0=lsc[:, 4:5], scalar1=-lam_init, scalar2=None, op0=ALU.add),
         reads=["lsc4"], writes=["nlam"])
    S.op("dve", lambda e: e.memset(lsc[:, 5:6], RMS_EPS), writes=["rmseps"])
    S.op("dve", lambda e: e.memset(lsc[:, 6:7], -0.5), writes=["mhalf"])
    S.op("dve", lambda e: e.tensor_scalar(out=gv[:], in0=gv[:], scalar1=(1.0 - lam_init), scalar2=None, op0=ALU.mult),
         reads=["gv_raw"], writes=["gv"])
    for i in range(2):
        S.op("dve", lambda e, i=i: e.memset(Vh[i][:], 1.0), writes=[("Vh", i)])

    cnt = [0]
    if C.debug:
        print("odd phase sbuf bytes remaining", nc.sbuf_bytes_remaining)
    for s in range(NSEQ):
        build_xT(C, x_in, s, xT, ident_f, xs)

        def inproj_units(h):
            par = h % NQB
            vpar = h % 2
            units = []

            def aug():
                for (t_, nm) in ((q1a, "q1"), (q2a, "q2")):
                    S.dma("sp", lambda e, t_=t_: e.dma_start(out=t_[par][64:68, :], in_=C.consts["augq"][h]), writes=[(nm + "_aug", par)])
                for (t_, nm) in ((k1a, "k1"), (k2a, "k2")):
                    S.dma("sp", lambda e, t_=t_: e.dma_start(out=t_[par][64:68, :], in_=C.consts["augk"][h]), writes=[(nm + "_aug", par)])

            def qk_unit(d1, d2, n1, n2, off, r, first):
                def f():
                    if first:
                        aug()
                    bi = 4
                    bk, bkey = B[bi], ("bank", bi)
                    for k in range(8):
                        S.op("pe", lambda e, k=k: e.matmul(
                            bk[:, :], lhsT=Win[:, k, off + h * 128:off + (h + 1) * 128], rhs=xT[:, k, r * 512:(r + 1) * 512],
                            start=(k == 0), stop=(k == 7)), reads=win_keys + ["xT"], writes=[bkey])
                    S.op("dve", lambda e: e.tensor_copy(out=d1[0:64, r * 512:(r + 1) * 512], in_=bk[0:64, :]),
                         reads=[bkey], writes=[(n1, par)])
                    S.op("dve", lambda e: e.tensor_copy(out=d2[0:64, r * 512:(r + 1) * 512], in_=bk[64:128, :]),
                         reads=[bkey], writes=[(n2, par)])
                return f

            def v_unit(g):
                def f():
                    bi = 4
                    bk, bkey = B[bi], ("bank", bi)
                    for tl in range(4):
                        t = g * 4 + tl
                        for k in range(8):
                            S.op("pe", lambda e, k=k, t=t, tl=tl: e.matmul(
                                bk[:, tl * 128:(tl + 1) * 128], lhsT=xT[:, k, t * 128:(t + 1) * 128],
                                rhs=Win[:, k, 2 * D + h * 128:2 * D + (h + 1) * 128], start=(k == 0), stop=(k == 7)),
                                reads=win_keys + ["xT"], writes=[bkey])
                    S.op("dve", lambda e: e.tensor_copy(out=Vh[vpar][:, g * 4:(g + 1) * 4, 0:128],
                                                        in_=bk[:, :].rearrange("p (t c) -> p t c", t=4)),
                         reads=[bkey], writes=[("Vh", vpar)])
                return f

            first = True
            for (d1, d2, n1, n2, off) in ((q1a[par], q2a[par], "q1", "q2", 0), (k1a[par], k2a[par], "k1", "k2", D)):
                for r in range(4):
                    units.append(qk_unit(d1, d2, n1, n2, off, r, first))
                    first = False
            for g in range(4):
                units.append(v_unit(g))
            return units

        def attn(h, fill):
            par = h % NQB
            vpar = h % 2
            rd1 = [("q1", par), ("k1", par), ("q1_aug", par), ("k1_aug", par)]
            rd2 = [("q2", par), ("k2", par), ("q2_aug", par), ("k2_aug", par)]
            QB = 256
            items = [(qb, j) for qb in range(SEQ // QB) for j in range(2 * qb + 2)]
            info = {}

            def emit_score(idx):
                qb, j = items[idx]
                d = j - 2 * qb
                n0 = max(0, d) * 128
                si = cnt[0] % 2
                pi = cnt[0] % 4
                cnt[0] += 1
                Sb, skey = B[5 + si], ("bank", 5 + si)
                P, pkey = pt[pi], ("pt", pi)
                info[idx] = (Sb, skey, P, pkey, d, n0)
                for (br, qq, kk, rd) in ((0, q1a[par], k1a[par], rd1), (1, q2a[par], k2a[par], rd2)):
                    S.op("pe", lambda e, br=br, qq=qq, kk=kk: e.matmul(
                        Sb[:, br * 256 + n0:(br + 1) * 256], lhsT=kk[0:68, j * 128:(j + 1) * 128],
                        rhs=qq[0:68, qb * QB + n0:(qb + 1) * QB], start=True, stop=True), reads=rd, writes=[skey])

            def emit_rest(idx):
                qb, j = items[idx]
                Sb, skey, P, pkey, d, n0 = info.pop(idx)
                ob_ = 2 if qb % 2 == 0 else 0
                O1, O2 = B[ob_], B[ob_ + 1]
                k1_, k2_ = ("bank", ob_), ("bank", ob_ + 1)
                if n0 == 0:
                    S.op("act", lambda e: e.activation(out=P[:, :], in_=Sb[:, :], func=AF.Exp, scale=0.125),
                         reads=[skey], writes=[pkey])
                else:
                    for br in range(2):
                        S.op("act", lambda e, br=br: e.activation(
                            out=P[:, br * 256 + n0:(br + 1) * 256], in_=Sb[:, br * 256 + n0:(br + 1) * 256], func=AF.Exp, scale=0.125),
                            reads=[skey], writes=[pkey])
                if d >= 0:
                    for br in range(2):
                        S.op("dve", lambda e, br=br: e.tensor_tensor(
                            out=P[:, br * 256 + n0:br * 256 + n0 + 128], in0=P[:, br * 256 + n0:br * 256 + n0 + 128],
                            in1=corr[:, h, :], op=ALU.mult), reads=[pkey, "corr"], writes=[pkey])
                for i in range(max(0, d), 2):
                    for (br, O, ok) in ((0, O1, k1_), (1, O2, k2_)):
                        S.op("pe", lambda e, O=O, i=i, br=br: e.matmul(
                            O[:, i * 129:(i + 1) * 129], lhsT=P[:, br * 256 + i * 128:br * 256 + (i + 1) * 128],
                            rhs=Vh[vpar][:, j, :], start=(j == 0), stop=(j == 2 * qb + i)), reads=[pkey, ("Vh", vpar)], writes=[ok])
                if j != 2 * qb + 1:
                    return
                S.op("dve", lambda e: e.reciprocal(out=rr[:, 0:2], in_=O1[:, 0:258].rearrange("p (i c) -> p i c", i=2)[:, :, 128]),
                     reads=[k1_], writes=["rr01"])
                S.op("dve", lambda e: e.reciprocal(out=rr[:, 2:4], in_=O2[:, 0:258].rearrange("p (i c) -> p i c", i=2)[:, :, 128]),
                     reads=[k2_], writes=["rr23"])
                S.op("dve", lambda e: e.tensor_scalar(out=rr[:, 2:4], in0=rr[:, 2:4], scalar1=lsc[:, 4:5], scalar2=None, op0=ALU.mult),
                     reads=["rr23", "nlam"], writes=["rr23"])
                for i in range(2):
                    qt = qb * 2 + i
                    S.op("dve", lambda e, i=i: e.tensor_scalar(out=ot[:], in0=O1[:, i * 129:i * 129 + 128], scalar1=rr[:, i:i + 1],
                                                               scalar2=None, op0=ALU.mult), reads=[k1_, "rr01"], writes=["ot"])
                    S.op("dve", lambda e, i=i: e.scalar_tensor_tensor(out=ot[:], in0=O2[:, i * 129:i * 129 + 128], scalar=rr[:, 2 + i:3 + i],
                                                                      in1=ot[:], op0=ALU.mult, op1=ALU.add), reads=[k2_, "rr23", "ot"], writes=["ot"])
                    S.op("dve", lambda e: e.scalar_tensor_tensor(out=junk[:], in0=ot[:], scalar=1.0, in1=ot[:], op0=ALU.mult, op1=ALU.mult,
                                                                 accum_out=rr[:, 4:5]), reads=["ot", "junk"], writes=["junk", "ss"])
                    S.op("dve", lambda e: e.tensor_scalar(out=rr[:, 6:7], in0=rr[:, 4:5], scalar1=1.0 / 128.0, scalar2=RMS_EPS, op0=ALU.mult,
                                                          op1=ALU.add), reads=["ss"], writes=["msq"])
                    S.op("pool", lambda e: e.tensor_tensor(out=rr[:, 5:6], in0=rr[:, 6:7], in1=lsc[:, 6:7], op=ALU.pow),
                         reads=["msq", "mhalf"], writes=["rs_"])
                    S.op("dve", lambda e, qt=qt: e.scalar_tensor_tensor(out=ohead[:, qt, :], in0=ot[:], scalar=rr[:, 5:6], in1=gv[:], op0=ALU.mult,
                                                                        op1=ALU.mult), reads=["ot", "rs_", "gv"], writes=["ohead"])

            stride = max(1, len(items) // (len(fill) + 1))
            emit_score(0)
            for idx in range(len(items)):
                if idx + 1 < len(items):
                    emit_score(idx + 1)
                emit_rest(idx)
                if fill and idx % stride == stride - 1:
                    fill.pop(0)()
            while fill:
                fill.pop(0)()

        def head_transposes(h):
            for g in range(2):
                for i in range(8):
                    S.op("pe", lambda e, i=i, g=g: e.transpose(out=C.pTr[:, i * 128:(i + 1) * 128], in_=ohead[:, g * 8 + i, :],
                                                               identity=ident_b[:]), reads=["ohead", "ident_b"], writes=["pTr"])
                S.op("act", lambda e, g=g: e.copy(out=catT[:, h, g * 1024:(g + 1) * 1024], in_=C.pTr[:, :]),
                     reads=["pTr"], writes=["catT"])

        for u_ in inproj_units(0):
            u_()
        for h in range(8):
            attn(h, inproj_units(h + 1) if h + 1 < 8 else [])
            head_transposes(h)

        outproj_ln(C, x_in, x_out, s, catT, Wout, wout_keys, gbc, bbc, xs, yts)
    end_phase(C)


def emit_moe(C, x_in, x_out, w_group, b_group, w_expert, b_expert, w_gate, w_up, w_down, ln_g, ln_b, xslots, yslots):
    new_phase(C, n_f32=6, n_tr=2)
    S, sb, nc = C.S, C.sb, C.nc
    B = C.banks
    NB = CAP // 128
    ident_f = sb("ident_f", [128, 128], F32)
    ident_b = sb("ident_b", [128, 128], BF16)
    tri = sb("tri", [128, 128], BF16)
    ones_b = sb("ones_b", [128, 128], BF16)
    basem1 = sb("basem1", [128, NEXP], F32)
    Wr = sb("Wr", [128, 8, 36], F32)
    brt = sb("brt", [128, 36], F32)
    gbc = sb("gbc", [128, D], F32)
    bbc = sb("bbc", [128, D], F32)
    xs = [sb("xs%d" % i, [128, D], F32) for i in range(4)]
    xb = [sb("xb%d" % i, [128, D], BF16) for i in range(2)]
    xTt = [sb("xTt%d" % i, [128, 8, 128], F32) for i in range(2)]
    lg = [sb("lg%d" % i, [128, 36], F32) for i in range(2)]
    me = [sb("me%d" % i, [128, 32], F32) for i in range(2)]
    oh1 = [sb("oh1_%d" % i, [128, 32], F32) for i in range(2)]
    oh2 = [sb("oh2_%d" % i, [128, 32], F32) for i in range(2)]
    ohb = [sb("ohb%d" % i, [128, 32], BF16) for i in range(2)]
    Q = [sb("Q%d" % i, [128, 32], F32) for i in range(2)]
    cntt = sb("cntt", [128, 32], F32)
    sc = [sb("sc%d" % i, [128, 16], F32) for i in range(2)]
    junk = [sb("junk%d" % i, [128, 32], F32) for i in range(2)]
    sf = [sb("sf%d" % i, [128, 2], F32) for i in range(2)]
    idx_all = sb("idx_all", [128, NT, 2], I32)
    w_all = sb("w_all", [128, NT, 2], F32)
    wg = [sb("wg%d" % i, [128, 8, 512], BF16) for i in range(2)]
    wu = [sb("wu%d" % i, [128, 8, 512], BF16) for i in range(2)]
    wd = [sb("wd%d" % i, [128, 4, D], BF16) for i in range(2)]
    xg = [sb("xg%d" % i, [128, D], BF16) for i in range(2)]
    XT = [sb("XT%d" % i, [128, 8, CAP], BF16) for i in range(2)]
    sg = [sb("sg%d" % i, [128, CAP], F32) for i in range(2)]
    hT = [sb("hT%d" % i, [128, 4, CAP], BF16) for i in range(2)]
    yo = [sb("yo%d" % i, [128, D], BF16) for i in range(2)]
    y1 = [sb("y1_%d" % i, [128, D], BF16) for i in range(4)]
    y2 = [sb("y2_%d" % i, [128, D], BF16) for i in range(4)]
    acc = [sb("acc%d" % i, [128, D], F32) for i in range(4)]
    alloc_ln(C)

    load_const(C, "ident_f", ident_f[:], "ident_f")
    load_const(C, "ident_b", ident_b[:], "ident_b")
    load_const(C, "tri", tri[:], "tri")
    load_const(C, "ones_b", ones_b[:], "ones_b")
    load_const(C, "basem1", basem1[:], "basem1")
    S.dma("sp", lambda e: e.dma_start(out=Wr[:, :, 0:4], in_=w_group.rearrange("(k p) n -> p k n", p=128)), writes=["Wr_g"])
    S.dma("sp", lambda e: e.dma_start(out=Wr[:, :, 4:36], in_=w_expert.rearrange("(k p) n -> p k n", p=128)), writes=["Wr_e"])
    S.dma("sp", lambda e: e.dma_start(out=brt[:, 0:4], in_=b_group.partition_broadcast(128)), writes=["br_g"])
    S.dma("sp", lambda e: e.dma_start(out=brt[:, 4:36], in_=b_expert.partition_broadcast(128)), writes=["br_e"])
    S.dma("sp", lambda e: e.dma_start(out=gbc[:], in_=ln_g.partition_broadcast(128)), writes=["gbc"])
    S.dma("sp", lambda e: e.dma_start(out=bbc[:], in_=ln_b.partition_broadcast(128)), writes=["bbc"])
    S.op("dve", lambda e: e.memset(cntt[:], 0.0), writes=["cntt"])

    def load_expert(e_):
        p = e_ % 2
        keys = []
        for (dst, src, nk, ncol, nm) in ((wg[p], w_gate[e_], 8, 512, "wg"), (wu[p], w_up[e_], 8, 512, "wu"), (wd[p], w_down[e_], 4, D, "wd")):
            v = src.rearrange("(k p) n -> p k n", p=128)
            half = nk // 2
            for a in range(2):
                kk = (nm, p, a)
                keys.append(kk)
                S.dma("pool", lambda e, dst=dst, v=v, a=a, half=half: e.dma_start(out=dst[:, a * half:(a + 1) * half, :],
                                                                                 in_=v[:, a * half:(a + 1) * half, :]), writes=[kk])
        return keys

    wkeys = {0: load_expert(0), 1: load_expert(1)}
    if not getattr(C, "trash_zeroed", False):
        C.trash_zeroed = True
        S.op("dve", lambda e: e.memset(yo[0][0:32, :], 0.0), writes=[("yo", 0, 0), ("yo", 0, 1)])
        S.dma("sp", lambda e: e.dma_start(out=yslots.rearrange("(e s) d -> e s d", s=STRIDE)[:, CAP, :], in_=yo[0][0:32, :]),
              reads=[("yo", 0, 0), ("yo", 0, 1)], writes=["ytrash"])
    scat_keys = []

    def route_tile(t):
        p = t % 2
        P_ = lambda n: (n, p)
        xkey = ("xs", p)
        lg_, me_, oh1_, oh2_, ohb_, Q_, sc_, junk_, sf_, xTt_ = lg[p], me[p], oh1[p], oh2[p], ohb[p], Q[p], sc[p], junk[p], sf[p], xTt[p]
        S.dma("sp", lambda e: e.dma_start(out=xs[p][:], in_=x_in[t * 128:(t + 1) * 128, :]), writes=[xkey])
        S.op("act", lambda e: e.copy(out=xb[p][:], in_=xs[p][:]), reads=[xkey], writes=[("xb", p)])
        for half in range(2):
            bi = 2 * p + half
            bk, bkey = B[bi], ("bank", bi)
            for j in range(4):
                k = half * 4 + j
                S.op("pe", lambda e, bk=bk, j=j, k=k: e.transpose(out=bk[:, j * 128:(j + 1) * 128], in_=xs[p][:, k * 128:(k + 1) * 128],
                                                                  identity=ident_f[:]), reads=[xkey, "ident_f"], writes=[bkey])
            if half == 0:
                S.op("dve", lambda e, bk=bk: e.tensor_copy(out=xTt_[:, 0:4, :], in_=bk[:, :].rearrange("p (j c) -> p j c", j=4)),
                     reads=[bkey], writes=[("xTt", p, 0)])
            else:
                S.op("act", lambda e, bk=bk: e.copy(out=xTt_[:, 4:8, :], in_=bk[:, :].rearrange("p (j c) -> p j c", j=4)),
                     reads=[bkey], writes=[("xTt", p, 1)])
        LR, lrkey = B[4 + p], ("bank", 4 + p)
        for k in range(8):
            S.op("pe", lambda e, k=k: e.matmul(LR[:, 0:36], lhsT=xTt_[:, k, :], rhs=Wr[:, k, :], start=(k == 0), stop=(k == 7)),
                 reads=[("xTt", p, 0), ("xTt", p, 1), "Wr_g", "Wr_e"], writes=[lrkey])
        yield
        S.op("dve", lambda e: e.tensor_tensor(out=lg_[:], in0=LR[:, 0:36], in1=brt[:], op=ALU.add), reads=[lrkey, "br_g", "br_e"], writes=[P_("lg")])
        S.op("dve", lambda e: e.reduce_max(out=sc_[:, 0:1], in_=lg_[:, 0:4], axis=AX.X), reads=[P_("lg")], writes=[P_("gmax")])
        S.op("dve", lambda e: e.tensor_scalar(out=sc_[:, 1:2], in0=sc_[:, 0:1], scalar1=-1.0, scalar2=None, op0=ALU.mult), reads=[P_("gmax")], writes=[P_("ngmax")])
        S.op("act", lambda e: e.activation(out=junk_[:, 0:4], in_=lg_[:, 0:4], func=AF.Exp, bias=sc_[:, 1:2], scale=1.0, accum_out=sc_[:, 2:3]),
             reads=[P_("lg"), P_("ngmax"), P_("junk")], writes=[P_("junk"), P_("gsum")])
        S.op("dve", lambda e: e.tensor_scalar(out=sc_[:, 4:8], in0=lg_[:, 0:4], scalar1=sc_[:, 0:1], scalar2=None, op0=ALU.is_equal),
             reads=[P_("lg"), P_("gmax")], writes=[P_("goh")])
        S.op("dve", lambda e: e.tensor_scalar(out=sc_[:, 4:8], in0=sc_[:, 4:8], scalar1=1e30, scalar2=-1e30, op0=ALU.mult, op1=ALU.add),
             reads=[P_("goh")], writes=[P_("pen")])
        for g in range(4):
            S.op("dve", lambda e, g=g: e.tensor_scalar(out=me_[:, g * 8:(g + 1) * 8], in0=lg_[:, 4 + g * 8:12 + g * 8], scalar1=sc_[:, 4 + g:5 + g],
                                                       scalar2=None, op0=ALU.add), reads=[P_("lg"), P_("pen")], writes=[P_("me")])
        S.op("dve", lambda e: e.reduce_max(out=sc_[:, 8:9], in_=me_[:], axis=AX.X), reads=[P_("me")], writes=[P_("m1")])
        S.op("dve", lambda e: e.tensor_scalar(out=oh1_[:], in0=me_[:], scalar1=sc_[:, 8:9], scalar2=None, op0=ALU.is_equal), reads=[P_("me"), P_("m1")], writes=[P_("oh1")])
        S.op("dve", lambda e: e.scalar_tensor_tensor(out=me_[:], in0=oh1_[:], scalar=-1e30, in1=me_[:], op0=ALU.mult, op1=ALU.add),
             reads=[P_("oh1"), P_("me")], writes=[P_("me")])
        S.op("dve", lambda e: e.reduce_max(out=sc_[:, 9:10], in_=me_[:], axis=AX.X), reads=[P_("me")], writes=[P_("m2")])
        S.op("dve", lambda e: e.tensor_scalar(out=oh2_[:], in0=me_[:], scalar1=sc_[:, 9:10], scalar2=None, op0=ALU.is_equal), reads=[P_("me"), P_("m2")], writes=[P_("oh2")])
        S.op("dve", lambda e: e.tensor_tensor(out=sc_[:, 10:11], in0=sc_[:, 9:10], in1=sc_[:, 8:9], op=ALU.subtract), reads=[P_("m1"), P_("m2")], writes=[P_("dm")])
        S.op("act", lambda e: e.activation(out=sc_[:, 10:11], in_=sc_[:, 10:11], func=AF.Exp), reads=[P_("dm")], writes=[P_("dm")])
        S.op("dve", lambda e: e.tensor_tensor(out=ohb_[:], in0=oh1_[:], in1=oh2_[:], op=ALU.add), reads=[P_("oh1"), P_("oh2")], writes=[P_("ohb")])
        S.op("pe", lambda e: e.matmul(LR[:, 64:96], lhsT=tri[:], rhs=ohb_[:], start=True, stop=True), reads=["tri", P_("ohb"), P_("lg")], writes=[lrkey])
        S.op("pe", lambda e: e.matmul(LR[:, 96:128], lhsT=ones_b[:], rhs=ohb_[:], start=True, stop=True), reads=["ones_b", P_("ohb")], writes=[lrkey])
        yield
        S.op("dve", lambda e: e.reciprocal(out=sc_[:, 3:4], in_=sc_[:, 2:3]), reads=[P_("gsum")], writes=[P_("gw")])
        S.op("dve", lambda e: e.tensor_scalar(out=sc_[:, 10:11], in0=sc_[:, 10:11], scalar1=1.0, scalar2=None, op0=ALU.add), reads=[P_("dm")], writes=[P_("dm")])
        S.op("dve", lambda e: e.reciprocal(out=sc_[:, 11:12], in_=sc_[:, 10:11]), reads=[P_("dm")], writes=[P_("sg1")])
        S.op("dve", lambda e: e.tensor_tensor(out=w_all[:, t, 0:1], in0=sc_[:, 11:12], in1=sc_[:, 3:4], op=ALU.mult), reads=[P_("sg1"), P_("gw")], writes=[("w_all", t)])
        S.op("dve", lambda e: e.tensor_tensor(out=w_all[:, t, 1:2], in0=sc_[:, 3:4], in1=w_all[:, t, 0:1], op=ALU.subtract),
             reads=[P_("gw"), ("w_all", t)], writes=[("w_all", t)])
        S.op("dve", lambda e: e.tensor_tensor(out=Q_[:], in0=LR[:, 64:96], in1=cntt[:], op=ALU.add), reads=[lrkey, "cntt"], writes=[P_("Q")])
        S.op("dve", lambda e: e.tensor_tensor(out=cntt[:], in0=LR[:, 96:128], in1=cntt[:], op=ALU.add), reads=[lrkey, "cntt", P_("Q")], writes=["cntt"])
        S.op("dve", lambda e: e.tensor_scalar(out=Q_[:], in0=Q_[:], scalar1=float(CAP + 1), scalar2=None, op0=ALU.min), reads=[P_("Q")], writes=[P_("Q")])
        S.op("dve", lambda e: e.tensor_tensor(out=Q_[:], in0=Q_[:], in1=basem1[:], op=ALU.add), reads=[P_("Q"), "basem1"], writes=[P_("Q")])
        S.op("dve", lambda e: e.scalar_tensor_tensor(out=junk_[:], in0=oh1_[:], scalar=1.0, in1=Q_[:], op0=ALU.mult, op1=ALU.mult, accum_out=sf_[:, 0:1]),
             reads=[P_("oh1"), P_("Q"), P_("junk")], writes=[P_("junk"), P_("sf0")])
        S.op("dve", lambda e: e.scalar_tensor_tensor(out=junk_[:], in0=oh2_[:], scalar=1.0, in1=Q_[:], op0=ALU.mult, op1=ALU.mult, accum_out=sf_[:, 1:2]),
             reads=[P_("oh2"), P_("Q"), P_("junk")], writes=[P_("junk"), P_("sf1")])
        S.op("dve", lambda e: e.tensor_copy(out=idx_all[:, t, :], in_=sf_[:]), reads=[P_("sf0"), P_("sf1")], writes=[("idx", t)])
        for kk in range(2):
            sk = ("scat", t, kk)
            scat_keys.append(sk)
            S.dma("pool", lambda e, kk=kk: e.indirect_dma_start(
                out=xslots, out_offset=bass.IndirectOffsetOnAxis(ap=idx_all[:, t, kk:kk + 1], axis=0), in_=xb[p][:], in_offset=None),
                reads=[("xb", p), ("idx", t)], writes=[sk])
        yield

    for t0 in range(0, NT, 2):
        gens = [route_tile(t0), route_tile(t0 + 1)]
        while gens:
            for g_ in list(gens):
                try:
                    next(g_)
                except StopIteration:
                    gens.remove(g_)

    ycnt = 0
    ykeys = []
    xgc = [0]

    def prep_block(e_, blk):
        p = e_ % 2
        gp = xgc[0] % 2
        xgc[0] += 1
        pT, pTk = C.pTrs[gp], ("pTr", gp)
        row0 = e_ * STRIDE + blk * 128
        S.dma("sp", lambda e: e.dma_start(out=xg[gp][:], in_=xslots[row0:row0 + 128, :]), reads=scat_keys, writes=[("xg", gp)])
        for k in range(8):
            S.op("pe", lambda e, k=k: e.transpose(out=pT[:, k * 128:(k + 1) * 128], in_=xg[gp][:, k * 128:(k + 1) * 128],
                                                  identity=ident_b[:]), reads=[("xg", gp), "ident_b"], writes=[pTk])
        S.op("dve", lambda e: e.tensor_copy(out=XT[p][:, :, blk * 128:(blk + 1) * 128],
                                            in_=pT[:, :].rearrange("p (k c) -> p k c", k=8)), reads=[pTk], writes=[("XT", p)])

    for blk in range(NB):
        prep_block(0, blk)
    for e_ in range(NEXP):
        p = e_ % 2
        if e_ >= 1 and e_ + 1 < NEXP:
            wkeys[e_ + 1] = load_expert(e_ + 1)
        wk = wkeys[e_]
        for hc in range(4):
            G, gkey = B[0 + hc % 2], ("bank", hc % 2)
            U, ukey = B[2 + hc % 2], ("bank", 2 + hc % 2)
            for (bk, bkey, w_) in ((G, gkey, wg[p]), (U, ukey, wu[p])):
                for k in range(8):
                    S.op("pe", lambda e, bk=bk, k=k, w_=w_, hc=hc, p=p: e.matmul(bk[:, 0:CAP], lhsT=w_[:, k, hc * 128:(hc + 1) * 128], rhs=XT[p][:, k, :],
                                                                                 start=(k == 0), stop=(k == 7)), reads=wk + [("XT", p)], writes=[bkey])
            S.op("act", lambda e, G=G, hc=hc: e.activation(out=sg[hc % 2][:], in_=G[:, 0:CAP], func=AF.Silu), reads=[gkey], writes=[("sg", hc % 2)])
            S.op("dve", lambda e, U=U, hc=hc, p=p: e.tensor_tensor(out=hT[p][:, hc, :], in0=U[:, 0:CAP], in1=sg[hc % 2][:], op=ALU.mult),
                 reads=[ukey, ("sg", hc % 2)], writes=[("hT", p)])
            if e_ + 1 < NEXP and hc < NB:
                prep_block(e_ + 1, hc)
        if e_ + 1 < NEXP:
            for blk in range(4, NB):
                prep_block(e_ + 1, blk)
        for blk in range(NB):
            yp = ycnt % 2
            ycnt += 1
            for half in range(2):
                Y, ykey = B[4 + half], ("bank", 4 + half)
                for hc in range(4):
                    S.op("pe", lambda e, Y=Y, hc=hc, blk=blk, half=half, p=p: e.matmul(
                        Y[:, :], lhsT=hT[p][:, hc, blk * 128:(blk + 1) * 128], rhs=wd[p][:, hc, half * 512:(half + 1) * 512],
                        start=(hc == 0), stop=(hc == 3)), reads=wk + [("hT", p)], writes=[ykey])
                if half == 0:
                    S.op("act", lambda e, Y=Y, yp=yp: e.copy(out=yo[yp][:, 0:512], in_=Y[:, :]), reads=[ykey], writes=[("yo", yp, 0)])
                else:
                    S.op("dve", lambda e, Y=Y, yp=yp: e.tensor_copy(out=yo[yp][:, 512:1024], in_=Y[:, :]), reads=[ykey], writes=[("yo", yp, 1)])
            row0 = e_ * STRIDE + blk * 128
            yk = ("ysl", e_, blk)
            ykeys.append(yk)
            S.dma("sp", lambda e, row0=row0, yp=yp: e.dma_start(out=yslots[row0:row0 + 128, :], in_=yo[yp][:]),
                  reads=[("yo", yp, 0), ("yo", yp, 1)], writes=[yk])

    L = C.ln

    def combine_tile(t):
        q = t % 4
        xkey, akey = ("xs", q), ("acc", q)
        a = acc[q]
        K_ = lambda n: (n, q)
        st, mv, rstd, nmr = L["stats"][:, q, :], L["mv"][:, q, :], L["rstd"][:, q:q + 1], L["nmr"][:, q:q + 1]
        S.dma("sp", lambda e: e.dma_start(out=xs[q][:], in_=x_in[t * 128:(t + 1) * 128, :]), writes=[xkey])
        for (yy, nm, kk) in ((y1[q], "y1", 0), (y2[q], "y2", 1)):
            S.dma("pool", lambda e, kk=kk, yy=yy: e.indirect_dma_start(
                out=yy[:], out_offset=None, in_=yslots, in_offset=bass.IndirectOffsetOnAxis(ap=idx_all[:, t, kk:kk + 1], axis=0)),
                reads=ykeys + [("idx", t), "ytrash"], writes=[(nm, q)])
        yield
        S.op("act", lambda e: e.activation(out=a[:], in_=y2[q][:], func=AF.Identity, scale=w_all[:, t, 1:2]),
             reads=[("y2", q), ("w_all", t)], writes=[akey])
        S.op("dve", lambda e: e.scalar_tensor_tensor(out=a[:], in0=y1[q][:], scalar=w_all[:, t, 0:1], in1=a[:], op0=ALU.mult, op1=ALU.add),
             reads=[("y1", q), ("w_all", t), akey], writes=[akey])
        S.op("dve", lambda e: e.scalar_tensor_tensor(out=a[:], in0=xs[q][:], scalar=ALPHA, in1=a[:], op0=ALU.mult, op1=ALU.add),
             reads=[xkey, akey], writes=[akey])
        S.op("dve", lambda e: e.bn_stats(out=st[:, 0:6], in_=a[:, 0:512]), reads=[akey], writes=[K_("ln_stats")])
        S.op("dve", lambda e: e.bn_stats(out=st[:, 6:12], in_=a[:, 512:1024]), reads=[akey], writes=[K_("ln_stats")])
        S.op("dve", lambda e: e.bn_aggr(out=mv, in_=st), reads=[K_("ln_stats")], writes=[K_("ln_mv")])
        S.op("act", lambda e: e.activation(out=rstd, in_=mv[:, 1:2], func=AF.Ln, bias=L["eps"][:], scale=1.0),
             reads=[K_("ln_mv"), "ln_eps"], writes=[K_("ln_rstd")])
        S.op("act", lambda e: e.activation(out=rstd, in_=rstd, func=AF.Exp, scale=-0.5), reads=[K_("ln_rstd")], writes=[K_("ln_rstd")])
        yield
        S.op("dve", lambda e: e.tensor_scalar(out=nmr, in0=mv[:, 0:1], scalar1=-1.0, scalar2=rstd, op0=ALU.mult, op1=ALU.mult),
             reads=[K_("ln_mv"), K_("ln_rstd")], writes=[K_("ln_nmr")])
        S.op("act", lambda e: e.activation(out=a[:], in_=a[:], func=AF.Identity, bias=nmr, scale=rstd),
             reads=[akey, K_("ln_nmr"), K_("ln_rstd")], writes=[akey])
        yield
        S.op("dve", lambda e: e.tensor_tensor(out=a[:], in0=a[:], in1=gbc[:], op=ALU.mult), reads=[akey, "gbc"], writes=[akey])
        S.op("dve", lambda e: e.tensor_tensor(out=a[:], in0=a[:], in1=bbc[:], op=ALU.add), reads=[akey, "bbc"], writes=[akey])
        S.dma("sp", lambda e: e.dma_start(out=x_out[t * 128:(t + 1) * 128, :], in_=a[:]), reads=[akey], writes=[("xout", t)])
        yield

    active = []
    for r in range(NT + 4):
        for g_ in list(active):
            try:
                next(g_)
            except StopIteration:
                active.remove(g_)
        if r < NT:
            g_ = combine_tile(r)
            next(g_)
            active.append(g_)
    end_phase(C)


def build_program(phases):
    nc = bass.Bass("TRN2", target_bir_lowering=False)
    C = Ctx()
    C.nc = nc
    C.debug = False
    outer = ExitStack()
    C.S = Sched(nc, outer)
    din = lambda name, shape, dt=F32: nc.dram_tensor(name, shape, dt, kind="ExternalInput").ap()
    C.consts = {k: din("c_" + k, shp, dt) for k, (shp, dt) in CONST_SPECS.items()}
    x_cur = din("x", [NTOK, D])
    need_moe = any(p[0] == "moe" for p in phases)
    if need_moe:
        xslots = nc.dram_tensor("xslots", [NSLOT, D], BF16, kind="Internal").ap()
        yslots = nc.dram_tensor("yslots", [NSLOT, D], BF16, kind="Internal").ap()
    for pi, (kind, layer) in enumerate(phases):
        last = pi == len(phases) - 1
        if last:
            x_next = nc.dram_tensor("y", [NTOK, D], F32, kind="ExternalOutput").ap()
        else:
            x_next = nc.dram_tensor("xact%d" % pi, [NTOK, D], F32, kind="Internal").ap()
        pre = "p%d_" % pi
        if kind == "even":
            emit_even(C, x_cur, x_next, din(pre + "w_in", [D, EVEN_IN]), din(pre + "b_forget", [8]), din(pre + "conv_w", [512, 3]),
                      din(pre + "w_out", [D, D]), din(pre + "ln_g", [D]), din(pre + "ln_b", [D]))
        elif kind == "odd":
            emit_odd(C, layer, x_cur, x_next, din(pre + "w_in", [D, 3 * D]), din(pre + "lam_q1", [64]), din(pre + "lam_k1", [64]),
                     din(pre + "lam_q2", [64]), din(pre + "lam_k2", [64]), din(pre + "subln_g", [128]), din(pre + "w_out", [D, D]),
                     din(pre + "ln_g", [D]), din(pre + "ln_b", [D]))
        else:
            emit_moe(C, x_cur, x_next, din(pre + "w_group", [D, 4]), din(pre + "b_group", [4]), din(pre + "w_expert", [D, 32]),
                     din(pre + "b_expert", [32]), din(pre + "w_gate", [NEXP, D, 512]), din(pre + "w_up", [NEXP, D, 512]),
                     din(pre + "w_down", [NEXP, 512, D]), din(pre + "ln_g", [D]), din(pre + "ln_b", [D]), xslots, yslots)
        x_cur = x_next
    outer.close()
    return nc


def phase_inputs(pi, kind, layer, inp):
    pre = "p%d_" % pi
    i = layer // 2
    c = np.ascontiguousarray
    if kind == "even":
        return {pre + "w_in": c(inp["ab_w_in"][i]), pre + "b_forget": c(inp["ab_b_forget"][i]), pre + "conv_w": c(inp["ab_conv_w"][i]),
                pre + "w_out": c(inp["ab_w_out"][i]), pre + "ln_g": c(inp["ln_mix_g"][layer]), pre + "ln_b": c(inp["ln_mix_b"][layer])}
    if kind == "odd":
        return {pre + "w_in": c(inp["c_w_in"][i]), pre + "lam_q1": c(inp["c_lam_q1"][i]), pre + "lam_k1": c(inp["c_lam_k1"][i]),
                pre + "lam_q2": c(inp["c_lam_q2"][i]), pre + "lam_k2": c(inp["c_lam_k2"][i]), pre + "subln_g": c(inp["c_subln_g"][i]),
                pre + "w_out": c(inp["c_w_out"][i]), pre + "ln_g": c(inp["ln_mix_g"][layer]), pre + "ln_b": c(inp["ln_mix_b"][layer])}
    return {pre + "w_group": c(inp["moe_w_group"][layer]), pre + "b_group": c(inp["moe_b_group"][layer]),
            pre + "w_expert": c(inp["moe_w_expert"][layer]), pre + "b_expert": c(inp["moe_b_expert"][layer]),
            pre + "w_gate": c(inp["moe_w_gate"][layer]), pre + "w_up": c(inp["moe_w_up"][layer]), pre + "w_down": c(inp["moe_w_down"][layer]),
            pre + "ln_g": c(inp["ln_ffn_g"][layer]), pre + "ln_b": c(inp["ln_ffn_b"][layer])}


ALL_PHASES = []
for _l in range(DEPTH):
    ALL_PHASES.append(("even" if _l % 2 == 0 else "odd", _l))
    ALL_PHASES.append(("moe", _l))

_PROG_CACHE = {}


def run_phases(phases, x_shards, inp, core_ids=None, trace=False):
    key = tuple((k, (l if k == "odd" else 0)) for k, l in phases)
    if key not in _PROG_CACHE:
        _PROG_CACHE[key] = build_program(phases)
    nc = _PROG_CACHE[key]
    consts = {"c_" + k: v for k, v in host_consts().items()}
    shared = dict(consts)
    for pi, (kind, layer) in enumerate(phases):
        shared.update(phase_inputs(pi, kind, layer, inp))
    in_maps = []
    for xs in x_shards:
        m = dict(shared)
        m["x"] = np.ascontiguousarray(xs)
        in_maps.append(m)
    core_ids = core_ids if core_ids is not None else list(range(len(x_shards)))
    if trace:
        res = run_bass_kernel_spmd(nc, in_maps, core_ids=core_ids, trace=True)
        print("TRACE exec_time_ns", res.exec_time_ns)
    else:
        res = run_bass_kernel_spmd(nc, in_maps, core_ids=core_ids)
    return [r["y"] for r in res.results]


def kernel(**inputs):
    inp = {k: np.asarray(v) for k, v in inputs.items()}
    x = inp["x"].astype(np.float32, copy=False)
    shards = [x[NSEQ * c:NSEQ * (c + 1)].reshape(NTOK, D) for c in range(N_CORES)]
    if MODE == "fused":
        outs = run_phases(ALL_PHASES, shards, inp)
    else:
        outs = shards
        for ph in ALL_PHASES:
            outs = run_phases([ph], outs, inp)
    out = np.stack([o.reshape(NSEQ, SEQ, D) for o in outs], axis=0).reshape(N_CORES * NSEQ, SEQ, D)
    return out.astype(np.float32, copy=False)
```
